# Optimizing a Trainium2 kernel written in Bass

```python
import math
import jax, jax.numpy as jnp
from jax import lax
import numpy as np

D_MODEL = 2048
BATCH = 1
SEQ = 8192
DEPTH = 2

D_SSM = D_MODEL // 2
SSM_GROUP = 16
N_SSM_GROUPS = D_SSM // SSM_GROUP
SSM_STATE = 64
N_DIR = 2
D_ATTN = D_MODEL // 2
N_HEADS = 8
HEAD_DIM = D_ATTN // (2 * N_HEADS)
ROT_DIM = HEAD_DIM // 4
ROPE_THETA = 500000.0
Q_BLOCK = 128
N_BRANCH = 2
IN_COLS = D_SSM + 3 * D_ATTN + N_BRANCH * D_MODEL
N_EXPERTS = 16
D_EXPERT = 1024
CAPACITY_FACTOR = 2
EPS = 1e-6

kernel_name = "hybrid_s5_diffattn_ec_moe_encoder"


def rms_norm(x, g):
    xf = x.astype(jnp.float32)
    y = xf * lax.rsqrt(jnp.mean(xf * xf, axis=-1, keepdims=True) + EPS) * g.astype(jnp.float32)
    return y.astype(x.dtype)


def lambda_init_for(layer_idx):
    return 0.8 - 0.6 * math.exp(-0.3 * layer_idx)


def rope_tables(seq_len):
    pos = jnp.arange(seq_len, dtype=jnp.float32)
    inv = jnp.power(ROPE_THETA, -jnp.arange(0, ROT_DIM, 2, dtype=jnp.float32) / ROT_DIM)
    ang = pos[:, None] * inv[None, :]
    return jnp.cos(ang), jnp.sin(ang)


def apply_partial_rope(x, cos, sin):
    half = ROT_DIM // 2
    c = cos[None, :, None, None, :].astype(x.dtype)
    s = sin[None, :, None, None, :].astype(x.dtype)
    x1 = x[..., :half]
    x2 = x[..., half:ROT_DIM]
    rot = jnp.concatenate([x1 * c - x2 * s, x2 * c + x1 * s], axis=-1)
    return jnp.concatenate([rot, x[..., ROT_DIM:]], axis=-1)


def _ssm_combine(e1, e2):
    a1, b1 = e1
    a2, b2 = e2
    return a1 * a2, a2 * b1 + b2


def s5_bidirectional(u, a_re, a_im, log_step, b_re, b_im, c_re, c_im, d_skip):
    B, L, _ = u.shape
    f32 = jnp.float32
    uf = u.astype(f32)
    ug = uf.reshape(B, L, N_SSM_GROUPS, SSM_GROUP).astype(jnp.complex64)
    lam = lax.complex(a_re.astype(f32), a_im.astype(f32))
    step = jnp.exp(log_step.astype(f32))[..., None]
    lam_bar = jnp.exp(lam * step)
    b_bar = ((lam_bar - 1.0) / lam)[..., None] * lax.complex(b_re.astype(f32), b_im.astype(f32))
    c = lax.complex(c_re.astype(f32), c_im.astype(f32))
    y = d_skip.astype(f32) * uf
    for direction in range(N_DIR):
        bu = jnp.einsum('blgh,gph->blgp', ug, b_bar[direction])
        a = jnp.broadcast_to(lam_bar[direction], bu.shape)
        _, states = lax.associative_scan(_ssm_combine, (a, bu), reverse=(direction == 1), axis=1)
        y = y + jnp.real(jnp.einsum('blgp,ghp->blgh', states, c[direction])).reshape(B, L, D_SSM)
    return y.astype(u.dtype)


def diff_attention(q, k, v, q_g, k_g, lq1, lk1, lq2, lk2, sub_g, lam_init, cos, sin):
    B, L, _ = q.shape
    q = q.reshape(B, L, N_HEADS, 2, HEAD_DIM)
    k = k.reshape(B, L, N_HEADS, 2, HEAD_DIM)
    v = v.reshape(B, L, N_HEADS, 2 * HEAD_DIM)
    q = apply_partial_rope(rms_norm(q, q_g), cos, sin) * (HEAD_DIM ** -0.5)
    k = apply_partial_rope(rms_norm(k, k_g), cos, sin)
    f32 = jnp.float32
    lam = (jnp.exp(jnp.sum(lq1.astype(f32) * lk1.astype(f32)))
           - jnp.exp(jnp.sum(lq2.astype(f32) * lk2.astype(f32))) + lam_init)
    nb = L // Q_BLOCK
    qb = q.reshape(B, nb, Q_BLOCK, N_HEADS, 2, HEAD_DIM).transpose(1, 0, 3, 4, 2, 5)
    kt = k.transpose(0, 2, 3, 1, 4)
    vt = v.transpose(0, 2, 1, 3)

    def block(q_blk):
        s = jnp.einsum('bhcqd,bhckd->bhcqk', q_blk, kt).astype(f32)
        p = jax.nn.softmax(s, axis=-1)
        a = p[:, :, 0] - lam * p[:, :, 1]
        return jnp.einsum('bhqk,bhkv->bhqv', a.astype(vt.dtype), vt)

    o = lax.map(block, qb)
    o = o.transpose(1, 0, 3, 2, 4).reshape(B, L, N_HEADS, 2 * HEAD_DIM)
    o = rms_norm(o, sub_g) * (1.0 - lam_init)
    return o.reshape(B, L, D_ATTN)


def expert_choice_moe(x, w_router, w_gate, w_up, w_down):
    B, T, D = x.shape
    cap = max(1, CAPACITY_FACTOR * T // N_EXPERTS)
    logits = jnp.einsum('btd,de->bte', x, w_router).astype(jnp.float32)
    aff = jax.nn.softmax(logits, axis=-1)
    gate, idx = lax.top_k(aff.transpose(0, 2, 1), cap)
    xe = jax.vmap(lambda xb, ib: xb[ib])(x, idx)
    h = jax.nn.silu(jnp.einsum('becd,edf->becf', xe, w_gate)) * jnp.einsum('becd,edf->becf', xe, w_up)
    ye = jnp.einsum('becf,efd->becd', h, w_down) * gate[..., None].astype(x.dtype)
    return jax.vmap(lambda yb, ib: jnp.zeros((T, D), yb.dtype).at[ib.reshape(-1)].add(yb.reshape(-1, D)))(ye, idx)


def setup_inputs(seed: int = 0) -> dict:
    key = jax.random.key(seed)
    ks = jax.random.split(key, 32)
    f32 = jnp.float32
    G, P, H = N_SSM_GROUPS, SSM_STATE, SSM_GROUP

    def nrm(k, shape, scale):
        return jax.random.normal(k, shape, f32) * scale

    def gain(k, shape):
        return 1.0 + 0.02 * jax.random.normal(k, shape, f32)

    n_idx = jnp.arange(P, dtype=f32)
    a_re = -0.5 + 0.01 * jax.random.normal(ks[3], (DEPTH, N_DIR, G, P), f32)
    a_im = math.pi * n_idx + 0.01 * jax.random.normal(ks[4], (DEPTH, N_DIR, G, P), f32)
    log_step = jax.random.uniform(ks[5], (DEPTH, N_DIR, G), f32, math.log(1e-3), math.log(1e-1))
    return {
        "x": jax.random.normal(ks[0], (BATCH, SEQ, D_MODEL), f32),
        "mix_norm_g": gain(ks[1], (DEPTH, D_MODEL)),
        "w_in": nrm(ks[2], (DEPTH, D_MODEL, IN_COLS), D_MODEL ** -0.5),
        "gate_b": nrm(ks[6], (DEPTH, N_BRANCH, D_MODEL), 0.01),
        "ssm_a_re": a_re,
        "ssm_a_im": a_im,
        "ssm_log_step": log_step,
        "ssm_b_re": nrm(ks[7], (DEPTH, N_DIR, G, P, H), (2 * H) ** -0.5),
        "ssm_b_im": nrm(ks[8], (DEPTH, N_DIR, G, P, H), (2 * H) ** -0.5),
        "ssm_c_re": nrm(ks[9], (DEPTH, N_DIR, G, H, P), P ** -0.5),
        "ssm_c_im": nrm(ks[10], (DEPTH, N_DIR, G, H, P), P ** -0.5),
        "ssm_d": nrm(ks[11], (DEPTH, D_SSM), 1.0),
        "w_glu": nrm(ks[12], (DEPTH, D_SSM, D_SSM), D_SSM ** -0.5),
        "q_norm_g": gain(ks[13], (DEPTH, HEAD_DIM)),
        "k_norm_g": gain(ks[14], (DEPTH, HEAD_DIM)),
        "lambda_q1": nrm(ks[15], (DEPTH, HEAD_DIM), 0.1),
        "lambda_k1": nrm(ks[16], (DEPTH, HEAD_DIM), 0.1),
        "lambda_q2": nrm(ks[17], (DEPTH, HEAD_DIM), 0.1),
        "lambda_k2": nrm(ks[18], (DEPTH, HEAD_DIM), 0.1),
        "subln_g": gain(ks[19], (DEPTH, 2 * HEAD_DIM)),
        "w_br_ssm": nrm(ks[20], (DEPTH, D_SSM, D_MODEL), D_SSM ** -0.5),
        "w_br_attn": nrm(ks[21], (DEPTH, D_ATTN, D_MODEL), D_ATTN ** -0.5),
        "w_out": nrm(ks[22], (DEPTH, D_MODEL, D_MODEL), D_MODEL ** -0.5),
        "ffn_norm_g": gain(ks[23], (DEPTH, D_MODEL)),
        "w_router": nrm(ks[24], (DEPTH, D_MODEL, N_EXPERTS), D_MODEL ** -0.5),
        "w_e_gate": nrm(ks[25], (DEPTH, N_EXPERTS, D_MODEL, D_EXPERT), D_MODEL ** -0.5),
        "w_e_up": nrm(ks[26], (DEPTH, N_EXPERTS, D_MODEL, D_EXPERT), D_MODEL ** -0.5),
        "w_e_down": nrm(ks[27], (DEPTH, N_EXPERTS, D_EXPERT, D_MODEL), D_EXPERT ** -0.5),
    }


def reference(x, mix_norm_g, w_in, gate_b, ssm_a_re, ssm_a_im, ssm_log_step, ssm_b_re, ssm_b_im,
              ssm_c_re, ssm_c_im, ssm_d, w_glu, q_norm_g, k_norm_g, lambda_q1, lambda_k1,
              lambda_q2, lambda_k2, subln_g, w_br_ssm, w_br_attn, w_out, ffn_norm_g, w_router,
              w_e_gate, w_e_up, w_e_down):
    B, L, _ = x.shape
    cos, sin = rope_tables(L)
    splits = [D_SSM, D_SSM + D_ATTN, D_SSM + 2 * D_ATTN, D_SSM + 3 * D_ATTN]
    for l in range(DEPTH):
        lam_init = lambda_init_for(l)
        h = rms_norm(x, mix_norm_g[l])
        proj = h @ w_in[l]
        u_ssm, q, k, v, gate_logits = jnp.split(proj, splits, axis=-1)
        y_s = s5_bidirectional(u_ssm, ssm_a_re[l], ssm_a_im[l], ssm_log_step[l], ssm_b_re[l],
                               ssm_b_im[l], ssm_c_re[l], ssm_c_im[l], ssm_d[l])
        y_s = jax.nn.gelu(y_s)
        y_s = y_s * jax.nn.sigmoid(y_s @ w_glu[l])
        y_a = diff_attention(q, k, v, q_norm_g[l], k_norm_g[l], lambda_q1[l], lambda_k1[l],
                             lambda_q2[l], lambda_k2[l], subln_g[l], lam_init, cos, sin)
        g = jax.nn.sigmoid((gate_logits.reshape(B, L, N_BRANCH, D_MODEL) + gate_b[l]).astype(jnp.float32)).astype(x.dtype)
        merged = g[:, :, 0] * (y_s @ w_br_ssm[l]) + g[:, :, 1] * (y_a @ w_br_attn[l])
        x = x + merged @ w_out[l]
        x = x + expert_choice_moe(rms_norm(x, ffn_norm_g[l]), w_router[l], w_e_gate[l],
                                  w_e_up[l], w_e_down[l])
    return x
```

```python
import contextlib
import math
import numpy as np
import ml_dtypes
import concourse.bass as bass
import concourse.mybir as mybir
from concourse.bass_utils import run_bass_kernel_spmd

F32 = mybir.dt.float32
BF16 = mybir.dt.bfloat16
I32 = mybir.dt.int32
ALU = mybir.AluOpType
AF = mybir.ActivationFunctionType
AX = mybir.AxisListType
IOA = bass.IndirectOffsetOnAxis

NCORES = 8
L = 8192
D = 2048
TPC = L // NCORES
EPS = 1e-6
COMPUTE = ("tensor", "vector", "scalar", "gpsimd")
BIG = 1.0e6


class Prog:
    def __init__(self, nc, same_engine_sync=True):
        self.nc = nc
        self.ops = []
        self.last_w = {}
        self.readers = {}
        self.stack = contextlib.ExitStack()
        self.same_engine_sync = same_engine_sync

    def sb(self, name, shape, dtype):
        return self.stack.enter_context(self.nc.sbuf_tensor(name, list(shape), dtype))

    def ps(self, name, shape, dtype=F32):
        return self.stack.enter_context(self.nc.psum_tensor(name, list(shape), dtype))

    def op(self, eng, fn, reads=(), writes=(), dma_key=None):
        idx = len(self.ops)
        deps = set()
        for k in reads:
            if k in self.last_w:
                deps.add(self.last_w[k])
        for k in writes:
            if k in self.last_w:
                deps.add(self.last_w[k])
            deps.update(self.readers.get(k, ()))
        for k in reads:
            self.readers.setdefault(k, []).append(idx)
        for k in writes:
            self.last_w[k] = idx
            self.readers[k] = []
        self.ops.append(dict(eng=eng, fn=fn, deps=deps, dma_key=dma_key, rw=(tuple(reads), tuple(writes))))
        return idx

    def alias(self, new_keys, old_keys):
        acc = []
        for k in old_keys:
            if k in self.last_w:
                acc.append(self.last_w[k])
            acc.extend(self.readers.get(k, ()))
        for k in new_keys:
            self.readers.setdefault(k, []).extend(acc)

    def I(self, eng, meth, reads, writes, *a, **kw):
        return self.op(eng, lambda e: getattr(e, meth)(*a, **kw), reads=reads, writes=writes)

    def dma(self, eng, out, in_, reads=(), writes=(), key=None, **kw):
        if key is None:
            key = writes[0] if writes else reads[0]
        return self.op(eng, lambda e: e.dma_start(out=out, in_=in_, **kw),
                       reads=reads, writes=writes, dma_key=("dma", key))

    def idma(self, out, out_off, in_, in_off, reads, writes, key, bounds=None):
        regs = self.__dict__.setdefault("_bregs", {})

        def fn(e):
            if bounds is None:
                return e.indirect_dma_start(out=out, out_offset=out_off, in_=in_, in_offset=in_off)
            if bounds not in regs:
                regs[bounds] = e.to_reg(bounds)
            return e.indirect_dma_start(out=out, out_offset=out_off, in_=in_, in_offset=in_off,
                                        bounds_check=regs[bounds], oob_is_err=False)
        return self.op("gpsimd", fn, reads=reads, writes=writes, dma_key=("dma", key))

    def emit(self):
        nc = self.nc
        ops = self.ops
        ses = self.same_engine_sync

        def skip(p, o):
            return (p["dma_key"] is None and o["dma_key"] is None and p["eng"] == o["eng"]
                    and (p["eng"] == "tensor" or not ses))

        signal = [o["dma_key"] is not None for o in ops]
        for o in ops:
            for d in o["deps"]:
                if not skip(ops[d], o):
                    signal[d] = True
        sem_names = [("eng", e) for e in COMPUTE]
        for o in ops:
            if o["dma_key"] is not None and o["dma_key"] not in sem_names:
                sem_names.append(o["dma_key"])
        sems = {}
        for n, sn in enumerate(sem_names):
            sems[sn] = self.stack.enter_context(nc.semaphore("s%d" % n))
        counter = {sn: 0 for sn in sem_names}
        sig = [None] * len(ops)
        for i, o in enumerate(ops):
            if not signal[i]:
                continue
            sk = o["dma_key"] if o["dma_key"] is not None else ("eng", o["eng"])
            counter[sk] += 16 if o["dma_key"] is not None else 1
            sig[i] = (sk, counter[sk])
        by_eng = {}
        for i, o in enumerate(ops):
            by_eng.setdefault(o["eng"], []).append(i)
        by_eng.setdefault("sync", [])
        with nc.Block() as block:
            for en in list(by_eng.keys()):
                def body(e, en=en):
                    waited = {}
                    for i in by_eng[en]:
                        o = ops[i]
                        need = {}
                        for d in o["deps"]:
                            if sig[d] is None or skip(ops[d], o):
                                continue
                            sk, v = sig[d]
                            if v > need.get(sk, 0):
                                need[sk] = v
                        for sk, v in need.items():
                            if waited.get(sk, 0) >= v:
                                continue
                            e.wait_ge(sems[sk], v)
                            waited[sk] = v
                        try:
                            ins = o["fn"](e)
                        except Exception:
                            print("EMIT FAIL op", i, en, o["rw"], o["dma_key"])
                            raise
                        if sig[i] is not None:
                            ins.then_inc(sems[sig[i][0]], 16 if o["dma_key"] is not None else 1)
                    if en == "sync":
                        for sk in sem_names:
                            if counter[sk] > 0:
                                e.wait_ge(sems[sk], counter[sk])
                getattr(block, en)(body)
        self.stack.close()


def new_nc():
    return bass.Bass("TRN2", target_bir_lowering=False)


def din(nc, name, shape, dt=F32):
    return nc.dram_tensor(name, list(shape), dt, kind="ExternalInput").ap()


def dout(nc, name, shape, dt=F32):
    return nc.dram_tensor(name, list(shape), dt, kind="ExternalOutput").ap()


class WStream:
    CB = 128

    def __init__(self, P, nslots=4, ktmax=16):
        self.P = P
        self.n = nslots
        self.wst = [P.sb("wst%d" % i, [128, ktmax, self.CB], F32) for i in range(nslots)]
        self.wbf = [P.sb("wbf%d" % i, [128, ktmax, self.CB], BF16) for i in range(nslots)]
        self.ctr = 0

    def load(self, W, KT, cb):
        s = self.ctr % self.n
        self.ctr += 1
        P = self.P
        CB = self.CB
        P.dma("sync", self.wst[s][:, 0:KT, :], W[:, cb * CB:(cb + 1) * CB].rearrange("(k p) c -> p k c", p=128),
              writes=["wst%d" % s])
        P.I("gpsimd", "tensor_copy", ["wst%d" % s], ["wbf%d" % s], out=self.wbf[s][:, 0:KT, :],
            in_=self.wst[s][:, 0:KT, :])
        return self.wbf[s], "wbf%d" % s


class PsRing:
    def __init__(self, P, n, prefix="ps"):
        self.t = [P.ps("%s%d" % (prefix, i), [128, 512], F32) for i in range(n)]
        self.k = ["%s%d" % (prefix, i) for i in range(n)]
        self.c = 0

    def next(self):
        i = self.c % len(self.t)
        self.c += 1
        return self.t[i], self.k[i]


def linear(P, ws, pr, srcs, N, T, evac):
    for m in range(N // 128):
        wb = [ws.load(W, KT, m) for (W, a, KT, ak) in srcs]
        for h in range(T // 512):
            outs = []
            for si, (W, a, KT, ak) in enumerate(srcs):
                ps, pk = pr.next()
                for k in range(KT):
                    P.I("tensor", "matmul", [wb[si][1], ak], [pk], ps[:], lhsT=wb[si][0][:, k, :],
                        rhs=a[:, k, h * 512:(h + 1) * 512], start=(k == 0), stop=(k == KT - 1))
                outs.append((ps, pk))
            evac(m, h, outs)


def rstd_from_ps(P, ps, pk, out_ap, okey, tmp, tkey, scale):
    P.I("scalar", "activation", [pk], [tkey], out=tmp, in_=ps, func=AF.Sqrt, bias=EPS, scale=scale)
    P.I("vector", "reciprocal", [tkey], [okey], out=out_ap, in_=tmp)


def build_P():
    nc = new_nc()
    xT = din(nc, "xT", [D, TPC])
    g = din(nc, "g", [128, 16])
    W = din(nc, "W", [D, 8192])
    out = dout(nc, "projT", [8192, TPC])
    P = Prog(nc)
    xf = [P.sb("xf%d" % i, [128, TPC], F32) for i in range(2)]
    hT = P.sb("hT", [128, 16, TPC], BF16)
    sq = [P.sb("sq%d" % i, [128, TPC], BF16) for i in range(2)]
    ones = P.sb("ones", [128, 128], BF16)
    gsb = P.sb("gsb", [128, 16], F32)
    rstd = P.sb("rstd", [128, TPC], F32)
    tmp = P.sb("tmp", [128, 512], F32)
    ost = [P.sb("ost%d" % i, [128, 512], F32) for i in range(4)]
    ws = WStream(P, 4, 16)
    pr = PsRing(P, 4)
    pss = [P.ps("pss%d" % i, [128, 512], F32) for i in range(2)]
    P.I("vector", "memset", [], ["ones"], ones[:], 1.0)
    P.dma("sync", gsb[:], g, writes=["gsb"])
    for k in range(16):
        s = k % 2
        P.dma("sync", xf[s][:], xT[k * 128:(k + 1) * 128, :], writes=["xf%d" % s])
        P.I("scalar", "activation", ["xf%d" % s, "gsb"], ["hT"], out=hT[:, k, :], in_=xf[s][:], func=AF.Copy,
            scale=gsb[:, k:k + 1])
        P.I("vector", "tensor_tensor", ["xf%d" % s], ["sq%d" % s], out=sq[s][:], in0=xf[s][:], in1=xf[s][:],
            op=ALU.mult)
        for h in range(2):
            P.I("tensor", "matmul", ["ones", "sq%d" % s], ["pss%d" % h], pss[h][:], lhsT=ones[:],
                rhs=sq[s][:, h * 512:(h + 1) * 512], start=(k == 0), stop=(k == 15))
    for h in range(2):
        rstd_from_ps(P, pss[h][:], "pss%d" % h, rstd[:, h * 512:(h + 1) * 512], "rstd%d" % h, tmp[:], "tmp",
                     1.0 / D)
    cnt = [0]

    def evac(m, h, outs):
        ps, pk = outs[0]
        s = cnt[0] % 4
        cnt[0] += 1
        P.I("vector", "tensor_tensor", [pk, "rstd%d" % h], ["ost%d" % s], out=ost[s][:], in0=ps[:],
            in1=rstd[:, h * 512:(h + 1) * 512], op=ALU.mult)
        P.dma("scalar", out[m * 128:(m + 1) * 128, h * 512:(h + 1) * 512], ost[s][:], reads=["ost%d" % s],
              key="ost%d" % s)

    linear(P, ws, pr, [(W, hT, 16, "hT")], 8192, TPC, evac)
    P.emit()
    return nc


def build_M():
    nc = new_nc()
    ysT = din(nc, "ysT", [1024, TPC])
    yaT = din(nc, "yaT", [1024, TPC])
    glT = din(nc, "glT", [4096, TPC])
    xT = din(nc, "xT", [D, TPC])
    w_glu = din(nc, "w_glu", [1024, 1024])
    w_brs = din(nc, "w_brs", [1024, D])
    w_bra = din(nc, "w_bra", [1024, D])
    w_out = din(nc, "w_out", [D, D])
    gate_b = din(nc, "gate_b", [128, 32])
    gffn = din(nc, "gffn", [128, 16])
    w_r = din(nc, "w_r", [128, 16, 16])
    x1T = dout(nc, "x1T", [D, TPC])
    xnT = dout(nc, "xnT", [D, TPC], BF16)
    affT = dout(nc, "affT", [16, TPC])
    P = Prog(nc)
    arena = P.sb("arena", [128, 16 * TPC], F32)
    ab = arena[:].bitcast(BF16)
    ys = ab[:, 0:8192].rearrange("p (k t) -> p k t", k=8)
    ys2 = ab[:, 8192:16384].rearrange("p (k t) -> p k t", k=8)
    ya = ab[:, 16384:24576].rearrange("p (k t) -> p k t", k=8)
    x1 = arena[:].rearrange("p (k t) -> p k t", k=16)
    merged = P.sb("merged", [128, 16, TPC], BF16)
    ldf = [P.sb("ldf%d" % i, [128, TPC], F32) for i in range(2)]
    gb = P.sb("gb", [128, 32], F32)
    gf = P.sb("gf", [128, 16], F32)
    wr = P.sb("wr", [128, 16, 16], F32)
    ones = P.sb("ones", [128, 128], BF16)
    ones16 = P.sb("ones16", [16, 16], F32)
    rstd = P.sb("rstd", [128, TPC], F32)
    tmp = P.sb("tmp", [128, 512], F32)
    tA = [P.sb("tA%d" % i, [128, 512], F32) for i in range(2)]
    tB = [P.sb("tB%d" % i, [128, 512], F32) for i in range(2)]
    glt = [P.sb("glt%d" % i, [128, 512], F32) for i in range(4)]
    gs = [P.sb("gs%d" % i, [128, 512], F32) for i in range(4)]
    sqb = [P.sb("sqb%d" % i, [128, 512], BF16) for i in range(2)]
    xnb = [P.sb("xnb%d" % i, [128, 512], BF16) for i in range(2)]
    esb = P.sb("esb", [16, 512], F32)
    rsb = P.sb("rsb", [16, 512], F32)
    asb = P.sb("asb", [16, 512], F32)
    ws = WStream(P, 4, 16)
    pr = PsRing(P, 4)
    pss = [P.ps("pss%d" % i, [128, 512], F32) for i in range(2)]
    plg = [P.ps("plg%d" % i, [16, 512], F32) for i in range(2)]
    P.I("vector", "memset", [], ["ones"], ones[:], 1.0)
    P.I("vector", "memset", [], ["ones16"], ones16[:], 1.0)
    P.dma("sync", gb[:], gate_b, writes=["gb"])
    P.dma("sync", gf[:], gffn, writes=["gf"])
    P.dma("sync", wr[:], w_r, writes=["wr"])
    for k in range(8):
        s = k % 2
        P.dma("sync", ldf[s][:], ysT[k * 128:(k + 1) * 128, :], writes=["ldf%d" % s])
        P.I("scalar", "activation", ["ldf%d" % s], ["ys"], out=ys[:, k, :], in_=ldf[s][:], func=AF.Gelu)
    for k in range(8):
        s = k % 2
        P.dma("sync", ldf[s][:], yaT[k * 128:(k + 1) * 128, :], writes=["ldf%d" % s])
        P.I("vector", "tensor_copy", ["ldf%d" % s], ["ya"], out=ya[:, k, :], in_=ldf[s][:])
    c1 = [0]

    def evac_glu(m, h, outs):
        ps, pk = outs[0]
        s = c1[0] % 2
        c1[0] += 1
        P.I("scalar", "activation", [pk], ["tA%d" % s], out=tA[s][:], in_=ps[:], func=AF.Sigmoid)
        P.I("vector", "tensor_tensor", ["tA%d" % s, "ys"], ["ys2"], out=ys2[:, m, h * 512:(h + 1) * 512],
            in0=ys[:, m, h * 512:(h + 1) * 512], in1=tA[s][:], op=ALU.mult)

    linear(P, ws, pr, [(w_glu, ys, 8, "ys")], 1024, TPC, evac_glu)
    c2 = [0]

    def evac_br(m, h, outs):
        (pa, pak), (pb, pbk) = outs
        s = c2[0] % 2
        c2[0] += 1
        for br in range(2):
            gi = 2 * s + br
            P.dma("sync", glt[gi][:], glT[br * D + m * 128: br * D + (m + 1) * 128, h * 512:(h + 1) * 512],
                  writes=["glt%d" % gi])
            P.I("scalar", "activation", ["glt%d" % gi, "gb"], ["gs%d" % gi], out=gs[gi][:], in_=glt[gi][:],
                func=AF.Sigmoid, bias=gb[:, br * 16 + m: br * 16 + m + 1])
        P.I("vector", "tensor_tensor", [pak, "gs%d" % (2 * s)], ["tA%d" % s], out=tA[s][:], in0=pa[:],
            in1=gs[2 * s][:], op=ALU.mult)
        P.I("vector", "tensor_tensor", [pbk, "gs%d" % (2 * s + 1)], ["tB%d" % s], out=tB[s][:], in0=pb[:],
            in1=gs[2 * s + 1][:], op=ALU.mult)
        P.I("vector", "tensor_tensor", ["tA%d" % s, "tB%d" % s], ["merged"],
            out=merged[:, m, h * 512:(h + 1) * 512], in0=tA[s][:], in1=tB[s][:], op=ALU.add)

    linear(P, ws, pr, [(w_brs, ys2, 8, "ys2"), (w_bra, ya, 8, "ya")], D, TPC, evac_br)
    P.alias(["x1"], ["ys", "ys2", "ya"])
    c3 = [0]

    def evac_out(m, h, outs):
        ps, pk = outs[0]
        s = c3[0] % 2
        c3[0] += 1
        sl = slice(h * 512, (h + 1) * 512)
        P.dma("sync", tA[s][:], xT[m * 128:(m + 1) * 128, sl], writes=["tA%d" % s])
        P.I("vector", "tensor_tensor", [pk, "tA%d" % s], ["x1"], out=x1[:, m, sl], in0=ps[:], in1=tA[s][:],
            op=ALU.add)
        P.dma("scalar", x1T[m * 128:(m + 1) * 128, sl], x1[:, m, sl], reads=["x1"], key="x1out")
        P.I("scalar", "activation", ["x1"], ["sqb%d" % s], out=sqb[s][:], in_=x1[:, m, sl], func=AF.Square)
        P.I("tensor", "matmul", ["ones", "sqb%d" % s], ["pss%d" % h], pss[h][:], lhsT=ones[:], rhs=sqb[s][:],
            start=(m == 0), stop=(m == 15))

    linear(P, ws, pr, [(w_out, merged, 16, "merged")], D, TPC, evac_out)
    for h in range(2):
        sl = slice(h * 512, (h + 1) * 512)
        rstd_from_ps(P, pss[h][:], "pss%d" % h, rstd[:, sl], "rstd%d" % h, tmp[:], "tmp", 1.0 / D)
        for m in range(16):
            s = m % 2
            P.I("vector", "scalar_tensor_tensor", ["x1", "gf", "rstd%d" % h], ["tB%d" % s], out=tB[s][:],
                in0=x1[:, m, sl], scalar=gf[:, m:m + 1], in1=rstd[:, sl], op0=ALU.mult, op1=ALU.mult)
            P.I("tensor", "matmul", ["wr", "tB%d" % s], ["plg%d" % h], plg[h][:], lhsT=wr[:, m, :], rhs=tB[s][:],
                start=(m == 0), stop=(m == 15))
            P.I("scalar", "activation", ["tB%d" % s], ["xnb%d" % s], out=xnb[s][:], in_=tB[s][:], func=AF.Copy)
            P.dma("scalar", xnT[m * 128:(m + 1) * 128, sl], xnb[s][:], reads=["xnb%d" % s], key="xnb%d" % s)
        P.I("scalar", "activation", ["plg%d" % h], ["esb"], out=esb[:], in_=plg[h][:], func=AF.Exp)
        P.I("tensor", "matmul", ["ones16", "esb"], ["plg%d" % h], plg[h][:], lhsT=ones16[:], rhs=esb[:],
            start=True, stop=True)
        P.I("vector", "reciprocal", ["plg%d" % h], ["rsb"], out=rsb[:], in_=plg[h][:])
        P.I("vector", "tensor_tensor", ["esb", "rsb"], ["asb"], out=asb[:], in0=esb[:], in1=rsb[:], op=ALU.mult)
        P.dma("scalar", affT[:, sl], asb[:], reads=["asb"], key="asb")
    P.emit()
    return nc


def build_E():
    nc = new_nc()
    affc = din(nc, "affc", [2, 128, 64])
    xn = din(nc, "xn", [L, D], BF16)
    wg = din(nc, "wg", [2, D, 1024])
    wu = din(nc, "wu", [2, D, 1024])
    wd = din(nc, "wd", [2, 1024, D])
    tokid = din(nc, "tokid", [128, 64, 2], I32)
    tri = din(nc, "tri", [128, 128])
    ebase = din(nc, "ebase", [128, 2])
    identd = din(nc, "identd", [128, 128], BF16)
    yeT = dout(nc, "yeT", [2, D, 1024])
    posrow = dout(nc, "posrow", [2, 128, 64], I32)
    idxl = [nc.dram_tensor("idxl%d" % i, [1024, 2], I32, kind="Internal").ap() for i in range(2)]
    P = Prog(nc)
    af = P.sb("af", [128, 2, 64], F32)
    tok = P.sb("tok", [128, 64, 2], I32)
    trs = P.sb("trs", [128, 128], F32)
    ebs = P.sb("ebs", [128, 2], F32)
    ebm = P.sb("ebm", [128, 2], F32)
    ident = P.sb("ident", [128, 128], BF16)
    onesf = P.sb("onesf", [128, 128], F32)
    ones64 = P.sb("ones64", [128, 64], F32)
    lo = P.sb("lo", [128, 2], F32)
    hi = P.sb("hi", [128, 2], F32)
    mid = P.sb("mid", [128, 2], F32)
    cntp = P.sb("cntp", [128, 2], F32)
    cond = P.sb("cond", [128, 2], F32)
    d1 = P.sb("d1", [128, 2], F32)
    d2 = P.sb("d2", [128, 2], F32)
    junk = P.sb("junk", [128, 2, 64], F32)
    mask = P.sb("mask", [128, 2, 64], F32)
    incl = P.sb("incl", [128, 2, 64], F32)
    tot = P.sb("tot", [128, 2], F32)
    offm1 = P.sb("offm1", [128, 2], F32)
    posf = P.sb("posf", [128, 2, 64], F32)
    t1 = P.sb("t1", [128, 2, 64], F32)
    t2 = P.sb("t2", [128, 2, 64], F32)
    posi = P.sb("posi", [128, 2, 64], I32)
    rowi = P.sb("rowi", [128, 2, 64], I32)
    idxs = P.sb("idxs", [128, 2, 8, 2], I32)
    xe = [P.sb("xe%d" % i, [128, D], BF16) for i in range(2)]
    xeT = P.sb("xeT", [128, 16, 1024], BF16)
    act = P.sb("act", [128, 8, 1024], BF16)
    tA = [P.sb("tA%d" % i, [128, 512], F32) for i in range(2)]
    ost = [P.sb("ost%d" % i, [128, 512], F32) for i in range(2)]
    ws = WStream(P, 4, 16)
    pr = PsRing(P, 4)
    pc = P.ps("pc", [128, 2], F32)
    pT = [P.ps("pT%d" % i, [128, 4, 128], BF16) for i in range(2)]
    P.dma("sync", af[:], affc.rearrange("e p f -> p e f"), writes=["af"])
    P.dma("sync", tok[:], tokid, writes=["tok"])
    P.dma("sync", trs[:], tri, writes=["trs"])
    P.dma("sync", ebs[:], ebase, writes=["ebs"])
    P.dma("sync", ident[:], identd, writes=["ident"])
    P.I("vector", "memset", [], ["onesf"], onesf[:], 1.0)
    P.I("vector", "memset", [], ["ones64"], ones64[:], 1.0)
    P.I("vector", "memset", [], ["lo"], lo[:], 0.0)
    P.I("vector", "memset", [], ["hi"], hi[:], 1.0)
    P.I("vector", "tensor_scalar", ["ebs"], ["ebm"], out=ebm[:], in0=ebs[:], scalar1=-BIG, scalar2=None,
        op0=ALU.add)
    V = "vector"
    for it in range(30):
        P.I(V, "tensor_tensor", ["lo", "hi"], ["mid"], out=mid[:], in0=lo[:], in1=hi[:], op=ALU.add)
        P.I(V, "tensor_scalar", ["mid"], ["mid"], out=mid[:], in0=mid[:], scalar1=0.5, scalar2=None, op0=ALU.mult)
        for e in range(2):
            P.I(V, "tensor_scalar", ["af", "mid"], ["junk"], out=junk[:, e, :], in0=af[:, e, :],
                scalar1=mid[:, e:e + 1], scalar2=None, op0=ALU.is_ge)
            P.I(V, "reduce_sum", ["junk"], ["cntp"], out=cntp[:, e:e + 1], in_=junk[:, e, :], axis=AX.X)
        P.I("tensor", "matmul", ["onesf", "cntp"], ["pc"], pc[:], lhsT=onesf[:], rhs=cntp[:], start=True, stop=True)
        P.I(V, "tensor_scalar", ["pc"], ["cond"], out=cond[:], in0=pc[:], scalar1=1024.0, scalar2=None,
            op0=ALU.is_ge)
        P.I(V, "tensor_tensor", ["mid", "lo"], ["d1"], out=d1[:], in0=mid[:], in1=lo[:], op=ALU.subtract)
        P.I(V, "tensor_tensor", ["d1", "cond"], ["d1"], out=d1[:], in0=d1[:], in1=cond[:], op=ALU.mult)
        P.I(V, "tensor_tensor", ["hi", "mid"], ["d2"], out=d2[:], in0=hi[:], in1=mid[:], op=ALU.subtract)
        P.I(V, "tensor_tensor", ["d2", "cond"], ["d2"], out=d2[:], in0=d2[:], in1=cond[:], op=ALU.mult)
        P.I(V, "tensor_tensor", ["lo", "d1"], ["lo"], out=lo[:], in0=lo[:], in1=d1[:], op=ALU.add)
        P.I(V, "tensor_tensor", ["mid", "d2"], ["hi"], out=hi[:], in0=mid[:], in1=d2[:], op=ALU.add)
    for e in range(2):
        P.I(V, "tensor_scalar", ["af", "lo"], ["mask"], out=mask[:, e, :], in0=af[:, e, :], scalar1=lo[:, e:e + 1],
            scalar2=None, op0=ALU.is_ge)
        P.I(V, "tensor_tensor_scan", ["mask", "ones64"], ["incl"], out=incl[:, e, :], data0=ones64[:],
            data1=mask[:, e, :], initial=0.0, op0=ALU.mult, op1=ALU.add)
        P.I(V, "tensor_copy", ["incl"], ["tot"], out=tot[:, e:e + 1], in_=incl[:, e, 63:64])
    P.I("tensor", "matmul", ["trs", "tot"], ["pc"], pc[:], lhsT=trs[:], rhs=tot[:], start=True, stop=True)
    P.I(V, "tensor_scalar", ["pc"], ["offm1"], out=offm1[:], in0=pc[:], scalar1=-1.0, scalar2=None, op0=ALU.add)
    for e in range(2):
        P.I(V, "tensor_scalar", ["incl", "offm1"], ["posf"], out=posf[:, e, :], in0=incl[:, e, :],
            scalar1=offm1[:, e:e + 1], scalar2=None, op0=ALU.add)
        P.I(V, "scalar_tensor_tensor", ["posf", "mask"], ["t1"], out=t1[:, e, :], in0=posf[:, e, :], scalar=-BIG,
            in1=mask[:, e, :], op0=ALU.add, op1=ALU.mult)
        P.I(V, "tensor_scalar", ["t1"], ["t1"], out=t1[:, e, :], in0=t1[:, e, :], scalar1=BIG, scalar2=None,
            op0=ALU.add)
        P.I(V, "tensor_copy", ["t1"], ["posi"], out=posi[:, e, :], in_=t1[:, e, :])
        P.I(V, "scalar_tensor_tensor", ["posf", "mask", "ebm"], ["t2"], out=t2[:, e, :], in0=posf[:, e, :],
            scalar=ebm[:, e:e + 1], in1=mask[:, e, :], op0=ALU.add, op1=ALU.mult)
        P.I(V, "tensor_scalar", ["t2"], ["t2"], out=t2[:, e, :], in0=t2[:, e, :], scalar1=BIG, scalar2=None,
            op0=ALU.add)
        P.I(V, "tensor_copy", ["t2"], ["rowi"], out=rowi[:, e, :], in_=t2[:, e, :])
    P.dma("sync", posrow.rearrange("e p f -> p e f"), rowi[:], reads=["rowi"], key="rowi")
    c0 = [0]
    for e in range(2):
        fk = []
        for f in range(64):
            P.idma(idxl[e], IOA(ap=posi[:, e, f:f + 1], axis=0), tok[:, f, :], None, reads=["posi", "tok"],
                   writes=["idxl%d_%d" % (e, f)], key="idxl%d" % e, bounds=1023)
            fk.append("idxl%d_%d" % (e, f))
        P.dma("sync", idxs[:, e, :, :], idxl[e].rearrange("(p j) o -> p j o", j=8), reads=fk, writes=["idxs%d" % e])
        for jt in range(8):
            s = jt % 2
            P.idma(xe[s][:, :], None, xn, IOA(ap=idxs[:, e, jt, 0:1], axis=0), reads=["idxs%d" % e],
                   writes=["xe%d" % s], key="xe%d" % s)
            for kq in range(4):
                ti = c0[0] % 2
                c0[0] += 1
                for j in range(4):
                    k = kq * 4 + j
                    P.I("tensor", "transpose", ["xe%d" % s, "ident"], ["pT%d" % ti], out=pT[ti][:, j, :],
                        in_=xe[s][:, k * 128:(k + 1) * 128], identity=ident[:])
                P.I("scalar" if kq % 2 else "vector", "tensor_copy" if kq % 2 == 0 else "copy", ["pT%d" % ti],
                    ["xeT"], out=xeT[:, kq * 4:(kq + 1) * 4, jt * 128:(jt + 1) * 128], in_=pT[ti][:])
        c1 = [0]

        def evac_ffn(m, h, outs):
            (pg, pgk), (pu, puk) = outs
            s = c1[0] % 2
            c1[0] += 1
            P.I("scalar", "activation", [pgk], ["tA%d" % s], out=tA[s][:], in_=pg[:], func=AF.Silu)
            P.I("vector", "tensor_tensor", ["tA%d" % s, puk], ["act"], out=act[:, m, h * 512:(h + 1) * 512],
                in0=tA[s][:], in1=pu[:], op=ALU.mult)

        linear(P, ws, pr, [(wg[e], xeT, 16, "xeT"), (wu[e], xeT, 16, "xeT")], 1024, 1024, evac_ffn)

        def evac_dn(m, h, outs, e=e):
            ps, pk = outs[0]
            s = c1[0] % 2
            c1[0] += 1
            P.I("scalar", "copy", [pk], ["ost%d" % s], out=ost[s][:], in_=ps[:])
            P.dma("sync", yeT[e, m * 128:(m + 1) * 128, h * 512:(h + 1) * 512], ost[s][:], reads=["ost%d" % s],
                  key="ost%d" % s)

        linear(P, ws, pr, [(wd[e], act, 8, "act")], D, 1024, evac_dn)
    P.emit()
    return nc


def build_C():
    nc = new_nc()
    x1 = din(nc, "x1", [TPC, D])
    ye = din(nc, "ye", [16 * 1024, D])
    prow = din(nc, "prow", [TPC, 16], I32)
    aff = din(nc, "aff", [TPC, 16])
    x2 = dout(nc, "x2", [TPC, D])
    P = Prog(nc)
    acc = [P.sb("acc%d" % i, [128, D], F32) for i in range(2)]
    prt = [P.sb("prt%d" % i, [128, 16], I32) for i in range(2)]
    aft = [P.sb("aft%d" % i, [128, 16], F32) for i in range(2)]
    gbuf = [P.sb("gb%d" % i, [128, D], F32) for i in range(4)]
    c = 0
    for t in range(TPC // 128):
        s = t % 2
        rs = slice(t * 128, (t + 1) * 128)
        P.dma("sync", acc[s][:], x1[rs, :], writes=["acc%d" % s])
        P.dma("sync", prt[s][:], prow[rs, :], writes=["prt%d" % s])
        P.dma("sync", aft[s][:], aff[rs, :], writes=["aft%d" % s])
        for e in range(16):
            b = c % 4
            c += 1
            P.I("gpsimd", "memset", [], ["gb%d" % b], gbuf[b][:], 0.0)
            P.idma(gbuf[b][:, :], None, ye, IOA(ap=prt[s][:, e:e + 1], axis=0), reads=["prt%d" % s],
                   writes=["gb%d" % b], key="gb%d" % b, bounds=16 * 1024 - 1)
            P.I("vector", "scalar_tensor_tensor", ["gb%d" % b, "aft%d" % s, "acc%d" % s], ["acc%d" % s],
                out=acc[s][:], in0=gbuf[b][:], scalar=aft[s][:, e:e + 1], in1=acc[s][:], op0=ALU.mult, op1=ALU.add)
        P.dma("scalar", x2[rs, :], acc[s][:], reads=["acc%d" % s], key="acc%d" % s)
    P.emit()
    return nc


def consts_E():
    tokid = np.ascontiguousarray(np.repeat(np.arange(L, dtype=np.int32).reshape(128, 64, 1), 2, axis=2))
    tri = (np.arange(128)[:, None] < np.arange(128)[None, :]).astype(np.float32)
    ident = np.eye(128, dtype=np.float32).astype(ml_dtypes.bfloat16)
    return tokid, tri, ident


def build_A():
    nc = new_nc()
    qT = din(nc, "qT", [128, L])
    kT = din(nc, "kT", [128, L])
    vT = din(nc, "vT", [128, L])
    ctab = din(nc, "ctab", [128, L])
    stab = din(nc, "stab", [128, L])
    small = din(nc, "small", [128, 8])
    lamin = din(nc, "lamin", [128, 4, 64])
    pmd = din(nc, "pmd", [128, 128], BF16)
    bdd = din(nc, "bdd", [128, 128], BF16)
    identd = din(nc, "identd", [128, 128], BF16)
    oT = dout(nc, "oT", [128, L])
    P = Prog(nc)
    V, A_, T_ = "vector", "scalar", "tensor"
    sm = P.sb("sm", [128, 8], F32)
    li = P.sb("li", [128, 4, 64], F32)
    pm = P.sb("pm", [128, 128], BF16)
    bd = P.sb("bd", [128, 128], BF16)
    ident = P.sb("ident", [128, 128], BF16)
    ones = P.sb("ones", [128, 128], BF16)
    lt = P.sb("lt", [128, 2, 64], F32)
    ls = P.sb("ls", [128, 8], F32)
    qr = P.sb("qr", [128, L], BF16)
    kr = P.sb("kr", [128, L], BF16)
    Vt = P.sb("Vt", [128, 64, 128], BF16)
    raw = [P.sb("raw%d" % i, [128, 512], F32) for i in range(3)]
    ct = [P.sb("ct%d" % i, [128, 512], F32) for i in range(2)]
    st = [P.sb("st%d" % i, [128, 512], F32) for i in range(2)]
    sqb = [P.sb("sqb%d" % i, [128, 512], BF16) for i in range(2)]
    qn = [P.sb("qn%d" % i, [128, 512], F32) for i in range(2)]
    qnb = [P.sb("qnb%d" % i, [128, 512], BF16) for i in range(2)]
    rs = [P.sb("rs%d" % i, [128, 512], F32) for i in range(2)]
    tmp = P.sb("tmp", [128, 512], F32)
    t1 = [P.sb("t1%d" % i, [128, 512], F32) for i in range(2)]
    t2 = [P.sb("t2%d" % i, [128, 512], F32) for i in range(2)]
    vb = [P.sb("vb%d" % i, [128, 512], BF16) for i in range(2)]
    pt = [P.sb("pt%d" % i, [128, 512], BF16) for i in range(4)]
    ost = [P.sb("ost%d" % i, [128, 512], F32) for i in range(2)]
    ring = PsRing(P, 3, "pr")
    acc = [P.ps("acc%d" % i, [128, 512], F32) for i in range(4)]
    pT = P.ps("pT", [128, 4, 128], BF16)
    P.dma("sync", sm[:], small, writes=["sm"])
    P.dma("sync", li[:], lamin, writes=["li"])
    P.dma("sync", pm[:], pmd, writes=["pm"])
    P.dma("sync", bd[:], bdd, writes=["bd"])
    P.dma("sync", ident[:], identd, writes=["ident"])
    P.I(V, "memset", [], ["ones"], ones[:], 1.0)
    for j in range(2):
        P.I(V, "tensor_tensor", ["li"], ["lt"], out=lt[:, j, :], in0=li[:, 2 * j, :], in1=li[:, 2 * j + 1, :],
            op=ALU.mult)
        P.I(V, "reduce_sum", ["lt"], ["ls"], out=ls[:, j:j + 1], in_=lt[:, j, :], axis=AX.X)
        P.I(A_, "activation", ["ls"], ["ls"], out=ls[:, 2 + j:3 + j], in_=ls[:, j:j + 1], func=AF.Exp)
    P.I(V, "tensor_tensor", ["ls"], ["ls"], out=ls[:, 4:5], in0=ls[:, 2:3], in1=ls[:, 3:4], op=ALU.subtract)
    P.I(V, "tensor_tensor", ["ls", "sm"], ["ls"], out=ls[:, 4:5], in0=ls[:, 4:5], in1=sm[:, 3:4], op=ALU.add)
    P.I(V, "tensor_scalar", ["ls"], ["ls"], out=ls[:, 5:6], in0=ls[:, 4:5], scalar1=-1.0, scalar2=None,
        op0=ALU.mult)
    P.I(V, "tensor_tensor", ["sm"], ["ls"], out=ls[:, 6:7], in0=sm[:, 2:3], in1=sm[:, 4:5], op=ALU.mult)
    P.I(V, "tensor_scalar", ["sm"], ["ls"], out=ls[:, 7:8], in0=sm[:, 0:1], scalar1=0.125, scalar2=None,
        op0=ALU.mult)
    cc = 0
    for tcx in range(L // 512):
        sl = slice(tcx * 512, (tcx + 1) * 512)
        s2 = tcx % 2
        P.dma("sync", ct[s2][:], ctab[:, sl], writes=["ct%d" % s2])
        P.dma("sync", st[s2][:], stab[:, sl], writes=["st%d" % s2])
        for (src, gap, gk, dst, dk) in ((qT, ls[:, 7:8], "ls", qr, "qr"), (kT, sm[:, 1:2], "sm", kr, "kr")):
            s = cc % 2
            r3 = cc % 3
            cc += 1
            P.dma("sync", raw[r3][:], src[:, sl], writes=["raw%d" % r3])
            P.I(A_, "activation", ["raw%d" % r3], ["sqb%d" % s], out=sqb[s][:], in_=raw[r3][:], func=AF.Square)
            ps, pk = ring.next()
            P.I(T_, "matmul", ["bd", "sqb%d" % s], [pk], ps[:], lhsT=bd[:], rhs=sqb[s][:], start=True, stop=True)
            rstd_from_ps(P, ps[:], pk, rs[s][:], "rs%d" % s, tmp[:], "tmp", 1.0 / 64)
            P.I(V, "scalar_tensor_tensor", ["raw%d" % r3, gk, "rs%d" % s], ["qn%d" % s], out=qn[s][:],
                in0=raw[r3][:], scalar=gap, in1=rs[s][:], op0=ALU.mult, op1=ALU.mult)
            P.I(A_, "activation", ["qn%d" % s], ["qnb%d" % s], out=qnb[s][:], in_=qn[s][:], func=AF.Copy)
            ps2, pk2 = ring.next()
            P.I(T_, "matmul", ["pm", "qnb%d" % s], [pk2], ps2[:], lhsT=pm[:], rhs=qnb[s][:], start=True, stop=True)
            P.I(V, "tensor_tensor", [pk2, "st%d" % s2], ["t1%d" % s], out=t1[s][:], in0=ps2[:], in1=st[s2][:],
                op=ALU.mult)
            P.I("gpsimd", "tensor_tensor", ["qn%d" % s, "ct%d" % s2], ["t2%d" % s], out=t2[s][:], in0=qn[s][:],
                in1=ct[s2][:], op=ALU.mult)
            P.I(V, "tensor_tensor", ["t1%d" % s, "t2%d" % s], [dk], out=dst[:, sl], in0=t1[s][:], in1=t2[s][:],
                op=ALU.add)
        r3 = cc % 3
        cc += 1
        P.dma("sync", raw[r3][:], vT[:, sl], writes=["raw%d" % r3])
        P.I("gpsimd", "tensor_copy", ["raw%d" % r3], ["vb%d" % s2], out=vb[s2][:], in_=raw[r3][:])
        for j in range(4):
            P.I(T_, "transpose", ["vb%d" % s2, "ident"], ["pT"], out=pT[:, j, :], in_=vb[s2][:, j * 128:(j + 1) * 128],
                identity=ident[:])
        P.I(A_, "copy", ["pT"], ["Vt"], out=Vt[:, tcx * 4:(tcx + 1) * 4, :], in_=pT[:])
    steps = [(qc, c, kt) for qc in range(L // 512) for c in range(2) for kt in range(64)]
    ns = len(steps)
    sps = [None] * ns

    def emit_S(i):
        qc, c, kt = steps[i]
        ps, pk = ring.next()
        P.I(T_, "matmul", ["kr", "qr"], [pk], ps[:], lhsT=kr[64 * c:64 * c + 64, kt * 128:(kt + 1) * 128],
            rhs=qr[64 * c:64 * c + 64, qc * 512:(qc + 1) * 512], start=True, stop=True)
        sps[i] = (ps, pk)

    def emit_rest(i):
        qc, c, kt = steps[i]
        ps, pk = sps[i]
        s = i % 4
        P.I(A_, "activation", [pk], ["pt%d" % s], out=pt[s][:], in_=ps[:], func=AF.Exp)
        P.I(T_, "matmul", ["Vt", "pt%d" % s], ["acc%d" % c], acc[c][:], lhsT=Vt[:, kt, :], rhs=pt[s][:],
            start=(kt == 0), stop=(kt == 63))
        P.I(T_, "matmul", ["ones", "pt%d" % s], ["acc%d" % (2 + c)], acc[2 + c][:], lhsT=ones[:], rhs=pt[s][:],
            start=(kt == 0), stop=(kt == 63))
        if c == 1 and kt == 63:
            sl = slice(qc * 512, (qc + 1) * 512)
            e = qc % 2
            for c2 in range(2):
                P.I(V, "reciprocal", ["acc%d" % (2 + c2)], ["rs%d" % c2], out=rs[c2][:], in_=acc[2 + c2][:])
                P.I(V, "tensor_tensor", ["acc%d" % c2, "rs%d" % c2], ["t1%d" % c2], out=t1[c2][:], in0=acc[c2][:],
                    in1=rs[c2][:], op=ALU.mult)
            P.I(V, "scalar_tensor_tensor", ["t11", "ls", "t10"], ["qn%d" % e], out=qn[e][:], in0=t1[1][:],
                scalar=ls[:, 5:6], in1=t1[0][:], op0=ALU.mult, op1=ALU.add)
            P.I("gpsimd", "tensor_tensor", ["qn%d" % e], ["sqb%d" % e], out=sqb[e][:], in0=qn[e][:], in1=qn[e][:],
                op=ALU.mult)
            ps3, pk3 = acc[2], "acc2"
            P.I(T_, "matmul", ["ones", "sqb%d" % e], [pk3], ps3[:], lhsT=ones[:], rhs=sqb[e][:], start=True, stop=True)
            rstd_from_ps(P, ps3[:], pk3, t2[e][:], "t2%d" % e, tmp[:], "tmp", 1.0 / 128)
            P.I(V, "scalar_tensor_tensor", ["qn%d" % e, "ls", "t2%d" % e], ["ost%d" % e], out=ost[e][:],
                in0=qn[e][:], scalar=ls[:, 6:7], in1=t2[e][:], op0=ALU.mult, op1=ALU.mult)
            P.dma("sync", oT[:, sl], ost[e][:], reads=["ost%d" % e], key="ost%d" % e)

    LOOK = 2
    for i in range(min(LOOK, ns)):
        emit_S(i)
    for i in range(ns):
        if i + LOOK < ns:
            emit_S(i + LOOK)
        emit_rest(i)
    P.emit()
    return nc


def consts_A():
    pos = np.arange(L, dtype=np.float64)
    inv = np.power(500000.0, -np.arange(0, 16, 2, dtype=np.float64) / 16)
    ang = (pos[:, None].astype(np.float32) * inv[None, :].astype(np.float32)).astype(np.float32).astype(np.float64)
    cos, sin = np.cos(ang).T, np.sin(ang).T
    ctab = np.ones((128, L), np.float32)
    stab = np.zeros((128, L), np.float32)
    pm = np.zeros((128, 128), np.float32)
    for half in range(2):
        b = half * 64
        ctab[b:b + 8] = cos
        ctab[b + 8:b + 16] = cos
        stab[b:b + 8] = -sin
        stab[b + 8:b + 16] = sin
        for i in range(8):
            pm[b + 8 + i, b + i] = 1.0
            pm[b + i, b + 8 + i] = 1.0
    bd = np.zeros((128, 128), np.float32)
    bd[:64, :64] = 1
    bd[64:, 64:] = 1
    bf = ml_dtypes.bfloat16
    return ctab, stab, pm.astype(bf), bd.astype(bf), np.eye(128, dtype=np.float32).astype(bf)


PI2 = 2.0 * math.pi
PI_SAFE = 3.141592


def build_S():
    nc = new_nc()
    U8 = din(nc, "U8", [8, 128, 1024])
    bT = din(nc, "bT", [128, 2, 1024])
    aB = din(nc, "aB", [128, 3, 1024])
    epart = din(nc, "epart", [128, 2])
    cP = din(nc, "cP", [128, 4, 16, 16])
    aC = din(nc, "aC", [128, 3, 16])
    ktab = din(nc, "ktab", [128, 16])
    seld = din(nc, "sel", [128, 3])
    dskd = din(nc, "dsk", [128, 8])
    matsd = din(nc, "mats", [128, 3, 128])
    cidxd = din(nc, "cidx", [128, 1024])
    y8 = dout(nc, "y8", [8, 128, 1024])
    P = Prog(nc)
    V, A_, T_, G_ = "vector", "scalar", "tensor", "gpsimd"
    PR = ["prep"]

    def tt(out, a, b, op, r=PR, w=PR, eng=V):
        P.I(eng, "tensor_tensor", r, w, out=out, in0=a, in1=b, op=op)

    def ts(out, a, s1, op0, s2=None, op1=None, r=PR, w=PR):
        kw = dict(out=out, in0=a, scalar1=s1, scalar2=s2, op0=op0)
        if op1 is not None:
            kw["op1"] = op1
        P.I(V, "tensor_scalar", r, w, **kw)

    def stt(out, a, s, b, op0, op1, r=PR, w=PR):
        P.I(V, "scalar_tensor_tensor", r, w, out=out, in0=a, scalar=s, in1=b, op0=op0, op1=op1)

    def act(out, a, func, r=PR, w=PR, **kw):
        P.I(A_, "activation", r, w, out=out, in_=a, func=func, **kw)

    G = [P.sb("G%d" % i, [128, 1024], F32) for i in range(20)]
    tki = P.sb("tki", [128, 1024], I32)
    sbT = P.sb("sbT", [128, 2, 1024], F32)
    saB = P.sb("saB", [128, 3, 1024], F32)
    sep = P.sb("sep", [128, 2], F32)
    scP = P.sb("scP", [128, 4, 16, 16], F32)
    saC = P.sb("saC", [128, 3, 16], F32)
    skt = P.sb("skt", [128, 16], F32)
    sel = P.sb("sel_s", [128, 3], F32)
    dsk = P.sb("dsk_s", [128, 8], F32)
    mats = P.sb("mats_s", [128, 3, 128], F32)
    cidx = P.sb("cidx_s", [128, 1024], F32)
    c16 = [P.sb("c16_%d" % i, [128, 16], F32) for i in range(12)]
    Lre = P.sb("Lre", [128, 16, 16], F32)
    Lim = P.sb("Lim", [128, 16, 16], F32)
    Cmat = P.sb("Cmat", [128, 2, 8, 128], BF16)
    Kc = P.sb("Kc", [128, 8, 128], BF16)
    W1 = P.sb("W1", [128, 16, 128], BF16)
    W2 = P.sb("W2", [128, 16, 128], BF16)
    thr = P.sb("thr", [128, 16], F32)
    r8 = P.sb("r8", [128, 16], F32)
    ustg = [P.sb("ustg%d" % i, [128, 1024], F32) for i in range(2)]
    U8b = [P.sb("U8b%d" % i, [128, 1024], BF16) for i in range(2)]
    Xin = [P.sb("Xin%d" % i, [128, 1024], BF16) for i in range(4)]
    ost = [P.sb("ost%d" % i, [128, 512], F32) for i in range(2)]
    ring = PsRing(P, 4, "pr")
    pk_ = [P.ps("pkk%d" % i, [128, 128], F32) for i in range(2)]
    pso = [P.ps("pso%d" % i, [128, 512], F32) for i in range(2)]
    for (t, src) in ((sbT, bT), (saB, aB), (sep, epart), (scP, cP), (saC, aC), (skt, ktab), (sel, seld), (dsk, dskd),
                     (mats, matsd), (cidx, cidxd)):
        P.dma("sync", t[:], src, writes=PR)

    def sincos(ang, sin_out, cos_out, n, r=PR, w=PR, ta=None, tkf=None):
        ta = ta[:, :n]
        tkf = tkf[:, :n]
        for shift, outp in ((0.0, sin_out), (math.pi / 2, cos_out)):
            ts(ta, ang, shift, ALU.add, r=r, w=w)
            ts(tki[:, :n], ta, 1.0 / PI2, ALU.mult, r=r, w=w)
            P.I(V, "tensor_copy", r, w, out=tkf, in_=tki[:, :n])
            stt(ta, tkf, -PI2, ta, ALU.mult, ALU.add, r=r, w=w)
            ts(ta, ta, -PI_SAFE, ALU.max, s2=PI_SAFE, op1=ALU.min, r=r, w=w)
            act(outp, ta, AF.Sin, r=r, w=w)

    stepC, drC, diC, nrC, denC, creC, cimC, t16a, t16b = [c[:] for c in c16[:9]]
    act(stepC, saC[:, 2, :], AF.Exp)
    tt(drC, saC[:, 0, :], stepC, ALU.mult)
    tt(diC, saC[:, 1, :], stepC, ALU.mult)
    g3 = lambda i: G[i][:, 0:256].rearrange("p (a b) -> p a b", a=16)
    argm, angk, magk, sk, ck = g3(0), g3(1), g3(2), g3(3), g3(4)
    kb = skt[:].unsqueeze(1).to_broadcast([128, 16, 16])
    tt(argm, drC.unsqueeze(2).to_broadcast([128, 16, 16]), kb, ALU.mult)
    tt(angk, diC.unsqueeze(2).to_broadcast([128, 16, 16]), kb, ALU.mult)
    act(G[2][:, 0:256], G[0][:, 0:256], AF.Exp)
    sincos(G[1][:, 0:256], G[3][:, 0:256], G[4][:, 0:256], 256, ta=G[5], tkf=G[6])
    tt(Lre[:], magk, ck, ALU.mult)
    tt(Lim[:], magk, sk, ALU.mult)
    ts(t16a, diC, 8.0, ALU.mult)
    ts(tki[:, 0:16], t16a, 1.0 / PI2, ALU.mult)
    P.I(V, "tensor_copy", PR, PR, out=t16b, in_=tki[:, 0:16])
    stt(thr[:], t16b, -PI2, t16a, ALU.mult, ALU.add)
    act(r8[:], drC, AF.Exp, scale=8.0)

    def coef(lbr, lbi, are, aim, cre, cim, nr, den, tmp):
        ts(nr, lbr, -1.0, ALU.add)
        tt(den, are, are, ALU.mult)
        tt(tmp, aim, aim, ALU.mult)
        tt(den, den, tmp, ALU.add)
        P.I(V, "reciprocal", PR, PR, out=den, in_=den)
        tt(cre, nr, are, ALU.mult)
        tt(tmp, lbi, aim, ALU.mult)
        tt(cre, cre, tmp, ALU.add)
        tt(cre, cre, den, ALU.mult)
        tt(cim, lbi, are, ALU.mult)
        tt(tmp, nr, aim, ALU.mult)
        tt(cim, cim, tmp, ALU.subtract)
        tt(cim, cim, den, ALU.mult)

    coef(Lre[:, :, 8], Lim[:, :, 8], saC[:, 0, :], saC[:, 1, :], creC, cimC, nrC, denC, t16a)
    Bbre, Bbim, tB = g3(0), g3(1), g3(2)
    crb = creC.unsqueeze(2).to_broadcast([128, 16, 16])
    cib = cimC.unsqueeze(2).to_broadcast([128, 16, 16])
    tt(Bbre, scP[:, 2], crb, ALU.mult)
    tt(tB, scP[:, 3], cib, ALU.mult)
    tt(Bbre, Bbre, tB, ALU.subtract)
    tt(Bbim, scP[:, 3], crb, ALU.mult)
    tt(tB, scP[:, 2], cib, ALU.mult)
    tt(Bbim, Bbim, tB, ALU.add)
    g4 = lambda i: G[i][:].rearrange("p (g i h) -> p g i h", g=8, i=8)
    QC = [G[8], G[9]]
    PB = [G[10], G[11]]

    def ksl(start, rev):
        return slice(start, start - 8 if start - 8 >= 0 else None, -1) if rev else slice(start, start + 8)

    def cmul_table(Are, Aim, d, ks, out, sB, rA, iA, t1, t2):
        dgs = slice(d * 8, d * 8 + 8)
        Lr = Lre[:, dgs, ks].unsqueeze(3).to_broadcast([128, 8, 8, 16])
        Li = Lim[:, dgs, ks].unsqueeze(3).to_broadcast([128, 8, 8, 16])
        Ar = Are[:, dgs, :].unsqueeze(2).to_broadcast([128, 8, 8, 16])
        Ai = Aim[:, dgs, :].unsqueeze(2).to_broadcast([128, 8, 8, 16])
        tt(rA, Ar, Lr, ALU.mult)
        tt(t1, Ai, Li, ALU.mult)
        tt(rA, rA, t1, ALU.subtract)
        tt(iA, Ar, Li, ALU.mult)
        tt(t2, Ai, Lr, ALU.mult)
        tt(iA, iA, t2, ALU.add)
        ts(rA, rA, sel[:, 0:1], ALU.mult)
        stt(out, iA, sB, rA, ALU.mult, ALU.add)

    for d in range(2):
        rev = (d == 1)
        cmul_table(scP[:, 0], scP[:, 1], d, ksl(15, True) if rev else ksl(8, False),
                   Cmat[:, d].rearrange("p g (i h) -> p g i h", i=8), sel[:, 1:2], g4(12), g4(13), g4(14), g4(15))
        cmul_table(scP[:, 0], scP[:, 1], d, ksl(7, True) if rev else ksl(7, False), g4(8 + d), sel[:, 1:2],
                   g4(12), g4(13), g4(14), g4(15))
        cmul_table(Bbre, Bbim, d, ksl(7, False) if rev else ksl(7, True), g4(10 + d), sel[:, 2:3],
                   g4(12), g4(13), g4(14), g4(15))
    k1 = G[12][:, 0:128]
    k2 = G[13][:, 0:128]
    k3 = G[14][:, 0:128]
    for g in range(8):
        for d in range(2):
            P.I(T_, "matmul", PR, ["pkk%d" % d], pk_[d][:], lhsT=PB[d][:, g * 128:(g + 1) * 128],
                rhs=QC[d][:, g * 128:(g + 1) * 128], start=True, stop=True)
        tt(k1, pk_[0][:], mats[:, 1, :], ALU.mult, r=PR + ["pkk0"])
        tt(k2, pk_[1][:], mats[:, 2, :], ALU.mult, r=PR + ["pkk1"])
        ts(k3, mats[:, 0, :], dsk[:, g:g + 1], ALU.mult)
        tt(k1, k1, k2, ALU.add)
        tt(Kc[:, g, :], k1, k3, ALU.add)
    stepB, drB, diB, mag1, s1, c1, nrB, denB, creB, cimB, tmpB = [G[i][:] for i in range(11)]
    act(stepB, saB[:, 2, :], AF.Exp)
    tt(drB, saB[:, 0, :], stepB, ALU.mult)
    tt(diB, saB[:, 1, :], stepB, ALU.mult)
    act(mag1, drB, AF.Exp)
    sincos(diB, s1, c1, 1024, ta=G[11], tkf=G[12])
    tt(c1, mag1, c1, ALU.mult)
    tt(s1, mag1, s1, ALU.mult)
    coef(c1, s1, saB[:, 0, :], saB[:, 1, :], creB, cimB, nrB, denB, tmpB)
    for d in range(2):
        hs = slice(d * 512, (d + 1) * 512)
        mage, ange, se, ce, Ere, Eim, t5, w1r, w1i = [G[i][:, 0:512] for i in range(11, 20)]
        act(mage, drB[:, hs], AF.Exp, scale=sep[:, d:d + 1])
        ts(ange, diB[:, hs], sep[:, d:d + 1], ALU.mult)
        sincos(ange, se, ce, 512, ta=G[0], tkf=G[3])
        tt(ce, mage, ce, ALU.mult)
        tt(se, mage, se, ALU.mult)
        tt(Ere, ce, creB[:, hs], ALU.mult)
        tt(t5, se, cimB[:, hs], ALU.mult)
        tt(Ere, Ere, t5, ALU.subtract)
        tt(Eim, ce, cimB[:, hs], ALU.mult)
        tt(t5, se, creB[:, hs], ALU.mult)
        tt(Eim, Eim, t5, ALU.add)
        tt(w1r, Ere, sbT[:, 0, hs], ALU.mult)
        tt(t5, Eim, sbT[:, 1, hs], ALU.mult)
        tt(w1r, w1r, t5, ALU.subtract)
        tt(w1i, Ere, sbT[:, 1, hs], ALU.mult)
        tt(t5, Eim, sbT[:, 0, hs], ALU.mult)
        tt(w1i, w1i, t5, ALU.add)
        dgs = slice(d * 8, d * 8 + 8)
        w1r3 = w1r.rearrange("p (g q) -> p g q", g=8)
        w1i3 = w1i.rearrange("p (g q) -> p g q", g=8)
        P.I(V, "tensor_copy", PR, PR, out=W1[:, dgs, 0:64], in_=w1r3)
        P.I(V, "tensor_copy", PR, PR, out=W1[:, dgs, 64:128], in_=w1i3)
        P.I(V, "tensor_copy", PR, PR, out=W2[:, dgs, 0:64], in_=w1i3)
        ts(W2[:, dgs, 64:128], w1r3, -1.0, ALU.mult)
    MK = ["tabA", "tabB", "S1", "S2", "Z1", "Z2", "ta", "tb", "ta2", "tb2", "sc"]
    P.alias(MK, PR)
    tabs = [(G[0], G[1], G[2]), (G[3], G[4], G[5])]
    S1, S2, Z1, Z2 = G[6], G[7], G[8], G[9]
    tA, tBb, tA2, tB2 = G[10], G[11], G[12], G[13]
    sc_ta, sc_tkf = G[14], G[15]
    it = 0
    for g in range(8):
        us = g % 2
        P.dma("sync", ustg[us][:], U8[g], writes=["ustg%d" % us])
        P.I(G_, "tensor_copy", ["ustg%d" % us], ["U8b%d" % us], out=U8b[us][:], in_=ustg[us][:])
        for d in range(2):
            dg = d * 8 + g
            tb = it % 2
            it += 1
            tk = ["tabA", "tabB"][tb]
            sinT, cosT, rT = tabs[tb]
            xs = (g % 2) * 2 + d
            xk = "Xin%d" % xs
            ts(sc_ta[:], cidx[:], thr[:, dg:dg + 1], ALU.mult, r=PR, w=["sc"])
            sincos(sc_ta[:], sinT[:], cosT[:], 1024, r=["sc", tk], w=["sc", tk], ta=G[16], tkf=G[17])
            ts(rT[:], cidx[:], 0.0, ALU.mult, s2=r8[:, dg:dg + 1], op1=ALU.add, r=PR + [tk], w=[tk])
            op_a, op_b = (ALU.add, ALU.subtract) if d == 0 else (ALU.subtract, ALU.add)
            for h in range(2):
                hs = slice(h * 512, (h + 1) * 512)
                o1, o1k = ring.next()
                o2, o2k = ring.next()
                P.I(T_, "matmul", PR + ["U8b%d" % us], [o1k], o1[:], lhsT=W1[:, dg, :], rhs=U8b[us][:, hs],
                    start=True, stop=True)
                P.I(T_, "matmul", PR + ["U8b%d" % us], [o2k], o2[:], lhsT=W2[:, dg, :], rhs=U8b[us][:, hs],
                    start=True, stop=True)
                tt(tA[:, hs], cosT[:, hs], o1[:], ALU.mult, r=[tk, o1k], w=["ta"])
                tt(tBb[:, hs], sinT[:, hs], o2[:], ALU.mult, r=[tk, o2k], w=["tb"])
                tt(S1[:, hs], tA[:, hs], tBb[:, hs], op_a, r=["ta", "tb"], w=["S1"], eng=G_)
                tt(tA2[:, hs], cosT[:, hs], o2[:], ALU.mult, r=[tk, o2k], w=["ta2"])
                tt(tB2[:, hs], sinT[:, hs], o1[:], ALU.mult, r=[tk, o1k], w=["tb2"])
                tt(S2[:, hs], tA2[:, hs], tB2[:, hs], op_b, r=["ta2", "tb2"], w=["S2"], eng=G_)
            rv = slice(None, None, -1) if d == 1 else slice(None)
            P.I(V, "tensor_tensor_scan", ["S1", tk], ["Z1"], out=Z1[:, rv], data0=rT[:, rv], data1=S1[:, rv],
                initial=0.0, op0=ALU.mult, op1=ALU.add)
            P.I(V, "tensor_tensor_scan", ["S2", tk], ["Z2"], out=Z2[:, rv], data0=rT[:, rv], data1=S2[:, rv],
                initial=0.0, op0=ALU.mult, op1=ALU.add)
            tt(tA[:], cosT[:], Z1[:], ALU.mult, r=[tk, "Z1"], w=["ta"])
            tt(tBb[:], sinT[:], Z2[:], ALU.mult, r=[tk, "Z2"], w=["tb"], eng=G_)
            if d == 0:
                P.I(V, "memset", [], [xk], Xin[xs][:, 0:1], 0.0)
                tt(Xin[xs][:, 1:1024], tA[:, 0:1023], tBb[:, 0:1023], ALU.subtract, r=["ta", "tb"], w=[xk])
            else:
                P.I(V, "memset", [], [xk], Xin[xs][:, 1023:1024], 0.0)
                tt(Xin[xs][:, 0:1023], tA[:, 1:1024], tBb[:, 1:1024], ALU.add, r=["ta", "tb"], w=[xk])
        for h in range(2):
            hs = slice(h * 512, (h + 1) * 512)
            x0 = (g % 2) * 2
            P.I(T_, "matmul", PR + ["U8b%d" % us], ["pso%d" % h], pso[h][:], lhsT=Kc[:, g, :], rhs=U8b[us][:, hs],
                start=True, stop=False)
            P.I(T_, "matmul", PR + ["Xin%d" % x0], ["pso%d" % h], pso[h][:], lhsT=Cmat[:, 0, g, :],
                rhs=Xin[x0][:, hs], start=False, stop=False)
            P.I(T_, "matmul", PR + ["Xin%d" % (x0 + 1)], ["pso%d" % h], pso[h][:], lhsT=Cmat[:, 1, g, :],
                rhs=Xin[x0 + 1][:, hs], start=False, stop=True)
            P.I(A_, "copy", ["pso%d" % h], ["ost%d" % h], out=ost[h][:], in_=pso[h][:])
            P.dma("sync", y8[g, :, hs], ost[h][:], reads=["ost%d" % h], key="ost%d" % h)
    P.emit()
    return nc


def consts_S():
    q = np.arange(128)
    epart = np.stack([7 - q // 16, q // 16], 1).astype(np.float32)
    ktab = np.tile(np.arange(-7, 9, dtype=np.float32)[None], (128, 1))
    sel = np.stack([(q < 64), -(q >= 64).astype(np.float32), (q >= 64)], 1).astype(np.float32)
    ii = q // 16
    ident = np.eye(128, dtype=np.float32)
    maskF = (ii[None, :] >= ii[:, None]).astype(np.float32)
    maskB = (ii[:, None] >= ii[None, :]).astype(np.float32)
    mats = np.ascontiguousarray(np.stack([ident, maskF, maskB], 1))
    cidx = np.tile(np.arange(1024, dtype=np.float32)[None], (128, 1))
    return epart, ktab, sel, mats, cidx


def prep_S(inp, l, c, uT_core):
    gs = slice(8 * c, 8 * c + 8)
    epart, ktab, sel, mats, cidx = consts_S()
    b_re, b_im = inp["ssm_b_re"][l][:, gs], inp["ssm_b_im"][l][:, gs]
    c_re, c_im = inp["ssm_c_re"][l][:, gs], inp["ssm_c_im"][l][:, gs]
    a_re, a_im, ls = inp["ssm_a_re"][l][:, gs], inp["ssm_a_im"][l][:, gs], inp["ssm_log_step"][l][:, gs]

    def bside(b):
        t = b.transpose(3, 0, 1, 2).reshape(16, 1024)
        return np.tile(t, (8, 1))
    bT = np.stack([bside(b_re), bside(b_im)], 1)
    flat = lambda a: np.broadcast_to(a.reshape(1, 1024), (128, 1024))
    aB = np.stack([flat(a_re), flat(a_im), flat(np.broadcast_to(ls[:, :, None], (2, 8, 64)))], 1)

    def cside_c(cc):
        t = cc.transpose(3, 0, 1, 2).reshape(64, 16, 16)
        return np.concatenate([t, t], 0)

    def cside_b(b):
        t = b.transpose(2, 0, 1, 3).reshape(64, 16, 16)
        return np.concatenate([t, t], 0)
    cP = np.stack([cside_c(c_re), cside_c(c_im), cside_b(b_re), cside_b(b_im)], 1)

    def cside_a(a):
        t = a.transpose(2, 0, 1).reshape(64, 16)
        return np.concatenate([t, t], 0)
    aC = np.stack([cside_a(a_re), cside_a(a_im), cside_a(np.broadcast_to(ls[:, :, None], (2, 8, 64)))], 1)
    dsk = np.tile(inp["ssm_d"][l][128 * c:128 * (c + 1)].reshape(8, 16).T, (8, 1))
    U8 = uT_core.reshape(8, 16, 1024, 8).transpose(0, 3, 1, 2).reshape(8, 128, 1024)
    f = lambda a: np.ascontiguousarray(a, dtype=np.float32)
    return dict(U8=f(U8), bT=f(bT), aB=f(aB), epart=f(epart), cP=f(cP), aC=f(aC), ktab=f(ktab), sel=f(sel),
                dsk=f(dsk), mats=f(mats), cidx=f(cidx))


def unpack_y8(y8):
    return np.ascontiguousarray(y8.reshape(8, 8, 16, 1024).transpose(0, 2, 3, 1).reshape(128, L))


_PROGS = {}


def _prog(name):
    if name not in _PROGS:
        _PROGS[name] = dict(P=build_P, S=build_S, A=build_A, M=build_M, E=build_E, C=build_C)[name]()
    return _PROGS[name]


def _run(name, maps):
    res = run_bass_kernel_spmd(_prog(name), maps, core_ids=list(range(NCORES)))
    return res.results


def _c(a, dt=None):
    return np.ascontiguousarray(a, dtype=dt)


def kernel(**inp):
    inp = {k: np.asarray(v) for k, v in inp.items()}
    x = _c(inp["x"][0], np.float32)
    ctab, stab, pm, bd, ident = consts_A()
    tokid, tri, _ = consts_E()
    for l in range(2):
        xT = _c(x.T)
        cs = lambda a, c: _c(a[:, c * TPC:(c + 1) * TPC])
        g = _c(inp["mix_norm_g"][l].reshape(16, 128).T)
        W = _c(inp["w_in"][l])
        r = _run("P", [dict(xT=cs(xT, c), g=g, W=W) for c in range(NCORES)])
        projT = np.concatenate([q["projT"] for q in r], axis=1)
        r = _run("S", [prep_S(inp, l, c, projT[128 * c:128 * (c + 1)]) for c in range(NCORES)])
        ysT = np.concatenate([unpack_y8(q["y8"]) for q in r], axis=0)
        lam_init = 0.8 - 0.6 * math.exp(-0.3 * l)
        small = np.zeros((128, 8), np.float32)
        small[:, 0] = np.tile(inp["q_norm_g"][l], 2)
        small[:, 1] = np.tile(inp["k_norm_g"][l], 2)
        small[:, 2] = inp["subln_g"][l]
        small[:, 3] = lam_init
        small[:, 4] = 1.0 - lam_init
        lamin = _c(np.broadcast_to(np.stack([inp["lambda_q1"][l], inp["lambda_k1"][l], inp["lambda_q2"][l],
                                             inp["lambda_k2"][l]])[None], (128, 4, 64)), np.float32)
        r = _run("A", [dict(qT=_c(projT[1024 + h * 128:1024 + (h + 1) * 128]),
                            kT=_c(projT[2048 + h * 128:2048 + (h + 1) * 128]),
                            vT=_c(projT[3072 + h * 128:3072 + (h + 1) * 128]), ctab=ctab, stab=stab, small=small,
                            lamin=lamin, pmd=pm, bdd=bd, identd=ident) for h in range(NCORES)])
        yaT = np.concatenate([q["oT"] for q in r], axis=0)
        glT = projT[4096:8192]
        gate_b = _c(inp["gate_b"][l].reshape(2, 16, 128).transpose(2, 0, 1).reshape(128, 32))
        gffn = _c(inp["ffn_norm_g"][l].reshape(16, 128).T)
        w_r = _c(inp["w_router"][l].reshape(16, 128, 16).transpose(1, 0, 2))
        r = _run("M", [dict(ysT=cs(ysT, c), yaT=cs(yaT, c), glT=cs(glT, c), xT=cs(xT, c), w_glu=_c(inp["w_glu"][l]),
                            w_brs=_c(inp["w_br_ssm"][l]), w_bra=_c(inp["w_br_attn"][l]), w_out=_c(inp["w_out"][l]),
                            gate_b=gate_b, gffn=gffn, w_r=w_r) for c in range(NCORES)])
        x1 = _c(np.concatenate([q["x1T"] for q in r], axis=1).T)
        xn = _c(np.concatenate([q["xnT"] for q in r], axis=1).T)
        aff = _c(np.concatenate([q["affT"] for q in r], axis=1).T)
        maps = []
        for c in range(NCORES):
            affc = _c(aff[:, 2 * c:2 * c + 2].T.reshape(2, 128, 64))
            ebase = _c(np.tile(np.array([[2 * c * 1024, (2 * c + 1) * 1024]], np.float32), (128, 1)))
            maps.append(dict(affc=affc, xn=xn, wg=_c(inp["w_e_gate"][l][2 * c:2 * c + 2]),
                             wu=_c(inp["w_e_up"][l][2 * c:2 * c + 2]), wd=_c(inp["w_e_down"][l][2 * c:2 * c + 2]),
                             tokid=tokid, tri=tri, ebase=ebase, identd=ident))
        r = _run("E", maps)
        yeT = np.stack([q["yeT"] for q in r]).reshape(16, D, 1024)
        posrow = np.stack([q["posrow"] for q in r]).reshape(16, L)
        ye = _c(yeT.reshape(16, D, 8, 128).transpose(0, 3, 2, 1).reshape(16 * 1024, D))
        prow = _c(posrow.T)
        rs = lambda a, c: _c(a[c * TPC:(c + 1) * TPC])
        r = _run("C", [dict(x1=rs(x1, c), ye=ye, prow=rs(prow, c), aff=rs(aff, c)) for c in range(NCORES)])
        x = np.concatenate([q["x2"] for q in r], axis=0)
    return x[None].astype(np.float32)
```

```python
import contextlib
import math
import numpy as np
import ml_dtypes
import concourse.bass as bass
import concourse.mybir as mybir
from concourse.bass_utils import run_bass_kernel_spmd

F32 = mybir.dt.float32
BF16 = mybir.dt.bfloat16
I32 = mybir.dt.int32
ALU = mybir.AluOpType
AF = mybir.ActivationFunctionType
AX = mybir.AxisListType
IOA = bass.IndirectOffsetOnAxis

NCORES = 8
L = 8192
D = 2048
TPC = L // NCORES
EPS = 1e-6
COMPUTE = ("tensor", "vector", "scalar", "gpsimd")
BIG = 1.0e6


class Prog:
    def __init__(self, nc, same_engine_sync=True):
        self.nc = nc
        self.ops = []
        self.last_w = {}
        self.readers = {}
        self.stack = contextlib.ExitStack()
        self.same_engine_sync = same_engine_sync

    def sb(self, name, shape, dtype):
        return self.stack.enter_context(self.nc.sbuf_tensor(name, list(shape), dtype))

    def ps(self, name, shape, dtype=F32):
        return self.stack.enter_context(self.nc.psum_tensor(name, list(shape), dtype))

    def op(self, eng, fn, reads=(), writes=(), dma_key=None):
        idx = len(self.ops)
        deps = set()
        for k in reads:
            if k in self.last_w:
                deps.add(self.last_w[k])
        for k in writes:
            if k in self.last_w:
                deps.add(self.last_w[k])
            deps.update(self.readers.get(k, ()))
        for k in reads:
            self.readers.setdefault(k, []).append(idx)
        for k in writes:
            self.last_w[k] = idx
            self.readers[k] = []
        self.ops.append(dict(eng=eng, fn=fn, deps=deps, dma_key=dma_key, rw=(tuple(reads), tuple(writes))))
        return idx

    def alias(self, new_keys, old_keys):
        acc = []
        for k in old_keys:
            if k in self.last_w:
                acc.append(self.last_w[k])
            acc.extend(self.readers.get(k, ()))
        for k in new_keys:
            self.readers.setdefault(k, []).extend(acc)

    def I(self, eng, meth, reads, writes, *a, **kw):
        return self.op(eng, lambda e: getattr(e, meth)(*a, **kw), reads=reads, writes=writes)

    def dma(self, eng, out, in_, reads=(), writes=(), key=None, **kw):
        if key is None:
            key = writes[0] if writes else reads[0]
        return self.op(eng, lambda e: e.dma_start(out=out, in_=in_, **kw),
                       reads=reads, writes=writes, dma_key=("dma", key))

    def idma(self, out, out_off, in_, in_off, reads, writes, key, bounds=None):
        regs = self.__dict__.setdefault("_bregs", {})

        def fn(e):
            if bounds is None:
                return e.indirect_dma_start(out=out, out_offset=out_off, in_=in_, in_offset=in_off)
            if bounds not in regs:
                regs[bounds] = e.to_reg(bounds)
            return e.indirect_dma_start(out=out, out_offset=out_off, in_=in_, in_offset=in_off,
                                        bounds_check=regs[bounds], oob_is_err=False)
        return self.op("gpsimd", fn, reads=reads, writes=writes, dma_key=("dma", key))

    def emit(self):
        nc = self.nc
        ops = self.ops
        ses = self.same_engine_sync

        def skip(p, o):
            return (p["dma_key"] is None and o["dma_key"] is None and p["eng"] == o["eng"]
                    and (p["eng"] == "tensor" or not ses))

        signal = [o["dma_key"] is not None for o in ops]
        for o in ops:
            for d in o["deps"]:
                if not skip(ops[d], o):
                    signal[d] = True
        sem_names = [("eng", e) for e in COMPUTE]
        for o in ops:
            if o["dma_key"] is not None and o["dma_key"] not in sem_names:
                sem_names.append(o["dma_key"])
        sems = {}
        for n, sn in enumerate(sem_names):
            sems[sn] = self.stack.enter_context(nc.semaphore("s%d" % n))
        counter = {sn: 0 for sn in sem_names}
        sig = [None] * len(ops)
        for i, o in enumerate(ops):
            if not signal[i]:
                continue
            sk = o["dma_key"] if o["dma_key"] is not None else ("eng", o["eng"])
            counter[sk] += 16 if o["dma_key"] is not None else 1
            sig[i] = (sk, counter[sk])
        by_eng = {}
        for i, o in enumerate(ops):
            by_eng.setdefault(o["eng"], []).append(i)
        by_eng.setdefault("sync", [])
        with nc.Block() as block:
            for en in list(by_eng.keys()):
                def body(e, en=en):
                    waited = {}
                    for i in by_eng[en]:
                        o = ops[i]
                        need = {}
                        for d in o["deps"]:
                            if sig[d] is None or skip(ops[d], o):
                                continue
                            sk, v = sig[d]
                            if v > need.get(sk, 0):
                                need[sk] = v
                        for sk, v in need.items():
                            if waited.get(sk, 0) >= v:
                                continue
                            e.wait_ge(sems[sk], v)
                            waited[sk] = v
                        try:
                            ins = o["fn"](e)
                        except Exception:
                            print("EMIT FAIL op", i, en, o["rw"], o["dma_key"])
                            raise
                        if sig[i] is not None:
                            ins.then_inc(sems[sig[i][0]], 16 if o["dma_key"] is not None else 1)
                    if en == "sync":
                        for sk in sem_names:
                            if counter[sk] > 0:
                                e.wait_ge(sems[sk], counter[sk])
                getattr(block, en)(body)
        self.stack.close()


def new_nc():
    return bass.Bass("TRN2", target_bir_lowering=False)


def din(nc, name, shape, dt=F32):
    return nc.dram_tensor(name, list(shape), dt, kind="ExternalInput").ap()


def dout(nc, name, shape, dt=F32):
    return nc.dram_tensor(name, list(shape), dt, kind="ExternalOutput").ap()


class WStream:
    CB = 128

    def __init__(self, P, nslots=4, ktmax=16):
        self.P = P
        self.n = nslots
        self.wst = [P.sb("wst%d" % i, [128, ktmax, self.CB], F32) for i in range(nslots)]
        self.wbf = [P.sb("wbf%d" % i, [128, ktmax, self.CB], BF16) for i in range(nslots)]
        self.ctr = 0

    def load(self, W, KT, cb):
        s = self.ctr % self.n
        self.ctr += 1
        P = self.P
        CB = self.CB
        P.dma("sync", self.wst[s][:, 0:KT, :], W[:, cb * CB:(cb + 1) * CB].rearrange("(k p) c -> p k c", p=128),
              writes=["wst%d" % s])
        P.I("gpsimd", "tensor_copy", ["wst%d" % s], ["wbf%d" % s], out=self.wbf[s][:, 0:KT, :],
            in_=self.wst[s][:, 0:KT, :])
        return self.wbf[s], "wbf%d" % s


class PsRing:
    def __init__(self, P, n, prefix="ps"):
        self.t = [P.ps("%s%d" % (prefix, i), [128, 512], F32) for i in range(n)]
        self.k = ["%s%d" % (prefix, i) for i in range(n)]
        self.c = 0

    def next(self):
        i = self.c % len(self.t)
        self.c += 1
        return self.t[i], self.k[i]


def linear(P, ws, pr, srcs, N, T, evac):
    for m in range(N // 128):
        wb = [ws.load(W, KT, m) for (W, a, KT, ak) in srcs]
        for h in range(T // 512):
            outs = []
            for si, (W, a, KT, ak) in enumerate(srcs):
                ps, pk = pr.next()
                for k in range(KT):
                    P.I("tensor", "matmul", [wb[si][1], ak], [pk], ps[:], lhsT=wb[si][0][:, k, :],
                        rhs=a[:, k, h * 512:(h + 1) * 512], start=(k == 0), stop=(k == KT - 1))
                outs.append((ps, pk))
            evac(m, h, outs)


def rstd_from_ps(P, ps, pk, out_ap, okey, tmp, tkey, scale):
    P.I("scalar", "activation", [pk], [tkey], out=tmp, in_=ps, func=AF.Sqrt, bias=EPS, scale=scale)
    P.I("vector", "reciprocal", [tkey], [okey], out=out_ap, in_=tmp)


def build_P():
    nc = new_nc()
    xT = din(nc, "xT", [D, TPC])
    g = din(nc, "g", [128, 16])
    W = din(nc, "W", [D, 8192])
    out = dout(nc, "projT", [8192, TPC])
    P = Prog(nc)
    xf = [P.sb("xf%d" % i, [128, TPC], F32) for i in range(2)]
    hT = P.sb("hT", [128, 16, TPC], BF16)
    sq = [P.sb("sq%d" % i, [128, TPC], BF16) for i in range(2)]
    ones = P.sb("ones", [128, 128], BF16)
    gsb = P.sb("gsb", [128, 16], F32)
    rstd = P.sb("rstd", [128, TPC], F32)
    tmp = P.sb("tmp", [128, 512], F32)
    ost = [P.sb("ost%d" % i, [128, 512], F32) for i in range(4)]
    ws = WStream(P, 4, 16)
    pr = PsRing(P, 4)
    pss = [P.ps("pss%d" % i, [128, 512], F32) for i in range(2)]
    P.I("vector", "memset", [], ["ones"], ones[:], 1.0)
    P.dma("sync", gsb[:], g, writes=["gsb"])
    for k in range(16):
        s = k % 2
        P.dma("sync", xf[s][:], xT[k * 128:(k + 1) * 128, :], writes=["xf%d" % s])
        P.I("scalar", "activation", ["xf%d" % s, "gsb"], ["hT"], out=hT[:, k, :], in_=xf[s][:], func=AF.Copy,
            scale=gsb[:, k:k + 1])
        P.I("vector", "tensor_tensor", ["xf%d" % s], ["sq%d" % s], out=sq[s][:], in0=xf[s][:], in1=xf[s][:],
            op=ALU.mult)
        for h in range(2):
            P.I("tensor", "matmul", ["ones", "sq%d" % s], ["pss%d" % h], pss[h][:], lhsT=ones[:],
                rhs=sq[s][:, h * 512:(h + 1) * 512], start=(k == 0), stop=(k == 15))
    for h in range(2):
        rstd_from_ps(P, pss[h][:], "pss%d" % h, rstd[:, h * 512:(h + 1) * 512], "rstd%d" % h, tmp[:], "tmp",
                     1.0 / D)
    cnt = [0]

    def evac(m, h, outs):
        ps, pk = outs[0]
        s = cnt[0] % 4
        cnt[0] += 1
        P.I("vector", "tensor_tensor", [pk, "rstd%d" % h], ["ost%d" % s], out=ost[s][:], in0=ps[:],
            in1=rstd[:, h * 512:(h + 1) * 512], op=ALU.mult)
        P.dma("scalar", out[m * 128:(m + 1) * 128, h * 512:(h + 1) * 512], ost[s][:], reads=["ost%d" % s],
              key="ost%d" % s)

    linear(P, ws, pr, [(W, hT, 16, "hT")], 8192, TPC, evac)
    P.emit()
    return nc


def build_M():
    nc = new_nc()
    ysT = din(nc, "ysT", [1024, TPC])
    yaT = din(nc, "yaT", [1024, TPC])
    glT = din(nc, "glT", [4096, TPC])
    xT = din(nc, "xT", [D, TPC])
    w_glu = din(nc, "w_glu", [1024, 1024])
    w_brs = din(nc, "w_brs", [1024, D])
    w_bra = din(nc, "w_bra", [1024, D])
    w_out = din(nc, "w_out", [D, D])
    gate_b = din(nc, "gate_b", [128, 32])
    gffn = din(nc, "gffn", [128, 16])
    w_r = din(nc, "w_r", [128, 16, 16])
    x1T = dout(nc, "x1T", [D, TPC])
    xnT = dout(nc, "xnT", [D, TPC], BF16)
    affT = dout(nc, "affT", [16, TPC])
    P = Prog(nc)
    arena = P.sb("arena", [128, 16 * TPC], F32)
    ab = arena[:].bitcast(BF16)
    ys = ab[:, 0:8192].rearrange("p (k t) -> p k t", k=8)
    ys2 = ab[:, 8192:16384].rearrange("p (k t) -> p k t", k=8)
    ya = ab[:, 16384:24576].rearrange("p (k t) -> p k t", k=8)
    x1 = arena[:].rearrange("p (k t) -> p k t", k=16)
    merged = P.sb("merged", [128, 16, TPC], BF16)
    ldf = [P.sb("ldf%d" % i, [128, TPC], F32) for i in range(2)]
    gb = P.sb("gb", [128, 32], F32)
    gf = P.sb("gf", [128, 16], F32)
    wr = P.sb("wr", [128, 16, 16], F32)
    ones = P.sb("ones", [128, 128], BF16)
    ones16 = P.sb("ones16", [16, 16], F32)
    rstd = P.sb("rstd", [128, TPC], F32)
    tmp = P.sb("tmp", [128, 512], F32)
    tA = [P.sb("tA%d" % i, [128, 512], F32) for i in range(2)]
    tB = [P.sb("tB%d" % i, [128, 512], F32) for i in range(2)]
    glt = [P.sb("glt%d" % i, [128, 512], F32) for i in range(4)]
    gs = [P.sb("gs%d" % i, [128, 512], F32) for i in range(4)]
    sqb = [P.sb("sqb%d" % i, [128, 512], BF16) for i in range(2)]
    xnb = [P.sb("xnb%d" % i, [128, 512], BF16) for i in range(2)]
    esb = P.sb("esb", [16, 512], F32)
    rsb = P.sb("rsb", [16, 512], F32)
    asb = P.sb("asb", [16, 512], F32)
    ws = WStream(P, 4, 16)
    pr = PsRing(P, 4)
    pss = [P.ps("pss%d" % i, [128, 512], F32) for i in range(2)]
    plg = [P.ps("plg%d" % i, [16, 512], F32) for i in range(2)]
    P.I("vector", "memset", [], ["ones"], ones[:], 1.0)
    P.I("vector", "memset", [], ["ones16"], ones16[:], 1.0)
    P.dma("sync", gb[:], gate_b, writes=["gb"])
    P.dma("sync", gf[:], gffn, writes=["gf"])
    P.dma("sync", wr[:], w_r, writes=["wr"])
    for k in range(8):
        s = k % 2
        P.dma("sync", ldf[s][:], ysT[k * 128:(k + 1) * 128, :], writes=["ldf%d" % s])
        P.I("scalar", "activation", ["ldf%d" % s], ["ys"], out=ys[:, k, :], in_=ldf[s][:], func=AF.Gelu)
    for k in range(8):
        s = k % 2
        P.dma("sync", ldf[s][:], yaT[k * 128:(k + 1) * 128, :], writes=["ldf%d" % s])
        P.I("vector", "tensor_copy", ["ldf%d" % s], ["ya"], out=ya[:, k, :], in_=ldf[s][:])
    c1 = [0]

    def evac_glu(m, h, outs):
        ps, pk = outs[0]
        s = c1[0] % 2
        c1[0] += 1
        P.I("scalar", "activation", [pk], ["tA%d" % s], out=tA[s][:], in_=ps[:], func=AF.Sigmoid)
        P.I("vector", "tensor_tensor", ["tA%d" % s, "ys"], ["ys2"], out=ys2[:, m, h * 512:(h + 1) * 512],
            in0=ys[:, m, h * 512:(h + 1) * 512], in1=tA[s][:], op=ALU.mult)

    linear(P, ws, pr, [(w_glu, ys, 8, "ys")], 1024, TPC, evac_glu)
    c2 = [0]

    def evac_br(m, h, outs):
        (pa, pak), (pb, pbk) = outs
        s = c2[0] % 2
        c2[0] += 1
        for br in range(2):
            gi = 2 * s + br
            P.dma("sync", glt[gi][:], glT[br * D + m * 128: br * D + (m + 1) * 128, h * 512:(h + 1) * 512],
                  writes=["glt%d" % gi])
            P.I("scalar", "activation", ["glt%d" % gi, "gb"], ["gs%d" % gi], out=gs[gi][:], in_=glt[gi][:],
                func=AF.Sigmoid, bias=gb[:, br * 16 + m: br * 16 + m + 1])
        P.I("vector", "tensor_tensor", [pak, "gs%d" % (2 * s)], ["tA%d" % s], out=tA[s][:], in0=pa[:],
            in1=gs[2 * s][:], op=ALU.mult)
        P.I("vector", "tensor_tensor", [pbk, "gs%d" % (2 * s + 1)], ["tB%d" % s], out=tB[s][:], in0=pb[:],
            in1=gs[2 * s + 1][:], op=ALU.mult)
        P.I("vector", "tensor_tensor", ["tA%d" % s, "tB%d" % s], ["merged"],
            out=merged[:, m, h * 512:(h + 1) * 512], in0=tA[s][:], in1=tB[s][:], op=ALU.add)

    linear(P, ws, pr, [(w_brs, ys2, 8, "ys2"), (w_bra, ya, 8, "ya")], D, TPC, evac_br)
    P.alias(["x1"], ["ys", "ys2", "ya"])
    c3 = [0]

    def evac_out(m, h, outs):
        ps, pk = outs[0]
        s = c3[0] % 2
        c3[0] += 1
        sl = slice(h * 512, (h + 1) * 512)
        P.dma("sync", tA[s][:], xT[m * 128:(m + 1) * 128, sl], writes=["tA%d" % s])
        P.I("vector", "tensor_tensor", [pk, "tA%d" % s], ["x1"], out=x1[:, m, sl], in0=ps[:], in1=tA[s][:],
            op=ALU.add)
        P.dma("scalar", x1T[m * 128:(m + 1) * 128, sl], x1[:, m, sl], reads=["x1"], key="x1out")
        P.I("scalar", "activation", ["x1"], ["sqb%d" % s], out=sqb[s][:], in_=x1[:, m, sl], func=AF.Square)
        P.I("tensor", "matmul", ["ones", "sqb%d" % s], ["pss%d" % h], pss[h][:], lhsT=ones[:], rhs=sqb[s][:],
            start=(m == 0), stop=(m == 15))

    linear(P, ws, pr, [(w_out, merged, 16, "merged")], D, TPC, evac_out)
    for h in range(2):
        sl = slice(h * 512, (h + 1) * 512)
        rstd_from_ps(P, pss[h][:], "pss%d" % h, rstd[:, sl], "rstd%d" % h, tmp[:], "tmp", 1.0 / D)
        for m in range(16):
            s = m % 2
            P.I("vector", "scalar_tensor_tensor", ["x1", "gf", "rstd%d" % h], ["tB%d" % s], out=tB[s][:],
                in0=x1[:, m, sl], scalar=gf[:, m:m + 1], in1=rstd[:, sl], op0=ALU.mult, op1=ALU.mult)
            P.I("tensor", "matmul", ["wr", "tB%d" % s], ["plg%d" % h], plg[h][:], lhsT=wr[:, m, :], rhs=tB[s][:],
                start=(m == 0), stop=(m == 15))
            P.I("scalar", "activation", ["tB%d" % s], ["xnb%d" % s], out=xnb[s][:], in_=tB[s][:], func=AF.Copy)
            P.dma("scalar", xnT[m * 128:(m + 1) * 128, sl], xnb[s][:], reads=["xnb%d" % s], key="xnb%d" % s)
        P.I("scalar", "activation", ["plg%d" % h], ["esb"], out=esb[:], in_=plg[h][:], func=AF.Exp)
        P.I("tensor", "matmul", ["ones16", "esb"], ["plg%d" % h], plg[h][:], lhsT=ones16[:], rhs=esb[:],
            start=True, stop=True)
        P.I("vector", "reciprocal", ["plg%d" % h], ["rsb"], out=rsb[:], in_=plg[h][:])
        P.I("vector", "tensor_tensor", ["esb", "rsb"], ["asb"], out=asb[:], in0=esb[:], in1=rsb[:], op=ALU.mult)
        P.dma("scalar", affT[:, sl], asb[:], reads=["asb"], key="asb")
    P.emit()
    return nc


def build_E():
    nc = new_nc()
    affc = din(nc, "affc", [2, 128, 64])
    xn = din(nc, "xn", [L, D], BF16)
    wg = din(nc, "wg", [2, D, 1024])
    wu = din(nc, "wu", [2, D, 1024])
    wd = din(nc, "wd", [2, 1024, D])
    tokid = din(nc, "tokid", [128, 64, 2], I32)
    tri = din(nc, "tri", [128, 128])
    ebase = din(nc, "ebase", [128, 2])
    identd = din(nc, "identd", [128, 128], BF16)
    yeT = dout(nc, "yeT", [2, D, 1024])
    posrow = dout(nc, "posrow", [2, 128, 64], I32)
    idxl = [nc.dram_tensor("idxl%d" % i, [1024, 2], I32, kind="Internal").ap() for i in range(2)]
    P = Prog(nc)
    af = P.sb("af", [128, 2, 64], F32)
    tok = P.sb("tok", [128, 64, 2], I32)
    trs = P.sb("trs", [128, 128], F32)
    ebs = P.sb("ebs", [128, 2], F32)
    ebm = P.sb("ebm", [128, 2], F32)
    ident = P.sb("ident", [128, 128], BF16)
    onesf = P.sb("onesf", [128, 128], F32)
    ones64 = P.sb("ones64", [128, 64], F32)
    lo = P.sb("lo", [128, 2], F32)
    hi = P.sb("hi", [128, 2], F32)
    mid = P.sb("mid", [128, 2], F32)
    cntp = P.sb("cntp", [128, 2], F32)
    cond = P.sb("cond", [128, 2], F32)
    d1 = P.sb("d1", [128, 2], F32)
    d2 = P.sb("d2", [128, 2], F32)
    junk = P.sb("junk", [128, 2, 64], F32)
    mask = P.sb("mask", [128, 2, 64], F32)
    incl = P.sb("incl", [128, 2, 64], F32)
    tot = P.sb("tot", [128, 2], F32)
    offm1 = P.sb("offm1", [128, 2], F32)
    posf = P.sb("posf", [128, 2, 64], F32)
    t1 = P.sb("t1", [128, 2, 64], F32)
    t2 = P.sb("t2", [128, 2, 64], F32)
    posi = P.sb("posi", [128, 2, 64], I32)
    rowi = P.sb("rowi", [128, 2, 64], I32)
    idxs = P.sb("idxs", [128, 2, 8, 2], I32)
    xe = [P.sb("xe%d" % i, [128, D], BF16) for i in range(2)]
    xeT = P.sb("xeT", [128, 16, 1024], BF16)
    act = P.sb("act", [128, 8, 1024], BF16)
    tA = [P.sb("tA%d" % i, [128, 512], F32) for i in range(2)]
    ost = [P.sb("ost%d" % i, [128, 512], F32) for i in range(2)]
    ws = WStream(P, 4, 16)
    pr = PsRing(P, 4)
    pc = P.ps("pc", [128, 2], F32)
    pT = [P.ps("pT%d" % i, [128, 4, 128], BF16) for i in range(2)]
    P.dma("sync", af[:], affc.rearrange("e p f -> p e f"), writes=["af"])
    P.dma("sync", tok[:], tokid, writes=["tok"])
    P.dma("sync", trs[:], tri, writes=["trs"])
    P.dma("sync", ebs[:], ebase, writes=["ebs"])
    P.dma("sync", ident[:], identd, writes=["ident"])
    P.I("vector", "memset", [], ["onesf"], onesf[:], 1.0)
    P.I("vector", "memset", [], ["ones64"], ones64[:], 1.0)
    P.I("vector", "memset", [], ["lo"], lo[:], 0.0)
    P.I("vector", "memset", [], ["hi"], hi[:], 1.0)
    P.I("vector", "tensor_scalar", ["ebs"], ["ebm"], out=ebm[:], in0=ebs[:], scalar1=-BIG, scalar2=None,
        op0=ALU.add)
    V = "vector"
    for it in range(30):
        P.I(V, "tensor_tensor", ["lo", "hi"], ["mid"], out=mid[:], in0=lo[:], in1=hi[:], op=ALU.add)
        P.I(V, "tensor_scalar", ["mid"], ["mid"], out=mid[:], in0=mid[:], scalar1=0.5, scalar2=None, op0=ALU.mult)
        for e in range(2):
            P.I(V, "tensor_scalar", ["af", "mid"], ["junk"], out=junk[:, e, :], in0=af[:, e, :],
                scalar1=mid[:, e:e + 1], scalar2=None, op0=ALU.is_ge)
            P.I(V, "reduce_sum", ["junk"], ["cntp"], out=cntp[:, e:e + 1], in_=junk[:, e, :], axis=AX.X)
        P.I("tensor", "matmul", ["onesf", "cntp"], ["pc"], pc[:], lhsT=onesf[:], rhs=cntp[:], start=True, stop=True)
        P.I(V, "tensor_scalar", ["pc"], ["cond"], out=cond[:], in0=pc[:], scalar1=1024.0, scalar2=None,
            op0=ALU.is_ge)
        P.I(V, "tensor_tensor", ["mid", "lo"], ["d1"], out=d1[:], in0=mid[:], in1=lo[:], op=ALU.subtract)
        P.I(V, "tensor_tensor", ["d1", "cond"], ["d1"], out=d1[:], in0=d1[:], in1=cond[:], op=ALU.mult)
        P.I(V, "tensor_tensor", ["hi", "mid"], ["d2"], out=d2[:], in0=hi[:], in1=mid[:], op=ALU.subtract)
        P.I(V, "tensor_tensor", ["d2", "cond"], ["d2"], out=d2[:], in0=d2[:], in1=cond[:], op=ALU.mult)
        P.I(V, "tensor_tensor", ["lo", "d1"], ["lo"], out=lo[:], in0=lo[:], in1=d1[:], op=ALU.add)
        P.I(V, "tensor_tensor", ["mid", "d2"], ["hi"], out=hi[:], in0=mid[:], in1=d2[:], op=ALU.add)
    for e in range(2):
        P.I(V, "tensor_scalar", ["af", "lo"], ["mask"], out=mask[:, e, :], in0=af[:, e, :], scalar1=lo[:, e:e + 1],
            scalar2=None, op0=ALU.is_ge)
        P.I(V, "tensor_tensor_scan", ["mask", "ones64"], ["incl"], out=incl[:, e, :], data0=ones64[:],
            data1=mask[:, e, :], initial=0.0, op0=ALU.mult, op1=ALU.add)
        P.I(V, "tensor_copy", ["incl"], ["tot"], out=tot[:, e:e + 1], in_=incl[:, e, 63:64])
    P.I("tensor", "matmul", ["trs", "tot"], ["pc"], pc[:], lhsT=trs[:], rhs=tot[:], start=True, stop=True)
    P.I(V, "tensor_scalar", ["pc"], ["offm1"], out=offm1[:], in0=pc[:], scalar1=-1.0, scalar2=None, op0=ALU.add)
    for e in range(2):
        P.I(V, "tensor_scalar", ["incl", "offm1"], ["posf"], out=posf[:, e, :], in0=incl[:, e, :],
            scalar1=offm1[:, e:e + 1], scalar2=None, op0=ALU.add)
        P.I(V, "scalar_tensor_tensor", ["posf", "mask"], ["t1"], out=t1[:, e, :], in0=posf[:, e, :], scalar=-BIG,
            in1=mask[:, e, :], op0=ALU.add, op1=ALU.mult)
        P.I(V, "tensor_scalar", ["t1"], ["t1"], out=t1[:, e, :], in0=t1[:, e, :], scalar1=BIG, scalar2=None,
            op0=ALU.add)
        P.I(V, "tensor_copy", ["t1"], ["posi"], out=posi[:, e, :], in_=t1[:, e, :])
        P.I(V, "scalar_tensor_tensor", ["posf", "mask", "ebm"], ["t2"], out=t2[:, e, :], in0=posf[:, e, :],
            scalar=ebm[:, e:e + 1], in1=mask[:, e, :], op0=ALU.add, op1=ALU.mult)
        P.I(V, "tensor_scalar", ["t2"], ["t2"], out=t2[:, e, :], in0=t2[:, e, :], scalar1=BIG, scalar2=None,
            op0=ALU.add)
        P.I(V, "tensor_copy", ["t2"], ["rowi"], out=rowi[:, e, :], in_=t2[:, e, :])
    P.dma("sync", posrow.rearrange("e p f -> p e f"), rowi[:], reads=["rowi"], key="rowi")
    c0 = [0]
    for e in range(2):
        fk = []
        for f in range(64):
            P.idma(idxl[e], IOA(ap=posi[:, e, f:f + 1], axis=0), tok[:, f, :], None, reads=["posi", "tok"],
                   writes=["idxl%d_%d" % (e, f)], key="idxl%d" % e, bounds=1023)
            fk.append("idxl%d_%d" % (e, f))
        P.dma("sync", idxs[:, e, :, :], idxl[e].rearrange("(p j) o -> p j o", j=8), reads=fk, writes=["idxs%d" % e])
        for jt in range(8):
            s = jt % 2
            P.idma(xe[s][:, :], None, xn, IOA(ap=idxs[:, e, jt, 0:1], axis=0), reads=["idxs%d" % e],
                   writes=["xe%d" % s], key="xe%d" % s)
            for kq in range(4):
                ti = c0[0] % 2
                c0[0] += 1
                for j in range(4):
                    k = kq * 4 + j
                    P.I("tensor", "transpose", ["xe%d" % s, "ident"], ["pT%d" % ti], out=pT[ti][:, j, :],
                        in_=xe[s][:, k * 128:(k + 1) * 128], identity=ident[:])
                P.I("scalar" if kq % 2 else "vector", "tensor_copy" if kq % 2 == 0 else "copy", ["pT%d" % ti],
                    ["xeT"], out=xeT[:, kq * 4:(kq + 1) * 4, jt * 128:(jt + 1) * 128], in_=pT[ti][:])
        c1 = [0]

        def evac_ffn(m, h, outs):
            (pg, pgk), (pu, puk) = outs
            s = c1[0] % 2
            c1[0] += 1
            P.I("scalar", "activation", [pgk], ["tA%d" % s], out=tA[s][:], in_=pg[:], func=AF.Silu)
            P.I("vector", "tensor_tensor", ["tA%d" % s, puk], ["act"], out=act[:, m, h * 512:(h + 1) * 512],
                in0=tA[s][:], in1=pu[:], op=ALU.mult)

        linear(P, ws, pr, [(wg[e], xeT, 16, "xeT"), (wu[e], xeT, 16, "xeT")], 1024, 1024, evac_ffn)

        def evac_dn(m, h, outs, e=e):
            ps, pk = outs[0]
            s = c1[0] % 2
            c1[0] += 1
            P.I("scalar", "copy", [pk], ["ost%d" % s], out=ost[s][:], in_=ps[:])
            P.dma("sync", yeT[e, m * 128:(m + 1) * 128, h * 512:(h + 1) * 512], ost[s][:], reads=["ost%d" % s],
                  key="ost%d" % s)

        linear(P, ws, pr, [(wd[e], act, 8, "act")], D, 1024, evac_dn)
    P.emit()
    return nc


def build_C():
    nc = new_nc()
    x1 = din(nc, "x1", [TPC, D])
    ye = din(nc, "ye", [16 * 1024, D])
    prow = din(nc, "prow", [TPC, 16], I32)
    aff = din(nc, "aff", [TPC, 16])
    x2 = dout(nc, "x2", [TPC, D])
    P = Prog(nc)
    acc = [P.sb("acc%d" % i, [128, D], F32) for i in range(2)]
    prt = [P.sb("prt%d" % i, [128, 16], I32) for i in range(2)]
    aft = [P.sb("aft%d" % i, [128, 16], F32) for i in range(2)]
    gbuf = [P.sb("gb%d" % i, [128, D], F32) for i in range(4)]
    c = 0
    for t in range(TPC // 128):
        s = t % 2
        rs = slice(t * 128, (t + 1) * 128)
        P.dma("sync", acc[s][:], x1[rs, :], writes=["acc%d" % s])
        P.dma("sync", prt[s][:], prow[rs, :], writes=["prt%d" % s])
        P.dma("sync", aft[s][:], aff[rs, :], writes=["aft%d" % s])
        for e in range(16):
            b = c % 4
            c += 1
            P.I("gpsimd", "memset", [], ["gb%d" % b], gbuf[b][:], 0.0)
            P.idma(gbuf[b][:, :], None, ye, IOA(ap=prt[s][:, e:e + 1], axis=0), reads=["prt%d" % s],
                   writes=["gb%d" % b], key="gb%d" % b, bounds=16 * 1024 - 1)
            P.I("vector", "scalar_tensor_tensor", ["gb%d" % b, "aft%d" % s, "acc%d" % s], ["acc%d" % s],
                out=acc[s][:], in0=gbuf[b][:], scalar=aft[s][:, e:e + 1], in1=acc[s][:], op0=ALU.mult, op1=ALU.add)
        P.dma("scalar", x2[rs, :], acc[s][:], reads=["acc%d" % s], key="acc%d" % s)
    P.emit()
    return nc


def consts_E():
    tokid = np.ascontiguousarray(np.repeat(np.arange(L, dtype=np.int32).reshape(128, 64, 1), 2, axis=2))
    tri = (np.arange(128)[:, None] < np.arange(128)[None, :]).astype(np.float32)
    ident = np.eye(128, dtype=np.float32).astype(ml_dtypes.bfloat16)
    return tokid, tri, ident


def build_A():
    nc = new_nc()
    qT = din(nc, "qT", [128, L])
    kT = din(nc, "kT", [128, L])
    vT = din(nc, "vT", [128, L])
    ctab = din(nc, "ctab", [128, L])
    stab = din(nc, "stab", [128, L])
    small = din(nc, "small", [128, 8])
    lamin = din(nc, "lamin", [128, 4, 64])
    pmd = din(nc, "pmd", [128, 128], BF16)
    bdd = din(nc, "bdd", [128, 128], BF16)
    identd = din(nc, "identd", [128, 128], BF16)
    oT = dout(nc, "oT", [128, L])
    P = Prog(nc)
    V, A_, T_ = "vector", "scalar", "tensor"
    sm = P.sb("sm", [128, 8], F32)
    li = P.sb("li", [128, 4, 64], F32)
    pm = P.sb("pm", [128, 128], BF16)
    bd = P.sb("bd", [128, 128], BF16)
    ident = P.sb("ident", [128, 128], BF16)
    ones = P.sb("ones", [128, 128], BF16)
    lt = P.sb("lt", [128, 2, 64], F32)
    ls = P.sb("ls", [128, 8], F32)
    qr = P.sb("qr", [128, L], BF16)
    kr = P.sb("kr", [128, L], BF16)
    Vt = P.sb("Vt", [128, 64, 128], BF16)
    raw = [P.sb("raw%d" % i, [128, 512], F32) for i in range(3)]
    ct = [P.sb("ct%d" % i, [128, 512], F32) for i in range(2)]
    st = [P.sb("st%d" % i, [128, 512], F32) for i in range(2)]
    sqb = [P.sb("sqb%d" % i, [128, 512], BF16) for i in range(2)]
    qn = [P.sb("qn%d" % i, [128, 512], F32) for i in range(2)]
    qnb = [P.sb("qnb%d" % i, [128, 512], BF16) for i in range(2)]
    rs = [P.sb("rs%d" % i, [128, 512], F32) for i in range(2)]
    tmp = P.sb("tmp", [128, 512], F32)
    t1 = [P.sb("t1%d" % i, [128, 512], F32) for i in range(2)]
    t2 = [P.sb("t2%d" % i, [128, 512], F32) for i in range(2)]
    vb = [P.sb("vb%d" % i, [128, 512], BF16) for i in range(2)]
    pt = [P.sb("pt%d" % i, [128, 512], BF16) for i in range(4)]
    ost = [P.sb("ost%d" % i, [128, 512], F32) for i in range(2)]
    sacc = [[P.sb("sacc%d_%d" % (i, j), [128, 512], F32) for j in range(2)] for i in range(2)]
    onesf = P.sb("onesf", [128, 128], F32)
    P.I("vector", "memset", [], ["onesf"], onesf[:], 1.0)
    ring = PsRing(P, 3, "pr")
    acc = [P.ps("acc%d" % i, [128, 512], F32) for i in range(4)]
    pT = P.ps("pT", [128, 4, 128], BF16)
    P.dma("sync", sm[:], small, writes=["sm"])
    P.dma("sync", li[:], lamin, writes=["li"])
    P.dma("sync", pm[:], pmd, writes=["pm"])
    P.dma("sync", bd[:], bdd, writes=["bd"])
    P.dma("sync", ident[:], identd, writes=["ident"])
    P.I(V, "memset", [], ["ones"], ones[:], 1.0)
    for j in range(2):
        P.I(V, "tensor_tensor", ["li"], ["lt"], out=lt[:, j, :], in0=li[:, 2 * j, :], in1=li[:, 2 * j + 1, :],
            op=ALU.mult)
        P.I(V, "reduce_sum", ["lt"], ["ls"], out=ls[:, j:j + 1], in_=lt[:, j, :], axis=AX.X)
        P.I(A_, "activation", ["ls"], ["ls"], out=ls[:, 2 + j:3 + j], in_=ls[:, j:j + 1], func=AF.Exp)
    P.I(V, "tensor_tensor", ["ls"], ["ls"], out=ls[:, 4:5], in0=ls[:, 2:3], in1=ls[:, 3:4], op=ALU.subtract)
    P.I(V, "tensor_tensor", ["ls", "sm"], ["ls"], out=ls[:, 4:5], in0=ls[:, 4:5], in1=sm[:, 3:4], op=ALU.add)
    P.I(V, "tensor_scalar", ["ls"], ["ls"], out=ls[:, 5:6], in0=ls[:, 4:5], scalar1=-1.0, scalar2=None,
        op0=ALU.mult)
    P.I(V, "tensor_tensor", ["sm"], ["ls"], out=ls[:, 6:7], in0=sm[:, 2:3], in1=sm[:, 4:5], op=ALU.mult)
    P.I(V, "tensor_scalar", ["sm"], ["ls"], out=ls[:, 7:8], in0=sm[:, 0:1], scalar1=0.125, scalar2=None,
        op0=ALU.mult)
    cc = 0
    for tcx in range(L // 512):
        sl = slice(tcx * 512, (tcx + 1) * 512)
        s2 = tcx % 2
        P.dma("sync", ct[s2][:], ctab[:, sl], writes=["ct%d" % s2])
        P.dma("sync", st[s2][:], stab[:, sl], writes=["st%d" % s2])
        for (src, gap, gk, dst, dk) in ((qT, ls[:, 7:8], "ls", qr, "qr"), (kT, sm[:, 1:2], "sm", kr, "kr")):
            s = cc % 2
            r3 = cc % 3
            cc += 1
            P.dma("sync", raw[r3][:], src[:, sl], writes=["raw%d" % r3])
            P.I(A_, "activation", ["raw%d" % r3], ["sqb%d" % s], out=sqb[s][:], in_=raw[r3][:], func=AF.Square)
            ps, pk = ring.next()
            P.I(T_, "matmul", ["bd", "sqb%d" % s], [pk], ps[:], lhsT=bd[:], rhs=sqb[s][:], start=True, stop=True)
            rstd_from_ps(P, ps[:], pk, rs[s][:], "rs%d" % s, tmp[:], "tmp", 1.0 / 64)
            P.I(V, "scalar_tensor_tensor", ["raw%d" % r3, gk, "rs%d" % s], ["qn%d" % s], out=qn[s][:],
                in0=raw[r3][:], scalar=gap, in1=rs[s][:], op0=ALU.mult, op1=ALU.mult)
            P.I(A_, "activation", ["qn%d" % s], ["qnb%d" % s], out=qnb[s][:], in_=qn[s][:], func=AF.Copy)
            ps2, pk2 = ring.next()
            P.I(T_, "matmul", ["pm", "qnb%d" % s], [pk2], ps2[:], lhsT=pm[:], rhs=qnb[s][:], start=True, stop=True)
            P.I(V, "tensor_tensor", [pk2, "st%d" % s2], ["t1%d" % s], out=t1[s][:], in0=ps2[:], in1=st[s2][:],
                op=ALU.mult)
            P.I("gpsimd", "tensor_tensor", ["qn%d" % s, "ct%d" % s2], ["t2%d" % s], out=t2[s][:], in0=qn[s][:],
                in1=ct[s2][:], op=ALU.mult)
            P.I(V, "tensor_tensor", ["t1%d" % s, "t2%d" % s], [dk], out=dst[:, sl], in0=t1[s][:], in1=t2[s][:],
                op=ALU.add)
        r3 = cc % 3
        cc += 1
        P.dma("sync", raw[r3][:], vT[:, sl], writes=["raw%d" % r3])
        P.I("gpsimd", "tensor_copy", ["raw%d" % r3], ["vb%d" % s2], out=vb[s2][:], in_=raw[r3][:])
        for j in range(4):
            P.I(T_, "transpose", ["vb%d" % s2, "ident"], ["pT"], out=pT[:, j, :], in_=vb[s2][:, j * 128:(j + 1) * 128],
                identity=ident[:])
        P.I(A_, "copy", ["pT"], ["Vt"], out=Vt[:, tcx * 4:(tcx + 1) * 4, :], in_=pT[:])
    steps = [(qc, c, kt) for qc in range(L // 512) for c in range(2) for kt in range(64)]
    ns = len(steps)
    sps = [None] * ns

    def emit_S(i):
        qc, c, kt = steps[i]
        ps, pk = ring.next()
        P.I(T_, "matmul", ["kr", "qr"], [pk], ps[:], lhsT=kr[64 * c:64 * c + 64, kt * 128:(kt + 1) * 128],
            rhs=qr[64 * c:64 * c + 64, qc * 512:(qc + 1) * 512], start=True, stop=True)
        sps[i] = (ps, pk)

    def emit_rest(i):
        qc, c, kt = steps[i]
        ps, pk = sps[i]
        s = i % 4
        P.I(A_, "activation", [pk], ["pt%d" % s], out=pt[s][:], in_=ps[:], func=AF.Exp)
        P.I(T_, "matmul", ["Vt", "pt%d" % s], ["acc%d" % c], acc[c][:], lhsT=Vt[:, kt, :], rhs=pt[s][:],
            start=(kt == 0), stop=(kt == 63))
        eg = kt % 2
        en_ = V if eg == 0 else "gpsimd"
        sk_ = "sacc%d_%d" % (c, eg)
        if kt < 2:
            P.I(en_, "tensor_copy", ["pt%d" % s], [sk_], out=sacc[c][eg][:], in_=pt[s][:])
        else:
            P.I(en_, "tensor_tensor", ["pt%d" % s, sk_], [sk_], out=sacc[c][eg][:], in0=sacc[c][eg][:], in1=pt[s][:],
                op=ALU.add)
        if kt == 63:
            for eg2 in range(2):
                P.I(T_, "matmul", ["onesf", "sacc%d_%d" % (c, eg2)], ["acc%d" % (2 + c)], acc[2 + c][:], lhsT=onesf[:],
                    rhs=sacc[c][eg2][:], start=(eg2 == 0), stop=(eg2 == 1))
        if c == 1 and kt == 63:
            sl = slice(qc * 512, (qc + 1) * 512)
            e = qc % 2
            for c2 in range(2):
                P.I(V, "reciprocal", ["acc%d" % (2 + c2)], ["rs%d" % c2], out=rs[c2][:], in_=acc[2 + c2][:])
                P.I(V, "tensor_tensor", ["acc%d" % c2, "rs%d" % c2], ["t1%d" % c2], out=t1[c2][:], in0=acc[c2][:],
                    in1=rs[c2][:], op=ALU.mult)
            P.I(V, "scalar_tensor_tensor", ["t11", "ls", "t10"], ["qn%d" % e], out=qn[e][:], in0=t1[1][:],
                scalar=ls[:, 5:6], in1=t1[0][:], op0=ALU.mult, op1=ALU.add)
            P.I("gpsimd", "tensor_tensor", ["qn%d" % e], ["sqb%d" % e], out=sqb[e][:], in0=qn[e][:], in1=qn[e][:],
                op=ALU.mult)
            ps3, pk3 = acc[2], "acc2"
            P.I(T_, "matmul", ["ones", "sqb%d" % e], [pk3], ps3[:], lhsT=ones[:], rhs=sqb[e][:], start=True, stop=True)
            rstd_from_ps(P, ps3[:], pk3, t2[e][:], "t2%d" % e, tmp[:], "tmp", 1.0 / 128)
            P.I(V, "scalar_tensor_tensor", ["qn%d" % e, "ls", "t2%d" % e], ["ost%d" % e], out=ost[e][:],
                in0=qn[e][:], scalar=ls[:, 6:7], in1=t2[e][:], op0=ALU.mult, op1=ALU.mult)
            P.dma("sync", oT[:, sl], ost[e][:], reads=["ost%d" % e], key="ost%d" % e)

    LOOK = 2
    for i in range(min(LOOK, ns)):
        emit_S(i)
    for i in range(ns):
        if i + LOOK < ns:
            emit_S(i + LOOK)
        emit_rest(i)
    P.emit()
    return nc


def consts_A():
    pos = np.arange(L, dtype=np.float64)
    inv = np.power(500000.0, -np.arange(0, 16, 2, dtype=np.float64) / 16)
    ang = (pos[:, None].astype(np.float32) * inv[None, :].astype(np.float32)).astype(np.float32).astype(np.float64)
    cos, sin = np.cos(ang).T, np.sin(ang).T
    ctab = np.ones((128, L), np.float32)
    stab = np.zeros((128, L), np.float32)
    pm = np.zeros((128, 128), np.float32)
    for half in range(2):
        b = half * 64
        ctab[b:b + 8] = cos
        ctab[b + 8:b + 16] = cos
        stab[b:b + 8] = -sin
        stab[b + 8:b + 16] = sin
        for i in range(8):
            pm[b + 8 + i, b + i] = 1.0
            pm[b + i, b + 8 + i] = 1.0
    bd = np.zeros((128, 128), np.float32)
    bd[:64, :64] = 1
    bd[64:, 64:] = 1
    bf = ml_dtypes.bfloat16
    return ctab, stab, pm.astype(bf), bd.astype(bf), np.eye(128, dtype=np.float32).astype(bf)


PI2 = 2.0 * math.pi
PI_SAFE = 3.141592


def build_S():
    nc = new_nc()
    U8 = din(nc, "U8", [8, 128, 1024])
    bT = din(nc, "bT", [128, 2, 1024])
    aB = din(nc, "aB", [128, 3, 1024])
    epart = din(nc, "epart", [128, 2])
    cP = din(nc, "cP", [128, 4, 16, 16])
    aC = din(nc, "aC", [128, 3, 16])
    ktab = din(nc, "ktab", [128, 16])
    seld = din(nc, "sel", [128, 3])
    dskd = din(nc, "dsk", [128, 8])
    matsd = din(nc, "mats", [128, 3, 128])
    cidxd = din(nc, "cidx", [128, 1024])
    y8 = dout(nc, "y8", [8, 128, 1024])
    P = Prog(nc)
    V, A_, T_, G_ = "vector", "scalar", "tensor", "gpsimd"
    PR = ["prep"]

    def tt(out, a, b, op, r=PR, w=PR, eng=V):
        P.I(eng, "tensor_tensor", r, w, out=out, in0=a, in1=b, op=op)

    def ts(out, a, s1, op0, s2=None, op1=None, r=PR, w=PR):
        kw = dict(out=out, in0=a, scalar1=s1, scalar2=s2, op0=op0)
        if op1 is not None:
            kw["op1"] = op1
        P.I(V, "tensor_scalar", r, w, **kw)

    def stt(out, a, s, b, op0, op1, r=PR, w=PR):
        P.I(V, "scalar_tensor_tensor", r, w, out=out, in0=a, scalar=s, in1=b, op0=op0, op1=op1)

    def act(out, a, func, r=PR, w=PR, **kw):
        P.I(A_, "activation", r, w, out=out, in_=a, func=func, **kw)

    G = [P.sb("G%d" % i, [128, 1024], F32) for i in range(20)]
    tki = P.sb("tki", [128, 1024], I32)
    sbT = P.sb("sbT", [128, 2, 1024], F32)
    saB = P.sb("saB", [128, 3, 1024], F32)
    sep = P.sb("sep", [128, 2], F32)
    scP = P.sb("scP", [128, 4, 16, 16], F32)
    saC = P.sb("saC", [128, 3, 16], F32)
    skt = P.sb("skt", [128, 16], F32)
    sel = P.sb("sel_s", [128, 3], F32)
    dsk = P.sb("dsk_s", [128, 8], F32)
    mats = P.sb("mats_s", [128, 3, 128], F32)
    cidx = P.sb("cidx_s", [128, 1024], F32)
    c16 = [P.sb("c16_%d" % i, [128, 16], F32) for i in range(12)]
    Lre = P.sb("Lre", [128, 16, 16], F32)
    Lim = P.sb("Lim", [128, 16, 16], F32)
    Cmat = P.sb("Cmat", [128, 2, 8, 128], BF16)
    Kc = P.sb("Kc", [128, 8, 128], BF16)
    W1 = P.sb("W1", [128, 16, 128], BF16)
    W2 = P.sb("W2", [128, 16, 128], BF16)
    thr = P.sb("thr", [128, 16], F32)
    r8 = P.sb("r8", [128, 16], F32)
    ustg = [P.sb("ustg%d" % i, [128, 1024], F32) for i in range(2)]
    U8b = [P.sb("U8b%d" % i, [128, 1024], BF16) for i in range(2)]
    Xin = [P.sb("Xin%d" % i, [128, 1024], BF16) for i in range(4)]
    ost = [P.sb("ost%d" % i, [128, 512], F32) for i in range(2)]
    ring = PsRing(P, 4, "pr")
    pk_ = [P.ps("pkk%d" % i, [128, 128], F32) for i in range(2)]
    pso = [P.ps("pso%d" % i, [128, 512], F32) for i in range(2)]
    for (t, src) in ((sbT, bT), (saB, aB), (sep, epart), (scP, cP), (saC, aC), (skt, ktab), (sel, seld), (dsk, dskd),
                     (mats, matsd), (cidx, cidxd)):
        P.dma("sync", t[:], src, writes=PR)

    def sincos(ang, sin_out, cos_out, n, r=PR, w=PR, ta=None, tkf=None):
        ta = ta[:, :n]
        tkf = tkf[:, :n]
        for shift, outp in ((0.0, sin_out), (math.pi / 2, cos_out)):
            ts(ta, ang, shift, ALU.add, r=r, w=w)
            ts(tki[:, :n], ta, 1.0 / PI2, ALU.mult, r=r, w=w)
            P.I(V, "tensor_copy", r, w, out=tkf, in_=tki[:, :n])
            stt(ta, tkf, -PI2, ta, ALU.mult, ALU.add, r=r, w=w)
            ts(ta, ta, -PI_SAFE, ALU.max, s2=PI_SAFE, op1=ALU.min, r=r, w=w)
            act(outp, ta, AF.Sin, r=r, w=w)

    stepC, drC, diC, nrC, denC, creC, cimC, t16a, t16b = [c[:] for c in c16[:9]]
    act(stepC, saC[:, 2, :], AF.Exp)
    tt(drC, saC[:, 0, :], stepC, ALU.mult)
    tt(diC, saC[:, 1, :], stepC, ALU.mult)
    g3 = lambda i: G[i][:, 0:256].rearrange("p (a b) -> p a b", a=16)
    argm, angk, magk, sk, ck = g3(0), g3(1), g3(2), g3(3), g3(4)
    kb = skt[:].unsqueeze(1).to_broadcast([128, 16, 16])
    tt(argm, drC.unsqueeze(2).to_broadcast([128, 16, 16]), kb, ALU.mult)
    tt(angk, diC.unsqueeze(2).to_broadcast([128, 16, 16]), kb, ALU.mult)
    act(G[2][:, 0:256], G[0][:, 0:256], AF.Exp)
    sincos(G[1][:, 0:256], G[3][:, 0:256], G[4][:, 0:256], 256, ta=G[5], tkf=G[6])
    tt(Lre[:], magk, ck, ALU.mult)
    tt(Lim[:], magk, sk, ALU.mult)
    ts(t16a, diC, 8.0, ALU.mult)
    ts(tki[:, 0:16], t16a, 1.0 / PI2, ALU.mult)
    P.I(V, "tensor_copy", PR, PR, out=t16b, in_=tki[:, 0:16])
    stt(thr[:], t16b, -PI2, t16a, ALU.mult, ALU.add)
    act(r8[:], drC, AF.Exp, scale=8.0)

    def coef(lbr, lbi, are, aim, cre, cim, nr, den, tmp):
        ts(nr, lbr, -1.0, ALU.add)
        tt(den, are, are, ALU.mult)
        tt(tmp, aim, aim, ALU.mult)
        tt(den, den, tmp, ALU.add)
        P.I(V, "reciprocal", PR, PR, out=den, in_=den)
        tt(cre, nr, are, ALU.mult)
        tt(tmp, lbi, aim, ALU.mult)
        tt(cre, cre, tmp, ALU.add)
        tt(cre, cre, den, ALU.mult)
        tt(cim, lbi, are, ALU.mult)
        tt(tmp, nr, aim, ALU.mult)
        tt(cim, cim, tmp, ALU.subtract)
        tt(cim, cim, den, ALU.mult)

    coef(Lre[:, :, 8], Lim[:, :, 8], saC[:, 0, :], saC[:, 1, :], creC, cimC, nrC, denC, t16a)
    Bbre, Bbim, tB = g3(0), g3(1), g3(2)
    crb = creC.unsqueeze(2).to_broadcast([128, 16, 16])
    cib = cimC.unsqueeze(2).to_broadcast([128, 16, 16])
    tt(Bbre, scP[:, 2], crb, ALU.mult)
    tt(tB, scP[:, 3], cib, ALU.mult)
    tt(Bbre, Bbre, tB, ALU.subtract)
    tt(Bbim, scP[:, 3], crb, ALU.mult)
    tt(tB, scP[:, 2], cib, ALU.mult)
    tt(Bbim, Bbim, tB, ALU.add)
    g4 = lambda i: G[i][:].rearrange("p (g i h) -> p g i h", g=8, i=8)
    QC = [G[8], G[9]]
    PB = [G[10], G[11]]

    def ksl(start, rev):
        return slice(start, start - 8 if start - 8 >= 0 else None, -1) if rev else slice(start, start + 8)

    def cmul_table(Are, Aim, d, ks, out, sB, rA, iA, t1, t2):
        dgs = slice(d * 8, d * 8 + 8)
        Lr = Lre[:, dgs, ks].unsqueeze(3).to_broadcast([128, 8, 8, 16])
        Li = Lim[:, dgs, ks].unsqueeze(3).to_broadcast([128, 8, 8, 16])
        Ar = Are[:, dgs, :].unsqueeze(2).to_broadcast([128, 8, 8, 16])
        Ai = Aim[:, dgs, :].unsqueeze(2).to_broadcast([128, 8, 8, 16])
        tt(rA, Ar, Lr, ALU.mult)
        tt(t1, Ai, Li, ALU.mult)
        tt(rA, rA, t1, ALU.subtract)
        tt(iA, Ar, Li, ALU.mult)
        tt(t2, Ai, Lr, ALU.mult)
        tt(iA, iA, t2, ALU.add)
        ts(rA, rA, sel[:, 0:1], ALU.mult)
        stt(out, iA, sB, rA, ALU.mult, ALU.add)

    for d in range(2):
        rev = (d == 1)
        cmul_table(scP[:, 0], scP[:, 1], d, ksl(15, True) if rev else ksl(8, False),
                   Cmat[:, d].rearrange("p g (i h) -> p g i h", i=8), sel[:, 1:2], g4(12), g4(13), g4(14), g4(15))
        cmul_table(scP[:, 0], scP[:, 1], d, ksl(7, True) if rev else ksl(7, False), g4(8 + d), sel[:, 1:2],
                   g4(12), g4(13), g4(14), g4(15))
        cmul_table(Bbre, Bbim, d, ksl(7, False) if rev else ksl(7, True), g4(10 + d), sel[:, 2:3],
                   g4(12), g4(13), g4(14), g4(15))
    k1 = G[12][:, 0:128]
    k2 = G[13][:, 0:128]
    k3 = G[14][:, 0:128]
    for g in range(8):
        for d in range(2):
            P.I(T_, "matmul", PR, ["pkk%d" % d], pk_[d][:], lhsT=PB[d][:, g * 128:(g + 1) * 128],
                rhs=QC[d][:, g * 128:(g + 1) * 128], start=True, stop=True)
        tt(k1, pk_[0][:], mats[:, 1, :], ALU.mult, r=PR + ["pkk0"])
        tt(k2, pk_[1][:], mats[:, 2, :], ALU.mult, r=PR + ["pkk1"])
        ts(k3, mats[:, 0, :], dsk[:, g:g + 1], ALU.mult)
        tt(k1, k1, k2, ALU.add)
        tt(Kc[:, g, :], k1, k3, ALU.add)
    stepB, drB, diB, mag1, s1, c1, nrB, denB, creB, cimB, tmpB = [G[i][:] for i in range(11)]
    act(stepB, saB[:, 2, :], AF.Exp)
    tt(drB, saB[:, 0, :], stepB, ALU.mult)
    tt(diB, saB[:, 1, :], stepB, ALU.mult)
    act(mag1, drB, AF.Exp)
    sincos(diB, s1, c1, 1024, ta=G[11], tkf=G[12])
    tt(c1, mag1, c1, ALU.mult)
    tt(s1, mag1, s1, ALU.mult)
    coef(c1, s1, saB[:, 0, :], saB[:, 1, :], creB, cimB, nrB, denB, tmpB)
    for d in range(2):
        hs = slice(d * 512, (d + 1) * 512)
        mage, ange, se, ce, Ere, Eim, t5, w1r, w1i = [G[i][:, 0:512] for i in range(11, 20)]
        act(mage, drB[:, hs], AF.Exp, scale=sep[:, d:d + 1])
        ts(ange, diB[:, hs], sep[:, d:d + 1], ALU.mult)
        sincos(ange, se, ce, 512, ta=G[0], tkf=G[3])
        tt(ce, mage, ce, ALU.mult)
        tt(se, mage, se, ALU.mult)
        tt(Ere, ce, creB[:, hs], ALU.mult)
        tt(t5, se, cimB[:, hs], ALU.mult)
        tt(Ere, Ere, t5, ALU.subtract)
        tt(Eim, ce, cimB[:, hs], ALU.mult)
        tt(t5, se, creB[:, hs], ALU.mult)
        tt(Eim, Eim, t5, ALU.add)
        tt(w1r, Ere, sbT[:, 0, hs], ALU.mult)
        tt(t5, Eim, sbT[:, 1, hs], ALU.mult)
        tt(w1r, w1r, t5, ALU.subtract)
        tt(w1i, Ere, sbT[:, 1, hs], ALU.mult)
        tt(t5, Eim, sbT[:, 0, hs], ALU.mult)
        tt(w1i, w1i, t5, ALU.add)
        dgs = slice(d * 8, d * 8 + 8)
        w1r3 = w1r.rearrange("p (g q) -> p g q", g=8)
        w1i3 = w1i.rearrange("p (g q) -> p g q", g=8)
        P.I(V, "tensor_copy", PR, PR, out=W1[:, dgs, 0:64], in_=w1r3)
        P.I(V, "tensor_copy", PR, PR, out=W1[:, dgs, 64:128], in_=w1i3)
        P.I(V, "tensor_copy", PR, PR, out=W2[:, dgs, 0:64], in_=w1i3)
        ts(W2[:, dgs, 64:128], w1r3, -1.0, ALU.mult)
    MK = ["tabA", "tabB", "S1", "S2", "Z1", "Z2", "ta", "tb", "ta2", "tb2", "sc"]
    P.alias(MK, PR)
    tabs = [(G[0], G[1], G[2]), (G[3], G[4], G[5])]
    S1, S2, Z1, Z2 = G[6], G[7], G[8], G[9]
    tA, tBb, tA2, tB2 = G[10], G[11], G[12], G[13]
    sc_ta, sc_tkf = G[14], G[15]
    it = 0
    for g in range(8):
        us = g % 2
        P.dma("sync", ustg[us][:], U8[g], writes=["ustg%d" % us])
        P.I(G_, "tensor_copy", ["ustg%d" % us], ["U8b%d" % us], out=U8b[us][:], in_=ustg[us][:])
        for d in range(2):
            dg = d * 8 + g
            tb = it % 2
            it += 1
            tk = ["tabA", "tabB"][tb]
            sinT, cosT, rT = tabs[tb]
            xs = (g % 2) * 2 + d
            xk = "Xin%d" % xs
            ts(sc_ta[:], cidx[:], thr[:, dg:dg + 1], ALU.mult, r=PR, w=["sc"])
            sincos(sc_ta[:], sinT[:], cosT[:], 1024, r=["sc", tk], w=["sc", tk], ta=G[16], tkf=G[17])
            ts(rT[:], cidx[:], 0.0, ALU.mult, s2=r8[:, dg:dg + 1], op1=ALU.add, r=PR + [tk], w=[tk])
            op_a, op_b = (ALU.add, ALU.subtract) if d == 0 else (ALU.subtract, ALU.add)
            for h in range(2):
                hs = slice(h * 512, (h + 1) * 512)
                o1, o1k = ring.next()
                o2, o2k = ring.next()
                P.I(T_, "matmul", PR + ["U8b%d" % us], [o1k], o1[:], lhsT=W1[:, dg, :], rhs=U8b[us][:, hs],
                    start=True, stop=True)
                P.I(T_, "matmul", PR + ["U8b%d" % us], [o2k], o2[:], lhsT=W2[:, dg, :], rhs=U8b[us][:, hs],
                    start=True, stop=True)
                tt(tA[:, hs], cosT[:, hs], o1[:], ALU.mult, r=[tk, o1k], w=["ta"])
                tt(tBb[:, hs], sinT[:, hs], o2[:], ALU.mult, r=[tk, o2k], w=["tb"])
                tt(S1[:, hs], tA[:, hs], tBb[:, hs], op_a, r=["ta", "tb"], w=["S1"], eng=G_)
                tt(tA2[:, hs], cosT[:, hs], o2[:], ALU.mult, r=[tk, o2k], w=["ta2"])
                tt(tB2[:, hs], sinT[:, hs], o1[:], ALU.mult, r=[tk, o1k], w=["tb2"])
                tt(S2[:, hs], tA2[:, hs], tB2[:, hs], op_b, r=["ta2", "tb2"], w=["S2"], eng=G_)
            rv = slice(None, None, -1) if d == 1 else slice(None)
            P.I(V, "tensor_tensor_scan", ["S1", tk], ["Z1"], out=Z1[:, rv], data0=rT[:, rv], data1=S1[:, rv],
                initial=0.0, op0=ALU.mult, op1=ALU.add)
            P.I(V, "tensor_tensor_scan", ["S2", tk], ["Z2"], out=Z2[:, rv], data0=rT[:, rv], data1=S2[:, rv],
                initial=0.0, op0=ALU.mult, op1=ALU.add)
            tt(tA[:], cosT[:], Z1[:], ALU.mult, r=[tk, "Z1"], w=["ta"])
            tt(tBb[:], sinT[:], Z2[:], ALU.mult, r=[tk, "Z2"], w=["tb"], eng=G_)
            if d == 0:
                P.I(V, "memset", [], [xk], Xin[xs][:, 0:1], 0.0)
                tt(Xin[xs][:, 1:1024], tA[:, 0:1023], tBb[:, 0:1023], ALU.subtract, r=["ta", "tb"], w=[xk])
            else:
                P.I(V, "memset", [], [xk], Xin[xs][:, 1023:1024], 0.0)
                tt(Xin[xs][:, 0:1023], tA[:, 1:1024], tBb[:, 1:1024], ALU.add, r=["ta", "tb"], w=[xk])
        for h in range(2):
            hs = slice(h * 512, (h + 1) * 512)
            x0 = (g % 2) * 2
            P.I(T_, "matmul", PR + ["U8b%d" % us], ["pso%d" % h], pso[h][:], lhsT=Kc[:, g, :], rhs=U8b[us][:, hs],
                start=True, stop=False)
            P.I(T_, "matmul", PR + ["Xin%d" % x0], ["pso%d" % h], pso[h][:], lhsT=Cmat[:, 0, g, :],
                rhs=Xin[x0][:, hs], start=False, stop=False)
            P.I(T_, "matmul", PR + ["Xin%d" % (x0 + 1)], ["pso%d" % h], pso[h][:], lhsT=Cmat[:, 1, g, :],
                rhs=Xin[x0 + 1][:, hs], start=False, stop=True)
            P.I(A_, "copy", ["pso%d" % h], ["ost%d" % h], out=ost[h][:], in_=pso[h][:])
            P.dma("sync", y8[g, :, hs], ost[h][:], reads=["ost%d" % h], key="ost%d" % h)
    P.emit()
    return nc


def consts_S():
    q = np.arange(128)
    epart = np.stack([7 - q // 16, q // 16], 1).astype(np.float32)
    ktab = np.tile(np.arange(-7, 9, dtype=np.float32)[None], (128, 1))
    sel = np.stack([(q < 64), -(q >= 64).astype(np.float32), (q >= 64)], 1).astype(np.float32)
    ii = q // 16
    ident = np.eye(128, dtype=np.float32)
    maskF = (ii[None, :] >= ii[:, None]).astype(np.float32)
    maskB = (ii[:, None] >= ii[None, :]).astype(np.float32)
    mats = np.ascontiguousarray(np.stack([ident, maskF, maskB], 1))
    cidx = np.tile(np.arange(1024, dtype=np.float32)[None], (128, 1))
    return epart, ktab, sel, mats, cidx


def prep_S(inp, l, c, uT_core):
    gs = slice(8 * c, 8 * c + 8)
    epart, ktab, sel, mats, cidx = consts_S()
    b_re, b_im = inp["ssm_b_re"][l][:, gs], inp["ssm_b_im"][l][:, gs]
    c_re, c_im = inp["ssm_c_re"][l][:, gs], inp["ssm_c_im"][l][:, gs]
    a_re, a_im, ls = inp["ssm_a_re"][l][:, gs], inp["ssm_a_im"][l][:, gs], inp["ssm_log_step"][l][:, gs]

    def bside(b):
        t = b.transpose(3, 0, 1, 2).reshape(16, 1024)
        return np.tile(t, (8, 1))
    bT = np.stack([bside(b_re), bside(b_im)], 1)
    flat = lambda a: np.broadcast_to(a.reshape(1, 1024), (128, 1024))
    aB = np.stack([flat(a_re), flat(a_im), flat(np.broadcast_to(ls[:, :, None], (2, 8, 64)))], 1)

    def cside_c(cc):
        t = cc.transpose(3, 0, 1, 2).reshape(64, 16, 16)
        return np.concatenate([t, t], 0)

    def cside_b(b):
        t = b.transpose(2, 0, 1, 3).reshape(64, 16, 16)
        return np.concatenate([t, t], 0)
    cP = np.stack([cside_c(c_re), cside_c(c_im), cside_b(b_re), cside_b(b_im)], 1)

    def cside_a(a):
        t = a.transpose(2, 0, 1).reshape(64, 16)
        return np.concatenate([t, t], 0)
    aC = np.stack([cside_a(a_re), cside_a(a_im), cside_a(np.broadcast_to(ls[:, :, None], (2, 8, 64)))], 1)
    dsk = np.tile(inp["ssm_d"][l][128 * c:128 * (c + 1)].reshape(8, 16).T, (8, 1))
    U8 = uT_core.reshape(8, 16, 1024, 8).transpose(0, 3, 1, 2).reshape(8, 128, 1024)
    f = lambda a: np.ascontiguousarray(a, dtype=np.float32)
    return dict(U8=f(U8), bT=f(bT), aB=f(aB), epart=f(epart), cP=f(cP), aC=f(aC), ktab=f(ktab), sel=f(sel),
                dsk=f(dsk), mats=f(mats), cidx=f(cidx))


def unpack_y8(y8):
    return np.ascontiguousarray(y8.reshape(8, 8, 16, 1024).transpose(0, 2, 3, 1).reshape(128, L))


_PROGS = {}


def _prog(name):
    if name not in _PROGS:
        _PROGS[name] = dict(P=build_P, S=build_S, A=build_A, M=build_M, E=build_E, C=build_C)[name]()
    return _PROGS[name]


def _run(name, maps):
    res = run_bass_kernel_spmd(_prog(name), maps, core_ids=list(range(NCORES)))
    return res.results


def _c(a, dt=None):
    return np.ascontiguousarray(a, dtype=dt)


def kernel(**inp):
    inp = {k: np.asarray(v) for k, v in inp.items()}
    x = _c(inp["x"][0], np.float32)
    ctab, stab, pm, bd, ident = consts_A()
    tokid, tri, _ = consts_E()
    for l in range(2):
        xT = _c(x.T)
        cs = lambda a, c: _c(a[:, c * TPC:(c + 1) * TPC])
        g = _c(inp["mix_norm_g"][l].reshape(16, 128).T)
        W = _c(inp["w_in"][l])
        r = _run("P", [dict(xT=cs(xT, c), g=g, W=W) for c in range(NCORES)])
        projT = np.concatenate([q["projT"] for q in r], axis=1)
        r = _run("S", [prep_S(inp, l, c, projT[128 * c:128 * (c + 1)]) for c in range(NCORES)])
        ysT = np.concatenate([unpack_y8(q["y8"]) for q in r], axis=0)
        lam_init = 0.8 - 0.6 * math.exp(-0.3 * l)
        small = np.zeros((128, 8), np.float32)
        small[:, 0] = np.tile(inp["q_norm_g"][l], 2)
        small[:, 1] = np.tile(inp["k_norm_g"][l], 2)
        small[:, 2] = inp["subln_g"][l]
        small[:, 3] = lam_init
        small[:, 4] = 1.0 - lam_init
        lamin = _c(np.broadcast_to(np.stack([inp["lambda_q1"][l], inp["lambda_k1"][l], inp["lambda_q2"][l],
                                             inp["lambda_k2"][l]])[None], (128, 4, 64)), np.float32)
        r = _run("A", [dict(qT=_c(projT[1024 + h * 128:1024 + (h + 1) * 128]),
                            kT=_c(projT[2048 + h * 128:2048 + (h + 1) * 128]),
                            vT=_c(projT[3072 + h * 128:3072 + (h + 1) * 128]), ctab=ctab, stab=stab, small=small,
                            lamin=lamin, pmd=pm, bdd=bd, identd=ident) for h in range(NCORES)])
        yaT = np.concatenate([q["oT"] for q in r], axis=0)
        glT = projT[4096:8192]
        gate_b = _c(inp["gate_b"][l].reshape(2, 16, 128).transpose(2, 0, 1).reshape(128, 32))
        gffn = _c(inp["ffn_norm_g"][l].reshape(16, 128).T)
        w_r = _c(inp["w_router"][l].reshape(16, 128, 16).transpose(1, 0, 2))
        r = _run("M", [dict(ysT=cs(ysT, c), yaT=cs(yaT, c), glT=cs(glT, c), xT=cs(xT, c), w_glu=_c(inp["w_glu"][l]),
                            w_brs=_c(inp["w_br_ssm"][l]), w_bra=_c(inp["w_br_attn"][l]), w_out=_c(inp["w_out"][l]),
                            gate_b=gate_b, gffn=gffn, w_r=w_r) for c in range(NCORES)])
        x1 = _c(np.concatenate([q["x1T"] for q in r], axis=1).T)
        xn = _c(np.concatenate([q["xnT"] for q in r], axis=1).T)
        aff = _c(np.concatenate([q["affT"] for q in r], axis=1).T)
        maps = []
        for c in range(NCORES):
            affc = _c(aff[:, 2 * c:2 * c + 2].T.reshape(2, 128, 64))
            ebase = _c(np.tile(np.array([[2 * c * 1024, (2 * c + 1) * 1024]], np.float32), (128, 1)))
            maps.append(dict(affc=affc, xn=xn, wg=_c(inp["w_e_gate"][l][2 * c:2 * c + 2]),
                             wu=_c(inp["w_e_up"][l][2 * c:2 * c + 2]), wd=_c(inp["w_e_down"][l][2 * c:2 * c + 2]),
                             tokid=tokid, tri=tri, ebase=ebase, identd=ident))
        r = _run("E", maps)
        yeT = np.stack([q["yeT"] for q in r]).reshape(16, D, 1024)
        posrow = np.stack([q["posrow"] for q in r]).reshape(16, L)
        ye = _c(yeT.reshape(16, D, 8, 128).transpose(0, 3, 2, 1).reshape(16 * 1024, D))
        prow = _c(posrow.T)
        rs = lambda a, c: _c(a[c * TPC:(c + 1) * TPC])
        r = _run("C", [dict(x1=rs(x1, c), ye=ye, prow=rs(prow, c), aff=rs(aff, c)) for c in range(NCORES)])
        x = np.concatenate([q["x2"] for q in r], axis=0)
    return x[None].astype(np.float32)
```

```python
import contextlib
import math
import numpy as np
import ml_dtypes
import concourse.bass as bass
import concourse.mybir as mybir
from concourse.bass_utils import run_bass_kernel_spmd

F32 = mybir.dt.float32
BF16 = mybir.dt.bfloat16
I32 = mybir.dt.int32
ALU = mybir.AluOpType
AF = mybir.ActivationFunctionType
AX = mybir.AxisListType
IOA = bass.IndirectOffsetOnAxis

NCORES = 8
L = 8192
D = 2048
TPC = L // NCORES
EPS = 1e-6
COMPUTE = ("tensor", "vector", "scalar", "gpsimd")
BIG = 1.0e6


class Prog:
    def __init__(self, nc, same_engine_sync=True):
        self.nc = nc
        self.ops = []
        self.last_w = {}
        self.readers = {}
        self.stack = contextlib.ExitStack()
        self.same_engine_sync = same_engine_sync

    def sb(self, name, shape, dtype):
        return self.stack.enter_context(self.nc.sbuf_tensor(name, list(shape), dtype))

    def ps(self, name, shape, dtype=F32):
        return self.stack.enter_context(self.nc.psum_tensor(name, list(shape), dtype))

    def op(self, eng, fn, reads=(), writes=(), dma_key=None):
        idx = len(self.ops)
        deps = set()
        for k in reads:
            if k in self.last_w:
                deps.add(self.last_w[k])
        for k in writes:
            if k in self.last_w:
                deps.add(self.last_w[k])
            deps.update(self.readers.get(k, ()))
        for k in reads:
            self.readers.setdefault(k, []).append(idx)
        for k in writes:
            self.last_w[k] = idx
            self.readers[k] = []
        self.ops.append(dict(eng=eng, fn=fn, deps=deps, dma_key=dma_key, rw=(tuple(reads), tuple(writes))))
        return idx

    def alias(self, new_keys, old_keys):
        acc = []
        for k in old_keys:
            if k in self.last_w:
                acc.append(self.last_w[k])
            acc.extend(self.readers.get(k, ()))
        for k in new_keys:
            self.readers.setdefault(k, []).extend(acc)

    def I(self, eng, meth, reads, writes, *a, **kw):
        return self.op(eng, lambda e: getattr(e, meth)(*a, **kw), reads=reads, writes=writes)

    def dma(self, eng, out, in_, reads=(), writes=(), key=None, **kw):
        if key is None:
            key = writes[0] if writes else reads[0]
        return self.op(eng, lambda e: e.dma_start(out=out, in_=in_, **kw),
                       reads=reads, writes=writes, dma_key=("dma", key))

    def idma(self, out, out_off, in_, in_off, reads, writes, key, bounds=None):
        regs = self.__dict__.setdefault("_bregs", {})

        def fn(e):
            if bounds is None:
                return e.indirect_dma_start(out=out, out_offset=out_off, in_=in_, in_offset=in_off)
            if bounds not in regs:
                regs[bounds] = e.to_reg(bounds)
            return e.indirect_dma_start(out=out, out_offset=out_off, in_=in_, in_offset=in_off,
                                        bounds_check=regs[bounds], oob_is_err=False)
        return self.op("gpsimd", fn, reads=reads, writes=writes, dma_key=("dma", key))

    def emit(self):
        nc = self.nc
        ops = self.ops
        ses = self.same_engine_sync

        def skip(p, o):
            return (p["dma_key"] is None and o["dma_key"] is None and p["eng"] == o["eng"]
                    and (p["eng"] == "tensor" or not ses))

        signal = [o["dma_key"] is not None for o in ops]
        for o in ops:
            for d in o["deps"]:
                if not skip(ops[d], o):
                    signal[d] = True
        sem_names = [("eng", e) for e in COMPUTE]
        for o in ops:
            if o["dma_key"] is not None and o["dma_key"] not in sem_names:
                sem_names.append(o["dma_key"])
        sems = {}
        for n, sn in enumerate(sem_names):
            sems[sn] = self.stack.enter_context(nc.semaphore("s%d" % n))
        counter = {sn: 0 for sn in sem_names}
        sig = [None] * len(ops)
        for i, o in enumerate(ops):
            if not signal[i]:
                continue
            sk = o["dma_key"] if o["dma_key"] is not None else ("eng", o["eng"])
            counter[sk] += 16 if o["dma_key"] is not None else 1
            sig[i] = (sk, counter[sk])
        by_eng = {}
        for i, o in enumerate(ops):
            by_eng.setdefault(o["eng"], []).append(i)
        by_eng.setdefault("sync", [])
        with nc.Block() as block:
            for en in list(by_eng.keys()):
                def body(e, en=en):
                    waited = {}
                    for i in by_eng[en]:
                        o = ops[i]
                        need = {}
                        for d in o["deps"]:
                            if sig[d] is None or skip(ops[d], o):
                                continue
                            sk, v = sig[d]
                            if v > need.get(sk, 0):
                                need[sk] = v
                        for sk, v in need.items():
                            if waited.get(sk, 0) >= v:
                                continue
                            e.wait_ge(sems[sk], v)
                            waited[sk] = v
                        try:
                            ins = o["fn"](e)
                        except Exception:
                            print("EMIT FAIL op", i, en, o["rw"], o["dma_key"])
                            raise
                        if sig[i] is not None:
                            ins.then_inc(sems[sig[i][0]], 16 if o["dma_key"] is not None else 1)
                    if en == "sync":
                        for sk in sem_names:
                            if counter[sk] > 0:
                                e.wait_ge(sems[sk], counter[sk])
                getattr(block, en)(body)
        self.stack.close()


def new_nc():
    return bass.Bass("TRN2", target_bir_lowering=False)


def din(nc, name, shape, dt=F32):
    return nc.dram_tensor(name, list(shape), dt, kind="ExternalInput").ap()


def dout(nc, name, shape, dt=F32):
    return nc.dram_tensor(name, list(shape), dt, kind="ExternalOutput").ap()


class WStream:
    CB = 128

    def __init__(self, P, nslots=4, ktmax=16):
        self.P = P
        self.n = nslots
        self.wst = [P.sb("wst%d" % i, [128, ktmax, self.CB], F32) for i in range(nslots)]
        self.wbf = [P.sb("wbf%d" % i, [128, ktmax, self.CB], BF16) for i in range(nslots)]
        self.ctr = 0

    def load(self, W, KT, cb):
        s = self.ctr % self.n
        self.ctr += 1
        P = self.P
        CB = self.CB
        P.dma("sync", self.wst[s][:, 0:KT, :], W[:, cb * CB:(cb + 1) * CB].rearrange("(k p) c -> p k c", p=128),
              writes=["wst%d" % s])
        P.I("gpsimd", "tensor_copy", ["wst%d" % s], ["wbf%d" % s], out=self.wbf[s][:, 0:KT, :],
            in_=self.wst[s][:, 0:KT, :])
        return self.wbf[s], "wbf%d" % s


class PsRing:
    def __init__(self, P, n, prefix="ps"):
        self.t = [P.ps("%s%d" % (prefix, i), [128, 512], F32) for i in range(n)]
        self.k = ["%s%d" % (prefix, i) for i in range(n)]
        self.c = 0

    def next(self):
        i = self.c % len(self.t)
        self.c += 1
        return self.t[i], self.k[i]


def linear(P, ws, pr, srcs, N, T, evac):
    for m in range(N // 128):
        wb = [ws.load(W, KT, m) for (W, a, KT, ak) in srcs]
        for h in range(T // 512):
            outs = []
            for si, (W, a, KT, ak) in enumerate(srcs):
                ps, pk = pr.next()
                for k in range(KT):
                    P.I("tensor", "matmul", [wb[si][1], ak], [pk], ps[:], lhsT=wb[si][0][:, k, :],
                        rhs=a[:, k, h * 512:(h + 1) * 512], start=(k == 0), stop=(k == KT - 1))
                outs.append((ps, pk))
            evac(m, h, outs)


def rstd_from_ps(P, ps, pk, out_ap, okey, tmp, tkey, scale):
    P.I("scalar", "activation", [pk], [tkey], out=tmp, in_=ps, func=AF.Sqrt, bias=EPS, scale=scale)
    P.I("vector", "reciprocal", [tkey], [okey], out=out_ap, in_=tmp)


def build_P():
    nc = new_nc()
    xT = din(nc, "xT", [D, TPC])
    g = din(nc, "g", [128, 16])
    W = din(nc, "W", [D, 8192])
    out = dout(nc, "projT", [8192, TPC])
    P = Prog(nc)
    xf = [P.sb("xf%d" % i, [128, TPC], F32) for i in range(2)]
    hT = P.sb("hT", [128, 16, TPC], BF16)
    sq = [P.sb("sq%d" % i, [128, TPC], BF16) for i in range(2)]
    ones = P.sb("ones", [128, 128], BF16)
    gsb = P.sb("gsb", [128, 16], F32)
    rstd = P.sb("rstd", [128, TPC], F32)
    tmp = P.sb("tmp", [128, 512], F32)
    ost = [P.sb("ost%d" % i, [128, 512], F32) for i in range(4)]
    ws = WStream(P, 4, 16)
    pr = PsRing(P, 4)
    pss = [P.ps("pss%d" % i, [128, 512], F32) for i in range(2)]
    P.I("vector", "memset", [], ["ones"], ones[:], 1.0)
    P.dma("sync", gsb[:], g, writes=["gsb"])
    for k in range(16):
        s = k % 2
        P.dma("sync", xf[s][:], xT[k * 128:(k + 1) * 128, :], writes=["xf%d" % s])
        P.I("scalar", "activation", ["xf%d" % s, "gsb"], ["hT"], out=hT[:, k, :], in_=xf[s][:], func=AF.Copy,
            scale=gsb[:, k:k + 1])
        P.I("vector", "tensor_tensor", ["xf%d" % s], ["sq%d" % s], out=sq[s][:], in0=xf[s][:], in1=xf[s][:],
            op=ALU.mult)
        for h in range(2):
            P.I("tensor", "matmul", ["ones", "sq%d" % s], ["pss%d" % h], pss[h][:], lhsT=ones[:],
                rhs=sq[s][:, h * 512:(h + 1) * 512], start=(k == 0), stop=(k == 15))
    for h in range(2):
        rstd_from_ps(P, pss[h][:], "pss%d" % h, rstd[:, h * 512:(h + 1) * 512], "rstd%d" % h, tmp[:], "tmp",
                     1.0 / D)
    cnt = [0]

    def evac(m, h, outs):
        ps, pk = outs[0]
        s = cnt[0] % 4
        cnt[0] += 1
        P.I("vector", "tensor_tensor", [pk, "rstd%d" % h], ["ost%d" % s], out=ost[s][:], in0=ps[:],
            in1=rstd[:, h * 512:(h + 1) * 512], op=ALU.mult)
        P.dma("scalar", out[m * 128:(m + 1) * 128, h * 512:(h + 1) * 512], ost[s][:], reads=["ost%d" % s],
              key="ost%d" % s)

    linear(P, ws, pr, [(W, hT, 16, "hT")], 8192, TPC, evac)
    P.emit()
    return nc


def build_M():
    nc = new_nc()
    ysT = din(nc, "ysT", [1024, TPC])
    yaT = din(nc, "yaT", [1024, TPC])
    glT = din(nc, "glT", [4096, TPC])
    xT = din(nc, "xT", [D, TPC])
    w_glu = din(nc, "w_glu", [1024, 1024])
    w_brs = din(nc, "w_brs", [1024, D])
    w_bra = din(nc, "w_bra", [1024, D])
    w_out = din(nc, "w_out", [D, D])
    gate_b = din(nc, "gate_b", [128, 32])
    gffn = din(nc, "gffn", [128, 16])
    w_r = din(nc, "w_r", [128, 16, 16])
    x1T = dout(nc, "x1T", [D, TPC])
    xnT = dout(nc, "xnT", [D, TPC], BF16)
    affT = dout(nc, "affT", [16, TPC])
    P = Prog(nc)
    arena = P.sb("arena", [128, 16 * TPC], F32)
    ab = arena[:].bitcast(BF16)
    ys = ab[:, 0:8192].rearrange("p (k t) -> p k t", k=8)
    ys2 = ab[:, 8192:16384].rearrange("p (k t) -> p k t", k=8)
    ya = ab[:, 16384:24576].rearrange("p (k t) -> p k t", k=8)
    x1 = arena[:].rearrange("p (k t) -> p k t", k=16)
    merged = P.sb("merged", [128, 16, TPC], BF16)
    ldf = [P.sb("ldf%d" % i, [128, TPC], F32) for i in range(2)]
    gb = P.sb("gb", [128, 32], F32)
    gf = P.sb("gf", [128, 16], F32)
    wr = P.sb("wr", [128, 16, 16], F32)
    ones = P.sb("ones", [128, 128], BF16)
    ones16 = P.sb("ones16", [16, 16], F32)
    rstd = P.sb("rstd", [128, TPC], F32)
    tmp = P.sb("tmp", [128, 512], F32)
    tA = [P.sb("tA%d" % i, [128, 512], F32) for i in range(2)]
    tB = [P.sb("tB%d" % i, [128, 512], F32) for i in range(2)]
    glt = [P.sb("glt%d" % i, [128, 512], F32) for i in range(4)]
    gs = [P.sb("gs%d" % i, [128, 512], F32) for i in range(4)]
    sqb = [P.sb("sqb%d" % i, [128, 512], BF16) for i in range(2)]
    xnb = [P.sb("xnb%d" % i, [128, 512], BF16) for i in range(2)]
    esb = P.sb("esb", [16, 512], F32)
    rsb = P.sb("rsb", [16, 512], F32)
    asb = P.sb("asb", [16, 512], F32)
    ws = WStream(P, 4, 16)
    pr = PsRing(P, 4)
    pss = [P.ps("pss%d" % i, [128, 512], F32) for i in range(2)]
    plg = [P.ps("plg%d" % i, [16, 512], F32) for i in range(2)]
    P.I("vector", "memset", [], ["ones"], ones[:], 1.0)
    P.I("vector", "memset", [], ["ones16"], ones16[:], 1.0)
    P.dma("sync", gb[:], gate_b, writes=["gb"])
    P.dma("sync", gf[:], gffn, writes=["gf"])
    P.dma("sync", wr[:], w_r, writes=["wr"])
    for k in range(8):
        s = k % 2
        P.dma("sync", ldf[s][:], ysT[k * 128:(k + 1) * 128, :], writes=["ldf%d" % s])
        P.I("scalar", "activation", ["ldf%d" % s], ["ys"], out=ys[:, k, :], in_=ldf[s][:], func=AF.Gelu)
    for k in range(8):
        s = k % 2
        P.dma("sync", ldf[s][:], yaT[k * 128:(k + 1) * 128, :], writes=["ldf%d" % s])
        P.I("vector", "tensor_copy", ["ldf%d" % s], ["ya"], out=ya[:, k, :], in_=ldf[s][:])
    c1 = [0]

    def evac_glu(m, h, outs):
        ps, pk = outs[0]
        s = c1[0] % 2
        c1[0] += 1
        P.I("scalar", "activation", [pk], ["tA%d" % s], out=tA[s][:], in_=ps[:], func=AF.Sigmoid)
        P.I("vector", "tensor_tensor", ["tA%d" % s, "ys"], ["ys2"], out=ys2[:, m, h * 512:(h + 1) * 512],
            in0=ys[:, m, h * 512:(h + 1) * 512], in1=tA[s][:], op=ALU.mult)

    linear(P, ws, pr, [(w_glu, ys, 8, "ys")], 1024, TPC, evac_glu)
    c2 = [0]

    def evac_br(m, h, outs):
        (pa, pak), (pb, pbk) = outs
        s = c2[0] % 2
        c2[0] += 1
        for br in range(2):
            gi = 2 * s + br
            P.dma("sync", glt[gi][:], glT[br * D + m * 128: br * D + (m + 1) * 128, h * 512:(h + 1) * 512],
                  writes=["glt%d" % gi])
            P.I("scalar", "activation", ["glt%d" % gi, "gb"], ["gs%d" % gi], out=gs[gi][:], in_=glt[gi][:],
                func=AF.Sigmoid, bias=gb[:, br * 16 + m: br * 16 + m + 1])
        P.I("vector", "tensor_tensor", [pak, "gs%d" % (2 * s)], ["tA%d" % s], out=tA[s][:], in0=pa[:],
            in1=gs[2 * s][:], op=ALU.mult)
        P.I("vector", "tensor_tensor", [pbk, "gs%d" % (2 * s + 1)], ["tB%d" % s], out=tB[s][:], in0=pb[:],
            in1=gs[2 * s + 1][:], op=ALU.mult)
        P.I("vector", "tensor_tensor", ["tA%d" % s, "tB%d" % s], ["merged"],
            out=merged[:, m, h * 512:(h + 1) * 512], in0=tA[s][:], in1=tB[s][:], op=ALU.add)

    linear(P, ws, pr, [(w_brs, ys2, 8, "ys2"), (w_bra, ya, 8, "ya")], D, TPC, evac_br)
    P.alias(["x1"], ["ys", "ys2", "ya"])
    c3 = [0]

    def evac_out(m, h, outs):
        ps, pk = outs[0]
        s = c3[0] % 2
        c3[0] += 1
        sl = slice(h * 512, (h + 1) * 512)
        P.dma("sync", tA[s][:], xT[m * 128:(m + 1) * 128, sl], writes=["tA%d" % s])
        P.I("vector", "tensor_tensor", [pk, "tA%d" % s], ["x1"], out=x1[:, m, sl], in0=ps[:], in1=tA[s][:],
            op=ALU.add)
        P.dma("scalar", x1T[m * 128:(m + 1) * 128, sl], x1[:, m, sl], reads=["x1"], key="x1out")
        P.I("scalar", "activation", ["x1"], ["sqb%d" % s], out=sqb[s][:], in_=x1[:, m, sl], func=AF.Square)
        P.I("tensor", "matmul", ["ones", "sqb%d" % s], ["pss%d" % h], pss[h][:], lhsT=ones[:], rhs=sqb[s][:],
            start=(m == 0), stop=(m == 15))

    linear(P, ws, pr, [(w_out, merged, 16, "merged")], D, TPC, evac_out)
    for h in range(2):
        sl = slice(h * 512, (h + 1) * 512)
        rstd_from_ps(P, pss[h][:], "pss%d" % h, rstd[:, sl], "rstd%d" % h, tmp[:], "tmp", 1.0 / D)
        for m in range(16):
            s = m % 2
            P.I("vector", "scalar_tensor_tensor", ["x1", "gf", "rstd%d" % h], ["tB%d" % s], out=tB[s][:],
                in0=x1[:, m, sl], scalar=gf[:, m:m + 1], in1=rstd[:, sl], op0=ALU.mult, op1=ALU.mult)
            P.I("tensor", "matmul", ["wr", "tB%d" % s], ["plg%d" % h], plg[h][:], lhsT=wr[:, m, :], rhs=tB[s][:],
                start=(m == 0), stop=(m == 15))
            P.I("scalar", "activation", ["tB%d" % s], ["xnb%d" % s], out=xnb[s][:], in_=tB[s][:], func=AF.Copy)
            P.dma("scalar", xnT[m * 128:(m + 1) * 128, sl], xnb[s][:], reads=["xnb%d" % s], key="xnb%d" % s)
        P.I("scalar", "activation", ["plg%d" % h], ["esb"], out=esb[:], in_=plg[h][:], func=AF.Exp)
        P.I("tensor", "matmul", ["ones16", "esb"], ["plg%d" % h], plg[h][:], lhsT=ones16[:], rhs=esb[:],
            start=True, stop=True)
        P.I("vector", "reciprocal", ["plg%d" % h], ["rsb"], out=rsb[:], in_=plg[h][:])
        P.I("vector", "tensor_tensor", ["esb", "rsb"], ["asb"], out=asb[:], in0=esb[:], in1=rsb[:], op=ALU.mult)
        P.dma("scalar", affT[:, sl], asb[:], reads=["asb"], key="asb")
    P.emit()
    return nc


def build_E():
    nc = new_nc()
    affc = din(nc, "affc", [2, 128, 64])
    xn = din(nc, "xn", [L, D], BF16)
    wg = din(nc, "wg", [2, D, 1024])
    wu = din(nc, "wu", [2, D, 1024])
    wd = din(nc, "wd", [2, 1024, D])
    tokid = din(nc, "tokid", [128, 64, 2], I32)
    tri = din(nc, "tri", [128, 128])
    ebase = din(nc, "ebase", [128, 2])
    identd = din(nc, "identd", [128, 128], BF16)
    yeT = dout(nc, "yeT", [2, D, 1024])
    posrow = dout(nc, "posrow", [2, 128, 64], I32)
    idxl = [nc.dram_tensor("idxl%d" % i, [1024, 2], I32, kind="Internal").ap() for i in range(2)]
    P = Prog(nc)
    af = P.sb("af", [128, 2, 64], F32)
    tok = P.sb("tok", [128, 64, 2], I32)
    trs = P.sb("trs", [128, 128], F32)
    ebs = P.sb("ebs", [128, 2], F32)
    ebm = P.sb("ebm", [128, 2], F32)
    ident = P.sb("ident", [128, 128], BF16)
    onesf = P.sb("onesf", [128, 128], F32)
    ones64 = P.sb("ones64", [128, 64], F32)
    lo = P.sb("lo", [128, 2], F32)
    hi = P.sb("hi", [128, 2], F32)
    mid = P.sb("mid", [128, 2], F32)
    cntp = P.sb("cntp", [128, 2], F32)
    cond = P.sb("cond", [128, 2], F32)
    d1 = P.sb("d1", [128, 2], F32)
    d2 = P.sb("d2", [128, 2], F32)
    junk = P.sb("junk", [128, 2, 64], F32)
    mask = P.sb("mask", [128, 2, 64], F32)
    incl = P.sb("incl", [128, 2, 64], F32)
    tot = P.sb("tot", [128, 2], F32)
    offm1 = P.sb("offm1", [128, 2], F32)
    posf = P.sb("posf", [128, 2, 64], F32)
    t1 = P.sb("t1", [128, 2, 64], F32)
    t2 = P.sb("t2", [128, 2, 64], F32)
    posi = P.sb("posi", [128, 2, 64], I32)
    rowi = P.sb("rowi", [128, 2, 64], I32)
    idxs = P.sb("idxs", [128, 2, 8, 2], I32)
    xe = [P.sb("xe%d" % i, [128, D], BF16) for i in range(2)]
    xeT = P.sb("xeT", [128, 16, 1024], BF16)
    act = P.sb("act", [128, 8, 1024], BF16)
    tA = [P.sb("tA%d" % i, [128, 512], F32) for i in range(2)]
    ost = [P.sb("ost%d" % i, [128, 512], F32) for i in range(2)]
    ws = WStream(P, 4, 16)
    pr = PsRing(P, 4)
    pc = P.ps("pc", [128, 2], F32)
    pT = [P.ps("pT%d" % i, [128, 4, 128], BF16) for i in range(2)]
    P.dma("sync", af[:], affc.rearrange("e p f -> p e f"), writes=["af"])
    P.dma("sync", tok[:], tokid, writes=["tok"])
    P.dma("sync", trs[:], tri, writes=["trs"])
    P.dma("sync", ebs[:], ebase, writes=["ebs"])
    P.dma("sync", ident[:], identd, writes=["ident"])
    P.I("vector", "memset", [], ["onesf"], onesf[:], 1.0)
    P.I("vector", "memset", [], ["ones64"], ones64[:], 1.0)
    P.I("vector", "memset", [], ["lo"], lo[:], 0.0)
    P.I("vector", "memset", [], ["hi"], hi[:], 1.0)
    P.I("vector", "tensor_scalar", ["ebs"], ["ebm"], out=ebm[:], in0=ebs[:], scalar1=-BIG, scalar2=None,
        op0=ALU.add)
    V = "vector"
    for it in range(30):
        P.I(V, "tensor_tensor", ["lo", "hi"], ["mid"], out=mid[:], in0=lo[:], in1=hi[:], op=ALU.add)
        P.I(V, "tensor_scalar", ["mid"], ["mid"], out=mid[:], in0=mid[:], scalar1=0.5, scalar2=None, op0=ALU.mult)
        for e in range(2):
            P.I(V, "tensor_scalar", ["af", "mid"], ["junk"], out=junk[:, e, :], in0=af[:, e, :],
                scalar1=mid[:, e:e + 1], scalar2=None, op0=ALU.is_ge)
            P.I(V, "reduce_sum", ["junk"], ["cntp"], out=cntp[:, e:e + 1], in_=junk[:, e, :], axis=AX.X)
        P.I("tensor", "matmul", ["onesf", "cntp"], ["pc"], pc[:], lhsT=onesf[:], rhs=cntp[:], start=True, stop=True)
        P.I(V, "tensor_scalar", ["pc"], ["cond"], out=cond[:], in0=pc[:], scalar1=1024.0, scalar2=None,
            op0=ALU.is_ge)
        P.I(V, "tensor_tensor", ["mid", "lo"], ["d1"], out=d1[:], in0=mid[:], in1=lo[:], op=ALU.subtract)
        P.I(V, "tensor_tensor", ["d1", "cond"], ["d1"], out=d1[:], in0=d1[:], in1=cond[:], op=ALU.mult)
        P.I(V, "tensor_tensor", ["hi", "mid"], ["d2"], out=d2[:], in0=hi[:], in1=mid[:], op=ALU.subtract)
        P.I(V, "tensor_tensor", ["d2", "cond"], ["d2"], out=d2[:], in0=d2[:], in1=cond[:], op=ALU.mult)
        P.I(V, "tensor_tensor", ["lo", "d1"], ["lo"], out=lo[:], in0=lo[:], in1=d1[:], op=ALU.add)
        P.I(V, "tensor_tensor", ["mid", "d2"], ["hi"], out=hi[:], in0=mid[:], in1=d2[:], op=ALU.add)
    for e in range(2):
        P.I(V, "tensor_scalar", ["af", "lo"], ["mask"], out=mask[:, e, :], in0=af[:, e, :], scalar1=lo[:, e:e + 1],
            scalar2=None, op0=ALU.is_ge)
        P.I(V, "tensor_tensor_scan", ["mask", "ones64"], ["incl"], out=incl[:, e, :], data0=ones64[:],
            data1=mask[:, e, :], initial=0.0, op0=ALU.mult, op1=ALU.add)
        P.I(V, "tensor_copy", ["incl"], ["tot"], out=tot[:, e:e + 1], in_=incl[:, e, 63:64])
    P.I("tensor", "matmul", ["trs", "tot"], ["pc"], pc[:], lhsT=trs[:], rhs=tot[:], start=True, stop=True)
    P.I(V, "tensor_scalar", ["pc"], ["offm1"], out=offm1[:], in0=pc[:], scalar1=-1.0, scalar2=None, op0=ALU.add)
    for e in range(2):
        P.I(V, "tensor_scalar", ["incl", "offm1"], ["posf"], out=posf[:, e, :], in0=incl[:, e, :],
            scalar1=offm1[:, e:e + 1], scalar2=None, op0=ALU.add)
        P.I(V, "scalar_tensor_tensor", ["posf", "mask"], ["t1"], out=t1[:, e, :], in0=posf[:, e, :], scalar=-BIG,
            in1=mask[:, e, :], op0=ALU.add, op1=ALU.mult)
        P.I(V, "tensor_scalar", ["t1"], ["t1"], out=t1[:, e, :], in0=t1[:, e, :], scalar1=BIG, scalar2=None,
            op0=ALU.add)
        P.I(V, "tensor_copy", ["t1"], ["posi"], out=posi[:, e, :], in_=t1[:, e, :])
        P.I(V, "scalar_tensor_tensor", ["posf", "mask", "ebm"], ["t2"], out=t2[:, e, :], in0=posf[:, e, :],
            scalar=ebm[:, e:e + 1], in1=mask[:, e, :], op0=ALU.add, op1=ALU.mult)
        P.I(V, "tensor_scalar", ["t2"], ["t2"], out=t2[:, e, :], in0=t2[:, e, :], scalar1=BIG, scalar2=None,
            op0=ALU.add)
        P.I(V, "tensor_copy", ["t2"], ["rowi"], out=rowi[:, e, :], in_=t2[:, e, :])
    P.dma("sync", posrow.rearrange("e p f -> p e f"), rowi[:], reads=["rowi"], key="rowi")
    c0 = [0]
    for e in range(2):
        fk = []
        for f in range(64):
            P.idma(idxl[e], IOA(ap=posi[:, e, f:f + 1], axis=0), tok[:, f, :], None, reads=["posi", "tok"],
                   writes=["idxl%d_%d" % (e, f)], key="idxl%d" % e, bounds=1023)
            fk.append("idxl%d_%d" % (e, f))
        P.dma("sync", idxs[:, e, :, :], idxl[e].rearrange("(p j) o -> p j o", j=8), reads=fk, writes=["idxs%d" % e])
        for jt in range(8):
            s = jt % 2
            P.idma(xe[s][:, :], None, xn, IOA(ap=idxs[:, e, jt, 0:1], axis=0), reads=["idxs%d" % e],
                   writes=["xe%d" % s], key="xe%d" % s)
            for kq in range(4):
                ti = c0[0] % 2
                c0[0] += 1
                for j in range(4):
                    k = kq * 4 + j
                    P.I("tensor", "transpose", ["xe%d" % s, "ident"], ["pT%d" % ti], out=pT[ti][:, j, :],
                        in_=xe[s][:, k * 128:(k + 1) * 128], identity=ident[:])
                P.I("scalar" if kq % 2 else "vector", "tensor_copy" if kq % 2 == 0 else "copy", ["pT%d" % ti],
                    ["xeT"], out=xeT[:, kq * 4:(kq + 1) * 4, jt * 128:(jt + 1) * 128], in_=pT[ti][:])
        c1 = [0]

        def evac_ffn(m, h, outs):
            (pg, pgk), (pu, puk) = outs
            s = c1[0] % 2
            c1[0] += 1
            P.I("scalar", "activation", [pgk], ["tA%d" % s], out=tA[s][:], in_=pg[:], func=AF.Silu)
            P.I("vector", "tensor_tensor", ["tA%d" % s, puk], ["act"], out=act[:, m, h * 512:(h + 1) * 512],
                in0=tA[s][:], in1=pu[:], op=ALU.mult)

        linear(P, ws, pr, [(wg[e], xeT, 16, "xeT"), (wu[e], xeT, 16, "xeT")], 1024, 1024, evac_ffn)

        def evac_dn(m, h, outs, e=e):
            ps, pk = outs[0]
            s = c1[0] % 2
            c1[0] += 1
            P.I("scalar", "copy", [pk], ["ost%d" % s], out=ost[s][:], in_=ps[:])
            P.dma("sync", yeT[e, m * 128:(m + 1) * 128, h * 512:(h + 1) * 512], ost[s][:], reads=["ost%d" % s],
                  key="ost%d" % s)

        linear(P, ws, pr, [(wd[e], act, 8, "act")], D, 1024, evac_dn)
    P.emit()
    return nc


def build_C():
    nc = new_nc()
    x1 = din(nc, "x1", [TPC, D])
    ye = din(nc, "ye", [16 * 1024, D])
    prow = din(nc, "prow", [TPC, 16], I32)
    aff = din(nc, "aff", [TPC, 16])
    x2 = dout(nc, "x2", [TPC, D])
    P = Prog(nc)
    acc = [P.sb("acc%d" % i, [128, D], F32) for i in range(2)]
    prt = [P.sb("prt%d" % i, [128, 16], I32) for i in range(2)]
    aft = [P.sb("aft%d" % i, [128, 16], F32) for i in range(2)]
    gbuf = [P.sb("gb%d" % i, [128, D], F32) for i in range(4)]
    c = 0
    for t in range(TPC // 128):
        s = t % 2
        rs = slice(t * 128, (t + 1) * 128)
        P.dma("sync", acc[s][:], x1[rs, :], writes=["acc%d" % s])
        P.dma("sync", prt[s][:], prow[rs, :], writes=["prt%d" % s])
        P.dma("sync", aft[s][:], aff[rs, :], writes=["aft%d" % s])
        for e in range(16):
            b = c % 4
            c += 1
            P.I("scalar", "memzero", [], ["gb%d" % b], gbuf[b][:])
            P.idma(gbuf[b][:, :], None, ye, IOA(ap=prt[s][:, e:e + 1], axis=0), reads=["prt%d" % s],
                   writes=["gb%d" % b], key="gb%d" % b, bounds=16 * 1024 - 1)
            P.I("vector", "scalar_tensor_tensor", ["gb%d" % b, "aft%d" % s, "acc%d" % s], ["acc%d" % s],
                out=acc[s][:], in0=gbuf[b][:], scalar=aft[s][:, e:e + 1], in1=acc[s][:], op0=ALU.mult, op1=ALU.add)
        P.dma("scalar", x2[rs, :], acc[s][:], reads=["acc%d" % s], key="acc%d" % s)
    P.emit()
    return nc


def consts_E():
    tokid = np.ascontiguousarray(np.repeat(np.arange(L, dtype=np.int32).reshape(128, 64, 1), 2, axis=2))
    tri = (np.arange(128)[:, None] < np.arange(128)[None, :]).astype(np.float32)
    ident = np.eye(128, dtype=np.float32).astype(ml_dtypes.bfloat16)
    return tokid, tri, ident


def build_A():
    nc = new_nc()
    qT = din(nc, "qT", [128, L])
    kT = din(nc, "kT", [128, L])
    vT = din(nc, "vT", [128, L])
    ctab = din(nc, "ctab", [128, L])
    stab = din(nc, "stab", [128, L])
    small = din(nc, "small", [128, 8])
    lamin = din(nc, "lamin", [128, 4, 64])
    pmd = din(nc, "pmd", [128, 128], BF16)
    bdd = din(nc, "bdd", [128, 128], BF16)
    identd = din(nc, "identd", [128, 128], BF16)
    oT = dout(nc, "oT", [128, L])
    P = Prog(nc)
    V, A_, T_ = "vector", "scalar", "tensor"
    sm = P.sb("sm", [128, 8], F32)
    li = P.sb("li", [128, 4, 64], F32)
    pm = P.sb("pm", [128, 128], BF16)
    bd = P.sb("bd", [128, 128], BF16)
    ident = P.sb("ident", [128, 128], BF16)
    ones = P.sb("ones", [128, 128], BF16)
    lt = P.sb("lt", [128, 2, 64], F32)
    ls = P.sb("ls", [128, 8], F32)
    qr = P.sb("qr", [128, L], BF16)
    kr = P.sb("kr", [128, L], BF16)
    Vt = P.sb("Vt", [128, 64, 128], BF16)
    raw = [P.sb("raw%d" % i, [128, 512], F32) for i in range(3)]
    ct = [P.sb("ct%d" % i, [128, 512], F32) for i in range(2)]
    st = [P.sb("st%d" % i, [128, 512], F32) for i in range(2)]
    sqb = [P.sb("sqb%d" % i, [128, 512], BF16) for i in range(2)]
    qn = [P.sb("qn%d" % i, [128, 512], F32) for i in range(2)]
    qnb = [P.sb("qnb%d" % i, [128, 512], BF16) for i in range(2)]
    rs = [P.sb("rs%d" % i, [128, 512], F32) for i in range(2)]
    tmp = P.sb("tmp", [128, 512], F32)
    t1 = [P.sb("t1%d" % i, [128, 512], F32) for i in range(2)]
    t2 = [P.sb("t2%d" % i, [128, 512], F32) for i in range(2)]
    vb = [P.sb("vb%d" % i, [128, 512], BF16) for i in range(2)]
    pt = [P.sb("pt%d" % i, [128, 512], BF16) for i in range(4)]
    ost = [P.sb("ost%d" % i, [128, 512], F32) for i in range(2)]
    sacc = [[P.sb("sacc%d_%d" % (i, j), [128, 512], F32) for j in range(2)] for i in range(2)]
    onesf = P.sb("onesf", [128, 128], F32)
    P.I("vector", "memset", [], ["onesf"], onesf[:], 1.0)
    ring = PsRing(P, 3, "pr")
    acc = [P.ps("acc%d" % i, [128, 512], F32) for i in range(4)]
    pT = P.ps("pT", [128, 4, 128], BF16)
    P.dma("sync", sm[:], small, writes=["sm"])
    P.dma("sync", li[:], lamin, writes=["li"])
    P.dma("sync", pm[:], pmd, writes=["pm"])
    P.dma("sync", bd[:], bdd, writes=["bd"])
    P.dma("sync", ident[:], identd, writes=["ident"])
    P.I(V, "memset", [], ["ones"], ones[:], 1.0)
    for j in range(2):
        P.I(V, "tensor_tensor", ["li"], ["lt"], out=lt[:, j, :], in0=li[:, 2 * j, :], in1=li[:, 2 * j + 1, :],
            op=ALU.mult)
        P.I(V, "reduce_sum", ["lt"], ["ls"], out=ls[:, j:j + 1], in_=lt[:, j, :], axis=AX.X)
        P.I(A_, "activation", ["ls"], ["ls"], out=ls[:, 2 + j:3 + j], in_=ls[:, j:j + 1], func=AF.Exp)
    P.I(V, "tensor_tensor", ["ls"], ["ls"], out=ls[:, 4:5], in0=ls[:, 2:3], in1=ls[:, 3:4], op=ALU.subtract)
    P.I(V, "tensor_tensor", ["ls", "sm"], ["ls"], out=ls[:, 4:5], in0=ls[:, 4:5], in1=sm[:, 3:4], op=ALU.add)
    P.I(V, "tensor_scalar", ["ls"], ["ls"], out=ls[:, 5:6], in0=ls[:, 4:5], scalar1=-1.0, scalar2=None,
        op0=ALU.mult)
    P.I(V, "tensor_tensor", ["sm"], ["ls"], out=ls[:, 6:7], in0=sm[:, 2:3], in1=sm[:, 4:5], op=ALU.mult)
    P.I(V, "tensor_scalar", ["sm"], ["ls"], out=ls[:, 7:8], in0=sm[:, 0:1], scalar1=0.125, scalar2=None,
        op0=ALU.mult)
    cc = 0
    for tcx in range(L // 512):
        sl = slice(tcx * 512, (tcx + 1) * 512)
        s2 = tcx % 2
        P.dma("sync", ct[s2][:], ctab[:, sl], writes=["ct%d" % s2])
        P.dma("sync", st[s2][:], stab[:, sl], writes=["st%d" % s2])
        for (src, gap, gk, dst, dk) in ((qT, ls[:, 7:8], "ls", qr, "qr"), (kT, sm[:, 1:2], "sm", kr, "kr")):
            s = cc % 2
            r3 = cc % 3
            cc += 1
            P.dma("sync", raw[r3][:], src[:, sl], writes=["raw%d" % r3])
            P.I(A_, "activation", ["raw%d" % r3], ["sqb%d" % s], out=sqb[s][:], in_=raw[r3][:], func=AF.Square)
            ps, pk = ring.next()
            P.I(T_, "matmul", ["bd", "sqb%d" % s], [pk], ps[:], lhsT=bd[:], rhs=sqb[s][:], start=True, stop=True)
            rstd_from_ps(P, ps[:], pk, rs[s][:], "rs%d" % s, tmp[:], "tmp", 1.0 / 64)
            P.I(V, "scalar_tensor_tensor", ["raw%d" % r3, gk, "rs%d" % s], ["qn%d" % s], out=qn[s][:],
                in0=raw[r3][:], scalar=gap, in1=rs[s][:], op0=ALU.mult, op1=ALU.mult)
            P.I(A_, "activation", ["qn%d" % s], ["qnb%d" % s], out=qnb[s][:], in_=qn[s][:], func=AF.Copy)
            ps2, pk2 = ring.next()
            P.I(T_, "matmul", ["pm", "qnb%d" % s], [pk2], ps2[:], lhsT=pm[:], rhs=qnb[s][:], start=True, stop=True)
            P.I(V, "tensor_tensor", [pk2, "st%d" % s2], ["t1%d" % s], out=t1[s][:], in0=ps2[:], in1=st[s2][:],
                op=ALU.mult)
            P.I("gpsimd", "tensor_tensor", ["qn%d" % s, "ct%d" % s2], ["t2%d" % s], out=t2[s][:], in0=qn[s][:],
                in1=ct[s2][:], op=ALU.mult)
            P.I(V, "tensor_tensor", ["t1%d" % s, "t2%d" % s], [dk], out=dst[:, sl], in0=t1[s][:], in1=t2[s][:],
                op=ALU.add)
        r3 = cc % 3
        cc += 1
        P.dma("sync", raw[r3][:], vT[:, sl], writes=["raw%d" % r3])
        P.I("gpsimd", "tensor_copy", ["raw%d" % r3], ["vb%d" % s2], out=vb[s2][:], in_=raw[r3][:])
        for j in range(4):
            P.I(T_, "transpose", ["vb%d" % s2, "ident"], ["pT"], out=pT[:, j, :], in_=vb[s2][:, j * 128:(j + 1) * 128],
                identity=ident[:])
        P.I(A_, "copy", ["pT"], ["Vt"], out=Vt[:, tcx * 4:(tcx + 1) * 4, :], in_=pT[:])
    steps = [(qc, c, kt) for qc in range(L // 512) for c in range(2) for kt in range(64)]
    ns = len(steps)
    sps = [None] * ns

    def emit_S(i):
        qc, c, kt = steps[i]
        ps, pk = ring.next()
        P.I(T_, "matmul", ["kr", "qr"], [pk], ps[:], lhsT=kr[64 * c:64 * c + 64, kt * 128:(kt + 1) * 128],
            rhs=qr[64 * c:64 * c + 64, qc * 512:(qc + 1) * 512], start=True, stop=True)
        sps[i] = (ps, pk)

    def emit_rest(i):
        qc, c, kt = steps[i]
        ps, pk = sps[i]
        s = i % 4
        P.I(A_, "activation", [pk], ["pt%d" % s], out=pt[s][:], in_=ps[:], func=AF.Exp)
        P.I(T_, "matmul", ["Vt", "pt%d" % s], ["acc%d" % c], acc[c][:], lhsT=Vt[:, kt, :], rhs=pt[s][:],
            start=(kt == 0), stop=(kt == 63))
        eg = kt % 2
        en_ = V if eg == 0 else "gpsimd"
        sk_ = "sacc%d_%d" % (c, eg)
        if kt < 2:
            P.I(en_, "tensor_copy", ["pt%d" % s], [sk_], out=sacc[c][eg][:], in_=pt[s][:])
        else:
            P.I(en_, "tensor_tensor", ["pt%d" % s, sk_], [sk_], out=sacc[c][eg][:], in0=sacc[c][eg][:], in1=pt[s][:],
                op=ALU.add)
        if kt == 63:
            for eg2 in range(2):
                P.I(T_, "matmul", ["onesf", "sacc%d_%d" % (c, eg2)], ["acc%d" % (2 + c)], acc[2 + c][:], lhsT=onesf[:],
                    rhs=sacc[c][eg2][:], start=(eg2 == 0), stop=(eg2 == 1))
        if c == 1 and kt == 63:
            sl = slice(qc * 512, (qc + 1) * 512)
            e = qc % 2
            for c2 in range(2):
                P.I(V, "reciprocal", ["acc%d" % (2 + c2)], ["rs%d" % c2], out=rs[c2][:], in_=acc[2 + c2][:])
                P.I(V, "tensor_tensor", ["acc%d" % c2, "rs%d" % c2], ["t1%d" % c2], out=t1[c2][:], in0=acc[c2][:],
                    in1=rs[c2][:], op=ALU.mult)
            P.I(V, "scalar_tensor_tensor", ["t11", "ls", "t10"], ["qn%d" % e], out=qn[e][:], in0=t1[1][:],
                scalar=ls[:, 5:6], in1=t1[0][:], op0=ALU.mult, op1=ALU.add)
            P.I("gpsimd", "tensor_tensor", ["qn%d" % e], ["sqb%d" % e], out=sqb[e][:], in0=qn[e][:], in1=qn[e][:],
                op=ALU.mult)
            ps3, pk3 = acc[2], "acc2"
            P.I(T_, "matmul", ["ones", "sqb%d" % e], [pk3], ps3[:], lhsT=ones[:], rhs=sqb[e][:], start=True, stop=True)
            rstd_from_ps(P, ps3[:], pk3, t2[e][:], "t2%d" % e, tmp[:], "tmp", 1.0 / 128)
            P.I(V, "scalar_tensor_tensor", ["qn%d" % e, "ls", "t2%d" % e], ["ost%d" % e], out=ost[e][:],
                in0=qn[e][:], scalar=ls[:, 6:7], in1=t2[e][:], op0=ALU.mult, op1=ALU.mult)
            P.dma("sync", oT[:, sl], ost[e][:], reads=["ost%d" % e], key="ost%d" % e)

    LOOK = 2
    for i in range(min(LOOK, ns)):
        emit_S(i)
    for i in range(ns):
        if i + LOOK < ns:
            emit_S(i + LOOK)
        emit_rest(i)
    P.emit()
    return nc


def consts_A():
    pos = np.arange(L, dtype=np.float64)
    inv = np.power(500000.0, -np.arange(0, 16, 2, dtype=np.float64) / 16)
    ang = (pos[:, None].astype(np.float32) * inv[None, :].astype(np.float32)).astype(np.float32).astype(np.float64)
    cos, sin = np.cos(ang).T, np.sin(ang).T
    ctab = np.ones((128, L), np.float32)
    stab = np.zeros((128, L), np.float32)
    pm = np.zeros((128, 128), np.float32)
    for half in range(2):
        b = half * 64
        ctab[b:b + 8] = cos
        ctab[b + 8:b + 16] = cos
        stab[b:b + 8] = -sin
        stab[b + 8:b + 16] = sin
        for i in range(8):
            pm[b + 8 + i, b + i] = 1.0
            pm[b + i, b + 8 + i] = 1.0
    bd = np.zeros((128, 128), np.float32)
    bd[:64, :64] = 1
    bd[64:, 64:] = 1
    bf = ml_dtypes.bfloat16
    return ctab, stab, pm.astype(bf), bd.astype(bf), np.eye(128, dtype=np.float32).astype(bf)


PI2 = 2.0 * math.pi
PI_SAFE = 3.141592


def build_S():
    nc = new_nc()
    U8 = din(nc, "U8", [8, 128, 1024])
    bT = din(nc, "bT", [128, 2, 1024])
    aB = din(nc, "aB", [128, 3, 1024])
    epart = din(nc, "epart", [128, 2])
    cP = din(nc, "cP", [128, 4, 16, 16])
    aC = din(nc, "aC", [128, 3, 16])
    ktab = din(nc, "ktab", [128, 16])
    seld = din(nc, "sel", [128, 3])
    dskd = din(nc, "dsk", [128, 8])
    matsd = din(nc, "mats", [128, 3, 128])
    cidxd = din(nc, "cidx", [128, 1024])
    y8 = dout(nc, "y8", [8, 128, 1024])
    P = Prog(nc)
    V, A_, T_, G_ = "vector", "scalar", "tensor", "gpsimd"
    PR = ["prep"]

    def tt(out, a, b, op, r=PR, w=PR, eng=V):
        P.I(eng, "tensor_tensor", r, w, out=out, in0=a, in1=b, op=op)

    def ts(out, a, s1, op0, s2=None, op1=None, r=PR, w=PR):
        kw = dict(out=out, in0=a, scalar1=s1, scalar2=s2, op0=op0)
        if op1 is not None:
            kw["op1"] = op1
        P.I(V, "tensor_scalar", r, w, **kw)

    def stt(out, a, s, b, op0, op1, r=PR, w=PR):
        P.I(V, "scalar_tensor_tensor", r, w, out=out, in0=a, scalar=s, in1=b, op0=op0, op1=op1)

    def act(out, a, func, r=PR, w=PR, **kw):
        P.I(A_, "activation", r, w, out=out, in_=a, func=func, **kw)

    G = [P.sb("G%d" % i, [128, 1024], F32) for i in range(20)]
    tki = P.sb("tki", [128, 1024], I32)
    sbT = P.sb("sbT", [128, 2, 1024], F32)
    saB = P.sb("saB", [128, 3, 1024], F32)
    sep = P.sb("sep", [128, 2], F32)
    scP = P.sb("scP", [128, 4, 16, 16], F32)
    saC = P.sb("saC", [128, 3, 16], F32)
    skt = P.sb("skt", [128, 16], F32)
    sel = P.sb("sel_s", [128, 3], F32)
    dsk = P.sb("dsk_s", [128, 8], F32)
    mats = P.sb("mats_s", [128, 3, 128], F32)
    cidx = P.sb("cidx_s", [128, 1024], F32)
    c16 = [P.sb("c16_%d" % i, [128, 16], F32) for i in range(12)]
    Lre = P.sb("Lre", [128, 16, 16], F32)
    Lim = P.sb("Lim", [128, 16, 16], F32)
    Cmat = P.sb("Cmat", [128, 2, 8, 128], BF16)
    Kc = P.sb("Kc", [128, 8, 128], BF16)
    W1 = P.sb("W1", [128, 16, 128], BF16)
    W2 = P.sb("W2", [128, 16, 128], BF16)
    thr = P.sb("thr", [128, 16], F32)
    r8 = P.sb("r8", [128, 16], F32)
    ustg = [P.sb("ustg%d" % i, [128, 1024], F32) for i in range(2)]
    U8b = [P.sb("U8b%d" % i, [128, 1024], BF16) for i in range(2)]
    Xin = [P.sb("Xin%d" % i, [128, 1024], BF16) for i in range(4)]
    ost = [P.sb("ost%d" % i, [128, 512], F32) for i in range(2)]
    ring = PsRing(P, 4, "pr")
    pk_ = [P.ps("pkk%d" % i, [128, 128], F32) for i in range(2)]
    pso = [P.ps("pso%d" % i, [128, 512], F32) for i in range(2)]
    for (t, src) in ((sbT, bT), (saB, aB), (sep, epart), (scP, cP), (saC, aC), (skt, ktab), (sel, seld), (dsk, dskd),
                     (mats, matsd), (cidx, cidxd)):
        P.dma("sync", t[:], src, writes=PR)

    def sincos(ang, sin_out, cos_out, n, r=PR, w=PR, ta=None, tkf=None):
        ta = ta[:, :n]
        tkf = tkf[:, :n]
        for shift, outp in ((0.0, sin_out), (math.pi / 2, cos_out)):
            ts(ta, ang, shift, ALU.add, r=r, w=w)
            ts(tki[:, :n], ta, 1.0 / PI2, ALU.mult, r=r, w=w)
            P.I(V, "tensor_copy", r, w, out=tkf, in_=tki[:, :n])
            stt(ta, tkf, -PI2, ta, ALU.mult, ALU.add, r=r, w=w)
            ts(ta, ta, -PI_SAFE, ALU.max, s2=PI_SAFE, op1=ALU.min, r=r, w=w)
            act(outp, ta, AF.Sin, r=r, w=w)

    stepC, drC, diC, nrC, denC, creC, cimC, t16a, t16b = [c[:] for c in c16[:9]]
    act(stepC, saC[:, 2, :], AF.Exp)
    tt(drC, saC[:, 0, :], stepC, ALU.mult)
    tt(diC, saC[:, 1, :], stepC, ALU.mult)
    g3 = lambda i: G[i][:, 0:256].rearrange("p (a b) -> p a b", a=16)
    argm, angk, magk, sk, ck = g3(0), g3(1), g3(2), g3(3), g3(4)
    kb = skt[:].unsqueeze(1).to_broadcast([128, 16, 16])
    tt(argm, drC.unsqueeze(2).to_broadcast([128, 16, 16]), kb, ALU.mult)
    tt(angk, diC.unsqueeze(2).to_broadcast([128, 16, 16]), kb, ALU.mult)
    act(G[2][:, 0:256], G[0][:, 0:256], AF.Exp)
    sincos(G[1][:, 0:256], G[3][:, 0:256], G[4][:, 0:256], 256, ta=G[5], tkf=G[6])
    tt(Lre[:], magk, ck, ALU.mult)
    tt(Lim[:], magk, sk, ALU.mult)
    ts(t16a, diC, 8.0, ALU.mult)
    ts(tki[:, 0:16], t16a, 1.0 / PI2, ALU.mult)
    P.I(V, "tensor_copy", PR, PR, out=t16b, in_=tki[:, 0:16])
    stt(thr[:], t16b, -PI2, t16a, ALU.mult, ALU.add)
    act(r8[:], drC, AF.Exp, scale=8.0)

    def coef(lbr, lbi, are, aim, cre, cim, nr, den, tmp):
        ts(nr, lbr, -1.0, ALU.add)
        tt(den, are, are, ALU.mult)
        tt(tmp, aim, aim, ALU.mult)
        tt(den, den, tmp, ALU.add)
        P.I(V, "reciprocal", PR, PR, out=den, in_=den)
        tt(cre, nr, are, ALU.mult)
        tt(tmp, lbi, aim, ALU.mult)
        tt(cre, cre, tmp, ALU.add)
        tt(cre, cre, den, ALU.mult)
        tt(cim, lbi, are, ALU.mult)
        tt(tmp, nr, aim, ALU.mult)
        tt(cim, cim, tmp, ALU.subtract)
        tt(cim, cim, den, ALU.mult)

    coef(Lre[:, :, 8], Lim[:, :, 8], saC[:, 0, :], saC[:, 1, :], creC, cimC, nrC, denC, t16a)
    Bbre, Bbim, tB = g3(0), g3(1), g3(2)
    crb = creC.unsqueeze(2).to_broadcast([128, 16, 16])
    cib = cimC.unsqueeze(2).to_broadcast([128, 16, 16])
    tt(Bbre, scP[:, 2], crb, ALU.mult)
    tt(tB, scP[:, 3], cib, ALU.mult)
    tt(Bbre, Bbre, tB, ALU.subtract)
    tt(Bbim, scP[:, 3], crb, ALU.mult)
    tt(tB, scP[:, 2], cib, ALU.mult)
    tt(Bbim, Bbim, tB, ALU.add)
    g4 = lambda i: G[i][:].rearrange("p (g i h) -> p g i h", g=8, i=8)
    QC = [G[8], G[9]]
    PB = [G[10], G[11]]

    def ksl(start, rev):
        return slice(start, start - 8 if start - 8 >= 0 else None, -1) if rev else slice(start, start + 8)

    def cmul_table(Are, Aim, d, ks, out, sB, rA, iA, t1, t2):
        dgs = slice(d * 8, d * 8 + 8)
        Lr = Lre[:, dgs, ks].unsqueeze(3).to_broadcast([128, 8, 8, 16])
        Li = Lim[:, dgs, ks].unsqueeze(3).to_broadcast([128, 8, 8, 16])
        Ar = Are[:, dgs, :].unsqueeze(2).to_broadcast([128, 8, 8, 16])
        Ai = Aim[:, dgs, :].unsqueeze(2).to_broadcast([128, 8, 8, 16])
        tt(rA, Ar, Lr, ALU.mult)
        tt(t1, Ai, Li, ALU.mult)
        tt(rA, rA, t1, ALU.subtract)
        tt(iA, Ar, Li, ALU.mult)
        tt(t2, Ai, Lr, ALU.mult)
        tt(iA, iA, t2, ALU.add)
        ts(rA, rA, sel[:, 0:1], ALU.mult)
        stt(out, iA, sB, rA, ALU.mult, ALU.add)

    for d in range(2):
        rev = (d == 1)
        cmul_table(scP[:, 0], scP[:, 1], d, ksl(15, True) if rev else ksl(8, False),
                   Cmat[:, d].rearrange("p g (i h) -> p g i h", i=8), sel[:, 1:2], g4(12), g4(13), g4(14), g4(15))
        cmul_table(scP[:, 0], scP[:, 1], d, ksl(7, True) if rev else ksl(7, False), g4(8 + d), sel[:, 1:2],
                   g4(12), g4(13), g4(14), g4(15))
        cmul_table(Bbre, Bbim, d, ksl(7, False) if rev else ksl(7, True), g4(10 + d), sel[:, 2:3],
                   g4(12), g4(13), g4(14), g4(15))
    k1 = G[12][:, 0:128]
    k2 = G[13][:, 0:128]
    k3 = G[14][:, 0:128]
    for g in range(8):
        for d in range(2):
            P.I(T_, "matmul", PR, ["pkk%d" % d], pk_[d][:], lhsT=PB[d][:, g * 128:(g + 1) * 128],
                rhs=QC[d][:, g * 128:(g + 1) * 128], start=True, stop=True)
        tt(k1, pk_[0][:], mats[:, 1, :], ALU.mult, r=PR + ["pkk0"])
        tt(k2, pk_[1][:], mats[:, 2, :], ALU.mult, r=PR + ["pkk1"])
        ts(k3, mats[:, 0, :], dsk[:, g:g + 1], ALU.mult)
        tt(k1, k1, k2, ALU.add)
        tt(Kc[:, g, :], k1, k3, ALU.add)
    stepB, drB, diB, mag1, s1, c1, nrB, denB, creB, cimB, tmpB = [G[i][:] for i in range(11)]
    act(stepB, saB[:, 2, :], AF.Exp)
    tt(drB, saB[:, 0, :], stepB, ALU.mult)
    tt(diB, saB[:, 1, :], stepB, ALU.mult)
    act(mag1, drB, AF.Exp)
    sincos(diB, s1, c1, 1024, ta=G[11], tkf=G[12])
    tt(c1, mag1, c1, ALU.mult)
    tt(s1, mag1, s1, ALU.mult)
    coef(c1, s1, saB[:, 0, :], saB[:, 1, :], creB, cimB, nrB, denB, tmpB)
    for d in range(2):
        hs = slice(d * 512, (d + 1) * 512)
        mage, ange, se, ce, Ere, Eim, t5, w1r, w1i = [G[i][:, 0:512] for i in range(11, 20)]
        act(mage, drB[:, hs], AF.Exp, scale=sep[:, d:d + 1])
        ts(ange, diB[:, hs], sep[:, d:d + 1], ALU.mult)
        sincos(ange, se, ce, 512, ta=G[0], tkf=G[3])
        tt(ce, mage, ce, ALU.mult)
        tt(se, mage, se, ALU.mult)
        tt(Ere, ce, creB[:, hs], ALU.mult)
        tt(t5, se, cimB[:, hs], ALU.mult)
        tt(Ere, Ere, t5, ALU.subtract)
        tt(Eim, ce, cimB[:, hs], ALU.mult)
        tt(t5, se, creB[:, hs], ALU.mult)
        tt(Eim, Eim, t5, ALU.add)
        tt(w1r, Ere, sbT[:, 0, hs], ALU.mult)
        tt(t5, Eim, sbT[:, 1, hs], ALU.mult)
        tt(w1r, w1r, t5, ALU.subtract)
        tt(w1i, Ere, sbT[:, 1, hs], ALU.mult)
        tt(t5, Eim, sbT[:, 0, hs], ALU.mult)
        tt(w1i, w1i, t5, ALU.add)
        dgs = slice(d * 8, d * 8 + 8)
        w1r3 = w1r.rearrange("p (g q) -> p g q", g=8)
        w1i3 = w1i.rearrange("p (g q) -> p g q", g=8)
        P.I(V, "tensor_copy", PR, PR, out=W1[:, dgs, 0:64], in_=w1r3)
        P.I(V, "tensor_copy", PR, PR, out=W1[:, dgs, 64:128], in_=w1i3)
        P.I(V, "tensor_copy", PR, PR, out=W2[:, dgs, 0:64], in_=w1i3)
        ts(W2[:, dgs, 64:128], w1r3, -1.0, ALU.mult)
    MK = ["tabA", "tabB", "S1", "S2", "Z1", "Z2", "ta", "tb", "ta2", "tb2", "sc"]
    P.alias(MK, PR)
    tabs = [(G[0], G[1], G[2]), (G[3], G[4], G[5])]
    S1, S2, Z1, Z2 = G[6], G[7], G[8], G[9]
    tA, tBb, tA2, tB2 = G[10], G[11], G[12], G[13]
    sc_ta, sc_tkf = G[14], G[15]
    it = 0
    for g in range(8):
        us = g % 2
        P.dma("sync", ustg[us][:], U8[g], writes=["ustg%d" % us])
        P.I(G_, "tensor_copy", ["ustg%d" % us], ["U8b%d" % us], out=U8b[us][:], in_=ustg[us][:])
        for d in range(2):
            dg = d * 8 + g
            tb = it % 2
            it += 1
            tk = ["tabA", "tabB"][tb]
            sinT, cosT, rT = tabs[tb]
            xs = (g % 2) * 2 + d
            xk = "Xin%d" % xs
            ts(sc_ta[:], cidx[:], thr[:, dg:dg + 1], ALU.mult, r=PR, w=["sc"])
            sincos(sc_ta[:], sinT[:], cosT[:], 1024, r=["sc", tk], w=["sc", tk], ta=G[16], tkf=G[17])
            ts(rT[:], cidx[:], 0.0, ALU.mult, s2=r8[:, dg:dg + 1], op1=ALU.add, r=PR + [tk], w=[tk])
            op_a, op_b = (ALU.add, ALU.subtract) if d == 0 else (ALU.subtract, ALU.add)
            for h in range(2):
                hs = slice(h * 512, (h + 1) * 512)
                o1, o1k = ring.next()
                o2, o2k = ring.next()
                P.I(T_, "matmul", PR + ["U8b%d" % us], [o1k], o1[:], lhsT=W1[:, dg, :], rhs=U8b[us][:, hs],
                    start=True, stop=True)
                P.I(T_, "matmul", PR + ["U8b%d" % us], [o2k], o2[:], lhsT=W2[:, dg, :], rhs=U8b[us][:, hs],
                    start=True, stop=True)
                tt(tA[:, hs], cosT[:, hs], o1[:], ALU.mult, r=[tk, o1k], w=["ta"])
                tt(tBb[:, hs], sinT[:, hs], o2[:], ALU.mult, r=[tk, o2k], w=["tb"])
                tt(S1[:, hs], tA[:, hs], tBb[:, hs], op_a, r=["ta", "tb"], w=["S1"], eng=G_)
                tt(tA2[:, hs], cosT[:, hs], o2[:], ALU.mult, r=[tk, o2k], w=["ta2"])
                tt(tB2[:, hs], sinT[:, hs], o1[:], ALU.mult, r=[tk, o1k], w=["tb2"])
                tt(S2[:, hs], tA2[:, hs], tB2[:, hs], op_b, r=["ta2", "tb2"], w=["S2"], eng=G_)
            rv = slice(None, None, -1) if d == 1 else slice(None)
            P.I(V, "tensor_tensor_scan", ["S1", tk], ["Z1"], out=Z1[:, rv], data0=rT[:, rv], data1=S1[:, rv],
                initial=0.0, op0=ALU.mult, op1=ALU.add)
            P.I(V, "tensor_tensor_scan", ["S2", tk], ["Z2"], out=Z2[:, rv], data0=rT[:, rv], data1=S2[:, rv],
                initial=0.0, op0=ALU.mult, op1=ALU.add)
            tt(tA[:], cosT[:], Z1[:], ALU.mult, r=[tk, "Z1"], w=["ta"])
            tt(tBb[:], sinT[:], Z2[:], ALU.mult, r=[tk, "Z2"], w=["tb"], eng=G_)
            if d == 0:
                P.I(V, "memset", [], [xk], Xin[xs][:, 0:1], 0.0)
                tt(Xin[xs][:, 1:1024], tA[:, 0:1023], tBb[:, 0:1023], ALU.subtract, r=["ta", "tb"], w=[xk])
            else:
                P.I(V, "memset", [], [xk], Xin[xs][:, 1023:1024], 0.0)
                tt(Xin[xs][:, 0:1023], tA[:, 1:1024], tBb[:, 1:1024], ALU.add, r=["ta", "tb"], w=[xk])
        for h in range(2):
            hs = slice(h * 512, (h + 1) * 512)
            x0 = (g % 2) * 2
            P.I(T_, "matmul", PR + ["U8b%d" % us], ["pso%d" % h], pso[h][:], lhsT=Kc[:, g, :], rhs=U8b[us][:, hs],
                start=True, stop=False)
            P.I(T_, "matmul", PR + ["Xin%d" % x0], ["pso%d" % h], pso[h][:], lhsT=Cmat[:, 0, g, :],
                rhs=Xin[x0][:, hs], start=False, stop=False)
            P.I(T_, "matmul", PR + ["Xin%d" % (x0 + 1)], ["pso%d" % h], pso[h][:], lhsT=Cmat[:, 1, g, :],
                rhs=Xin[x0 + 1][:, hs], start=False, stop=True)
            P.I(A_, "copy", ["pso%d" % h], ["ost%d" % h], out=ost[h][:], in_=pso[h][:])
            P.dma("sync", y8[g, :, hs], ost[h][:], reads=["ost%d" % h], key="ost%d" % h)
    P.emit()
    return nc


def consts_S():
    q = np.arange(128)
    epart = np.stack([7 - q // 16, q // 16], 1).astype(np.float32)
    ktab = np.tile(np.arange(-7, 9, dtype=np.float32)[None], (128, 1))
    sel = np.stack([(q < 64), -(q >= 64).astype(np.float32), (q >= 64)], 1).astype(np.float32)
    ii = q // 16
    ident = np.eye(128, dtype=np.float32)
    maskF = (ii[None, :] >= ii[:, None]).astype(np.float32)
    maskB = (ii[:, None] >= ii[None, :]).astype(np.float32)
    mats = np.ascontiguousarray(np.stack([ident, maskF, maskB], 1))
    cidx = np.tile(np.arange(1024, dtype=np.float32)[None], (128, 1))
    return epart, ktab, sel, mats, cidx


def prep_S(inp, l, c, uT_core):
    gs = slice(8 * c, 8 * c + 8)
    epart, ktab, sel, mats, cidx = consts_S()
    b_re, b_im = inp["ssm_b_re"][l][:, gs], inp["ssm_b_im"][l][:, gs]
    c_re, c_im = inp["ssm_c_re"][l][:, gs], inp["ssm_c_im"][l][:, gs]
    a_re, a_im, ls = inp["ssm_a_re"][l][:, gs], inp["ssm_a_im"][l][:, gs], inp["ssm_log_step"][l][:, gs]

    def bside(b):
        t = b.transpose(3, 0, 1, 2).reshape(16, 1024)
        return np.tile(t, (8, 1))
    bT = np.stack([bside(b_re), bside(b_im)], 1)
    flat = lambda a: np.broadcast_to(a.reshape(1, 1024), (128, 1024))
    aB = np.stack([flat(a_re), flat(a_im), flat(np.broadcast_to(ls[:, :, None], (2, 8, 64)))], 1)

    def cside_c(cc):
        t = cc.transpose(3, 0, 1, 2).reshape(64, 16, 16)
        return np.concatenate([t, t], 0)

    def cside_b(b):
        t = b.transpose(2, 0, 1, 3).reshape(64, 16, 16)
        return np.concatenate([t, t], 0)
    cP = np.stack([cside_c(c_re), cside_c(c_im), cside_b(b_re), cside_b(b_im)], 1)

    def cside_a(a):
        t = a.transpose(2, 0, 1).reshape(64, 16)
        return np.concatenate([t, t], 0)
    aC = np.stack([cside_a(a_re), cside_a(a_im), cside_a(np.broadcast_to(ls[:, :, None], (2, 8, 64)))], 1)
    dsk = np.tile(inp["ssm_d"][l][128 * c:128 * (c + 1)].reshape(8, 16).T, (8, 1))
    U8 = uT_core.reshape(8, 16, 1024, 8).transpose(0, 3, 1, 2).reshape(8, 128, 1024)
    f = lambda a: np.ascontiguousarray(a, dtype=np.float32)
    return dict(U8=f(U8), bT=f(bT), aB=f(aB), epart=f(epart), cP=f(cP), aC=f(aC), ktab=f(ktab), sel=f(sel),
                dsk=f(dsk), mats=f(mats), cidx=f(cidx))


def unpack_y8(y8):
    return np.ascontiguousarray(y8.reshape(8, 8, 16, 1024).transpose(0, 2, 3, 1).reshape(128, L))


_PROGS = {}


def _prog(name):
    if name not in _PROGS:
        _PROGS[name] = dict(P=build_P, S=build_S, A=build_A, M=build_M, E=build_E, C=build_C)[name]()
    return _PROGS[name]


def _run(name, maps):
    res = run_bass_kernel_spmd(_prog(name), maps, core_ids=list(range(NCORES)))
    return res.results


def _c(a, dt=None):
    return np.ascontiguousarray(a, dtype=dt)


def kernel(**inp):
    inp = {k: np.asarray(v) for k, v in inp.items()}
    x = _c(inp["x"][0], np.float32)
    ctab, stab, pm, bd, ident = consts_A()
    tokid, tri, _ = consts_E()
    for l in range(2):
        xT = _c(x.T)
        cs = lambda a, c: _c(a[:, c * TPC:(c + 1) * TPC])
        g = _c(inp["mix_norm_g"][l].reshape(16, 128).T)
        W = _c(inp["w_in"][l])
        r = _run("P", [dict(xT=cs(xT, c), g=g, W=W) for c in range(NCORES)])
        projT = np.concatenate([q["projT"] for q in r], axis=1)
        r = _run("S", [prep_S(inp, l, c, projT[128 * c:128 * (c + 1)]) for c in range(NCORES)])
        ysT = np.concatenate([unpack_y8(q["y8"]) for q in r], axis=0)
        lam_init = 0.8 - 0.6 * math.exp(-0.3 * l)
        small = np.zeros((128, 8), np.float32)
        small[:, 0] = np.tile(inp["q_norm_g"][l], 2)
        small[:, 1] = np.tile(inp["k_norm_g"][l], 2)
        small[:, 2] = inp["subln_g"][l]
        small[:, 3] = lam_init
        small[:, 4] = 1.0 - lam_init
        lamin = _c(np.broadcast_to(np.stack([inp["lambda_q1"][l], inp["lambda_k1"][l], inp["lambda_q2"][l],
                                             inp["lambda_k2"][l]])[None], (128, 4, 64)), np.float32)
        r = _run("A", [dict(qT=_c(projT[1024 + h * 128:1024 + (h + 1) * 128]),
                            kT=_c(projT[2048 + h * 128:2048 + (h + 1) * 128]),
                            vT=_c(projT[3072 + h * 128:3072 + (h + 1) * 128]), ctab=ctab, stab=stab, small=small,
                            lamin=lamin, pmd=pm, bdd=bd, identd=ident) for h in range(NCORES)])
        yaT = np.concatenate([q["oT"] for q in r], axis=0)
        glT = projT[4096:8192]
        gate_b = _c(inp["gate_b"][l].reshape(2, 16, 128).transpose(2, 0, 1).reshape(128, 32))
        gffn = _c(inp["ffn_norm_g"][l].reshape(16, 128).T)
        w_r = _c(inp["w_router"][l].reshape(16, 128, 16).transpose(1, 0, 2))
        r = _run("M", [dict(ysT=cs(ysT, c), yaT=cs(yaT, c), glT=cs(glT, c), xT=cs(xT, c), w_glu=_c(inp["w_glu"][l]),
                            w_brs=_c(inp["w_br_ssm"][l]), w_bra=_c(inp["w_br_attn"][l]), w_out=_c(inp["w_out"][l]),
                            gate_b=gate_b, gffn=gffn, w_r=w_r) for c in range(NCORES)])
        x1 = _c(np.concatenate([q["x1T"] for q in r], axis=1).T)
        xn = _c(np.concatenate([q["xnT"] for q in r], axis=1).T)
        aff = _c(np.concatenate([q["affT"] for q in r], axis=1).T)
        maps = []
        for c in range(NCORES):
            affc = _c(aff[:, 2 * c:2 * c + 2].T.reshape(2, 128, 64))
            ebase = _c(np.tile(np.array([[2 * c * 1024, (2 * c + 1) * 1024]], np.float32), (128, 1)))
            maps.append(dict(affc=affc, xn=xn, wg=_c(inp["w_e_gate"][l][2 * c:2 * c + 2]),
                             wu=_c(inp["w_e_up"][l][2 * c:2 * c + 2]), wd=_c(inp["w_e_down"][l][2 * c:2 * c + 2]),
                             tokid=tokid, tri=tri, ebase=ebase, identd=ident))
        r = _run("E", maps)
        yeT = np.stack([q["yeT"] for q in r]).reshape(16, D, 1024)
        posrow = np.stack([q["posrow"] for q in r]).reshape(16, L)
        ye = _c(yeT.reshape(16, D, 8, 128).transpose(0, 3, 2, 1).reshape(16 * 1024, D))
        prow = _c(posrow.T)
        rs = lambda a, c: _c(a[c * TPC:(c + 1) * TPC])
        r = _run("C", [dict(x1=rs(x1, c), ye=ye, prow=rs(prow, c), aff=rs(aff, c)) for c in range(NCORES)])
        x = np.concatenate([q["x2"] for q in r], axis=0)
    return x[None].astype(np.float32)
```
